# Optimizing a Trainium2 kernel written in Bass

```python
import math
import jax
import jax.numpy as jnp
from jax import lax
import numpy as np

D_MODEL = 2048
BATCH = 4
SEQ = 4096
DEPTH = 4

CHUNK = 128
EPS = 1e-6
D_FF = 4 * D_MODEL
N_RET_LAYERS = (DEPTH + 1) // 2
N_MLSTM_LAYERS = DEPTH // 2

RET_HEADS = D_MODEL // 256
RET_DK = D_MODEL // RET_HEADS
RET_QK = RET_HEADS * RET_DK
RET_V = 2 * D_MODEL
RET_DV = RET_V // RET_HEADS
RET_IN = 2 * RET_QK + 2 * RET_V
ROPE_BASE = 10000.0

ML_HEADS = 8
ML_QK = D_MODEL // 2
ML_DQK = ML_QK // ML_HEADS
ML_V = D_MODEL
ML_DV = ML_V // ML_HEADS
ML_IN = 2 * ML_QK + 2 * ML_V + 2 * ML_HEADS
GATE_SOFTCAP = 15.0

kernel_name = "hybrid_retention_mlstm_sqrelu_sandwich"


def rms_norm(x, g):
    xf = x.astype(jnp.float32)
    y = xf * lax.rsqrt(jnp.mean(xf * xf, axis=-1, keepdims=True) + EPS)
    return (y * g.astype(jnp.float32)).astype(x.dtype)


def rotary(t, cos, sin):
    t1, t2 = jnp.split(t, 2, axis=-1)
    return jnp.concatenate([t1 * cos - t2 * sin, t2 * cos + t1 * sin], axis=-1)


def to_chunks(t):
    b, s, h, d = t.shape
    return t.reshape(b, s // CHUNK, CHUNK, h, d).transpose(1, 0, 3, 2, 4)


def gate_chunks(t):
    b, s, h = t.shape
    return t.reshape(b, s // CHUNK, CHUNK, h).transpose(1, 0, 3, 2)


def from_chunks(t):
    nc, b, h, c, d = t.shape
    return t.transpose(1, 0, 3, 2, 4).reshape(b, nc * c, h, d)


def retention(h, w_in, w_out, cos, sin):
    b, s, _ = h.shape
    proj = h @ w_in
    q, k, v, g = jnp.split(proj, [RET_QK, 2 * RET_QK, 2 * RET_QK + RET_V], axis=-1)
    q = rotary(q.reshape(b, s, RET_HEADS, RET_DK).astype(jnp.float32), cos, sin)
    k = rotary(k.reshape(b, s, RET_HEADS, RET_DK).astype(jnp.float32), cos, sin) * (RET_DK ** -0.5)
    v = v.reshape(b, s, RET_HEADS, RET_DV).astype(jnp.float32)

    log_gamma = jnp.log1p(-jnp.power(2.0, -5.0 - jnp.arange(RET_HEADS, dtype=jnp.float32)))
    idx = jnp.arange(CHUNK, dtype=jnp.float32)
    rel = idx[:, None] - idx[None, :]
    decay = jnp.where(rel >= 0, jnp.exp(jnp.maximum(rel, 0.0) * log_gamma[:, None, None]), 0.0)
    xi = jnp.exp((idx + 1.0) * log_gamma[:, None])[..., None]
    zeta = jnp.exp((CHUNK - 1.0 - idx) * log_gamma[:, None])[..., None]
    g_chunk = jnp.exp(CHUNK * log_gamma)[:, None, None]

    def step(state, xs):
        qc, kc, vc = xs
        scores = jnp.einsum('bhnd,bhmd->bhnm', qc, kc) * decay
        inner = jnp.einsum('bhnm,bhme->bhne', scores, vc)
        cross = jnp.einsum('bhnd,bhde->bhne', qc, state) * xi
        new_state = state * g_chunk + jnp.einsum('bhmd,bhme->bhde', kc * zeta, vc)
        return new_state, inner + cross

    state0 = jnp.zeros((b, RET_HEADS, RET_DK, RET_DV), jnp.float32)
    _, y = lax.scan(step, state0, (to_chunks(q), to_chunks(k), to_chunks(v)))
    y = from_chunks(y)
    mu = jnp.mean(y, axis=-1, keepdims=True)
    var = jnp.mean(jnp.square(y - mu), axis=-1, keepdims=True)
    y = ((y - mu) * lax.rsqrt(var + EPS)).reshape(b, s, RET_V).astype(h.dtype)
    return (jax.nn.silu(g) * y) @ w_out


def mlstm(h, w_in, b_gate, norm_g, w_out):
    b, s, _ = h.shape
    proj = h @ w_in
    q, k, v, o, gates = jnp.split(
        proj, [ML_QK, 2 * ML_QK, 2 * ML_QK + ML_V, 2 * ML_QK + 2 * ML_V], axis=-1)
    gates = gates.astype(jnp.float32) + b_gate.astype(jnp.float32)
    gates = GATE_SOFTCAP * jnp.tanh(gates / GATE_SOFTCAP)
    i_log = gates[..., :ML_HEADS]
    f_log = jax.nn.log_sigmoid(gates[..., ML_HEADS:])
    q = q.reshape(b, s, ML_HEADS, ML_DQK).astype(jnp.float32)
    k = k.reshape(b, s, ML_HEADS, ML_DQK).astype(jnp.float32) * (ML_DQK ** -0.5)
    v = v.reshape(b, s, ML_HEADS, ML_DV).astype(jnp.float32)
    causal = jnp.tril(jnp.ones((CHUNK, CHUNK), dtype=bool))

    def step(carry, xs):
        c_st, n_st, m_st = carry
        qc, kc, vc, ic, fc = xs
        bcum = jnp.cumsum(fc, axis=-1)
        d_log = bcum[..., :, None] - bcum[..., None, :] + ic[..., None, :]
        d_log = jnp.where(causal, d_log, -jnp.inf)
        inter_log = bcum + m_st[..., None]
        m_out = jnp.maximum(inter_log, jnp.max(d_log, axis=-1))
        d_w = jnp.exp(d_log - m_out[..., None])
        inter_w = jnp.exp(inter_log - m_out)
        scores = jnp.einsum('bhnd,bhsd->bhns', qc, kc) * d_w
        num = jnp.einsum('bhns,bhse->bhne', scores, vc) \
            + inter_w[..., None] * jnp.einsum('bhnd,bhde->bhne', qc, c_st)
        den = jnp.sum(scores, axis=-1) + inter_w * jnp.einsum('bhnd,bhd->bhn', qc, n_st)
        h_out = num / jnp.maximum(jnp.abs(den), jnp.exp(-m_out))[..., None]
        b_tot = bcum[..., -1]
        w_log = b_tot[..., None] - bcum + ic
        m_new = jnp.maximum(b_tot + m_st, jnp.max(w_log, axis=-1))
        w_s = jnp.exp(w_log - m_new[..., None])
        carry_decay = jnp.exp(b_tot + m_st - m_new)
        kw = kc * w_s[..., None]
        c_new = carry_decay[..., None, None] * c_st + jnp.einsum('bhsd,bhse->bhde', kw, vc)
        n_new = carry_decay[..., None] * n_st + jnp.sum(kw, axis=2)
        return (c_new, n_new, m_new), h_out

    carry0 = (jnp.zeros((b, ML_HEADS, ML_DQK, ML_DV), jnp.float32),
              jnp.zeros((b, ML_HEADS, ML_DQK), jnp.float32),
              jnp.zeros((b, ML_HEADS), jnp.float32))
    _, y = lax.scan(step, carry0, (to_chunks(q), to_chunks(k), to_chunks(v),
                                   gate_chunks(i_log), gate_chunks(f_log)))
    y = from_chunks(y)
    y = y * lax.rsqrt(jnp.mean(y * y, axis=-1, keepdims=True) + EPS)
    y = (y.reshape(b, s, ML_V) * norm_g.astype(jnp.float32)).astype(h.dtype)
    return (y * jax.nn.sigmoid(o)) @ w_out


def sqrelu_mlp(h, w1, w2):
    return jnp.square(jax.nn.relu(h @ w1)) @ w2


def setup_inputs(seed: int = 0) -> dict:
    key = jax.random.key(seed)
    ks = jax.random.split(key, 12)
    f32 = jnp.float32
    x = jax.random.normal(ks[0], (BATCH, SEQ, D_MODEL), f32)
    positions = jnp.broadcast_to(jnp.arange(SEQ, dtype=jnp.int32), (BATCH, SEQ))
    norm_g = 1.0 + 0.05 * jax.random.normal(ks[1], (DEPTH, 4, D_MODEL), f32)
    ret_w_in = jax.random.normal(ks[2], (N_RET_LAYERS, D_MODEL, RET_IN), f32) * D_MODEL ** -0.5
    ret_w_out = jax.random.normal(ks[3], (N_RET_LAYERS, RET_V, D_MODEL), f32) * RET_V ** -0.5
    mlstm_w_in = jax.random.normal(ks[4], (N_MLSTM_LAYERS, D_MODEL, ML_IN), f32) * D_MODEL ** -0.5
    i_bias = 0.1 * jax.random.normal(ks[5], (N_MLSTM_LAYERS, ML_HEADS), f32)
    f_bias = jnp.linspace(3.0, 6.0, ML_HEADS, dtype=f32)[None, :] \
        + 0.1 * jax.random.normal(ks[6], (N_MLSTM_LAYERS, ML_HEADS), f32)
    mlstm_b_gate = jnp.concatenate([i_bias, f_bias], axis=-1)
    mlstm_norm_g = 1.0 + 0.05 * jax.random.normal(ks[7], (N_MLSTM_LAYERS, ML_V), f32)
    mlstm_w_out = jax.random.normal(ks[8], (N_MLSTM_LAYERS, ML_V, D_MODEL), f32) * ML_V ** -0.5
    mlp_w1 = jax.random.normal(ks[9], (DEPTH, D_MODEL, D_FF), f32) * D_MODEL ** -0.5
    mlp_w2 = jax.random.normal(ks[10], (DEPTH, D_FF, D_MODEL), f32) * D_FF ** -0.5
    return {"x": x, "positions": positions, "norm_g": norm_g,
            "ret_w_in": ret_w_in, "ret_w_out": ret_w_out,
            "mlstm_w_in": mlstm_w_in, "mlstm_b_gate": mlstm_b_gate,
            "mlstm_norm_g": mlstm_norm_g, "mlstm_w_out": mlstm_w_out,
            "mlp_w1": mlp_w1, "mlp_w2": mlp_w2}


def reference(x, positions, norm_g, ret_w_in, ret_w_out, mlstm_w_in, mlstm_b_gate,
              mlstm_norm_g, mlstm_w_out, mlp_w1, mlp_w2):
    inv_freq = jnp.power(ROPE_BASE, -jnp.linspace(0.0, 1.0, RET_DK // 2, dtype=jnp.float32))
    ang = positions.astype(jnp.float32)[..., None, None] * inv_freq
    cos, sin = jnp.cos(ang), jnp.sin(ang)
    for i in range(DEPTH):
        g = norm_g[i]
        h = rms_norm(x, g[0])
        if i % 2 == 0:
            j = i // 2
            y = retention(h, ret_w_in[j], ret_w_out[j], cos, sin)
        else:
            j = i // 2
            y = mlstm(h, mlstm_w_in[j], mlstm_b_gate[j], mlstm_norm_g[j], mlstm_w_out[j])
        x = x + rms_norm(y, g[1])
        y = sqrelu_mlp(rms_norm(x, g[2]), mlp_w1[i], mlp_w2[i])
        x = x + rms_norm(y, g[3])
    return x
```

```python
import math
from contextlib import ExitStack
import numpy as np
import ml_dtypes
import concourse.bass as bass
import concourse.mybir as mybir
from concourse.bass_utils import run_bass_kernel_spmd

F32 = mybir.dt.float32
BF16 = mybir.dt.bfloat16
I32 = mybir.dt.int32
AF = mybir.ActivationFunctionType
ALU = mybir.AluOpType
AX = mybir.AxisListType

D = 2048
DC = 16
DFF = 8192
EPS = 1e-6
RET_H = 8
RET_DK = 256
RET_DV = 512
RET_IN = 12288
ML_H = 8
ML_DQK = 128
ML_DV = 256
ML_IN = 6160
SOFTCAP = 15.0
PI = math.pi


class Buf:
    __slots__ = ("name", "w", "r")

    def __init__(self, name=""):
        self.name = name
        self.w = None
        self.r = {}


class Chan:
    def __init__(self, name, sem, step):
        self.name = name
        self.sem = sem
        self.step = step
        self.cnt = 0


class Tracker:
    def __init__(self, nc, stack):
        self.nc = nc
        self.stack = stack
        self.engs = {"pe": nc.tensor, "act": nc.scalar, "dve": nc.vector,
                     "pool": nc.gpsimd, "sp": nc.sync}
        self.chan = {}
        for n in self.engs:
            sem = stack.enter_context(nc.semaphore("s_" + n))
            self.chan[n] = Chan(n, sem, 1)
        self.waited = {n: {} for n in self.engs}
        self.ninst = {n: 0 for n in self.engs}
        self.nwait = 0
        self.dma_pool = {}

    def dma_chan(self, name):
        if name in self.dma_pool:
            return self.dma_pool[name]
        sem = self.stack.enter_context(self.nc.semaphore("d_" + name))
        c = Chan("d_" + name, sem, 16)
        self.chan[c.name] = c
        self.dma_pool[name] = c
        return c

    def _deps(self, eng, reads, writes):
        deps = {}

        def add(c, n):
            if deps.get(c, 0) < n:
                deps[c] = n
        for b in reads:
            if b.w is not None:
                add(*b.w)
        for b in writes:
            if b.w is not None and b.w[0] != eng:
                add(*b.w)
            for c, n in b.r.items():
                if c != eng:
                    add(c, n)
        return deps

    def _emit_waits(self, eng, deps):
        e = self.engs[eng]
        wd = self.waited[eng]
        for c, n in deps.items():
            if c == eng and eng == "pe":
                continue
            if self.chan[c].step == 16:
                n = max(n, self.chan[c].cnt)
            if wd.get(c, 0) >= n:
                continue
            e.wait_ge(self.chan[c].sem, n)
            wd[c] = n
            self.nwait += 1

    def op(self, eng, fn, reads=(), writes=(), signal=True):
        deps = self._deps(eng, reads, writes)
        self._emit_waits(eng, deps)
        ins = fn(self.engs[eng])
        ch = self.chan[eng]
        self.ninst[eng] += 1
        if signal:
            ch.cnt += 1
            ins.then_inc(ch.sem, 1)
            tag = (eng, ch.cnt)
        else:
            tag = (eng, ch.cnt + 1)
        for b in writes:
            b.w = tag
            b.r = {}
        for b in reads:
            if b.r.get(eng, 0) < tag[1]:
                b.r[eng] = tag[1]
        return ins

    def dma(self, q, ch, pairs, reads=(), writes=(), **kw):
        deps = self._deps("__dma__", reads, writes)
        self._emit_waits(q, deps)
        e = self.engs[q]
        for (o, i) in pairs:
            e.dma_start(out=o, in_=i, **kw).then_inc(ch.sem, 16)
            ch.cnt += 16
            self.ninst[q] += 1
        tag = (ch.name, ch.cnt)
        for b in writes:
            b.w = tag
            b.r = {}
        for b in reads:
            if b.r.get(ch.name, 0) < tag[1]:
                b.r[ch.name] = tag[1]

    def barrier(self):
        for eng in self.engs:
            deps = {}
            for c in self.chan.values():
                if c.cnt > 0 and c.name != eng:
                    deps[c.name] = c.cnt
            self._emit_waits(eng, deps)


def host_consts():
    c = {}
    idx = np.arange(128)
    c["ones_f"] = np.ones((128, 128), np.float32)
    c["ident_f"] = np.eye(128, dtype=np.float32)
    c["ident_b"] = np.eye(128, dtype=np.float32).astype(ml_dtypes.bfloat16)
    c["tri_f"] = (idx[:, None] <= idx[None, :]).astype(np.float32)
    c["negm_f"] = np.where(idx[:, None] <= idx[None, :], 0.0, -30000.0).astype(np.float32)
    lg = np.log1p(-np.power(2.0, -5.0 - np.arange(RET_H, dtype=np.float64)))
    rel = (idx[None, :] - idx[:, None]).astype(np.float64)
    decT = np.where(rel[None] >= 0, np.exp(np.maximum(rel[None], 0) * lg[:, None, None]), 0.0)
    c["decayT"] = np.ascontiguousarray(decT.transpose(1, 0, 2)).astype(np.float32)
    xi = np.exp((idx[None, :] + 1.0) * lg[:, None])
    c["xirep"] = np.ascontiguousarray(np.broadcast_to(xi[None], (128, RET_H, 128))).astype(np.float32)
    zeta = np.exp((127.0 - idx[:, None]) * lg[None, :])
    c["zeta"] = zeta.astype(np.float32)
    c["gchunk"] = [float(np.exp(128.0 * v)) for v in lg]
    invf = np.power(np.float32(10000.0), -np.linspace(0.0, 1.0, 128, dtype=np.float32)).astype(np.float32)
    c["invf_col"] = invf.reshape(128, 1).copy()
    c["invf_rep"] = np.ascontiguousarray(np.broadcast_to(invf[None, :], (128, 128))).astype(np.float32)
    return c


CONST_NAMES = ["ones_f", "ident_f", "ident_b", "tri_f", "negm_f", "decayT", "xirep", "zeta",
               "invf_col", "invf_rep"]


class P:
    pass


def build(T, n_layers=4, dbg=False):
    HC = host_consts()
    TG = min(T, 2048)
    NG = T // TG
    NCH = T // 128
    nc_real = bass.Bass("TRN2", target_bir_lowering=False)

    class NCW:
        def __init__(self, n):
            self._n = n
            self._uid = 0

        def __getattr__(self, a):
            return getattr(self._n, a)

        def sbuf_tensor(self, name, shape, dtype):
            self._uid += 1
            return self._n.sbuf_tensor(f"{name}_u{self._uid}", shape, dtype)

        def psum_tensor(self, name, shape, dtype):
            self._uid += 1
            return self._n.psum_tensor(f"{name}_u{self._uid}", shape, dtype)

    nc = NCW(nc_real)
    dt = nc.dram_tensor

    def din(name, shape, dtype):
        return dt(name, list(shape), dtype, kind="ExternalInput").ap()

    def dscr(name, shape, dtype):
        return dt(name, list(shape), dtype, kind="Internal").ap()

    xT_in = din("xT", [D, T], F32)
    pos_in = din("pos", [1, T], I32)
    postm_in = din("pos_tm", [128, NCH], I32)
    gcol_in = din("gcol", [128, 16, DC], F32)
    ret_w_in = din("ret_w_in", [2, D, RET_IN], F32)
    ret_w_out = din("ret_w_out", [2, 2 * D, D], F32)
    ml_w_in = din("ml_w_in", [2, D, ML_IN], F32)
    ml_bg = din("ml_bg", [128, 2, 16], F32)
    ml_ng = din("ml_ng", [128, 2, D], F32)
    ml_w_out = din("ml_w_out", [2, D, D], F32)
    w1_in = din("mlp_w1", [4, D, DFF], F32)
    w2_in = din("mlp_w2", [4, DFF, D], F32)
    cin = {n: din("c_" + n, HC[n].shape, BF16 if n == "ident_b" else F32) for n in CONST_NAMES}
    outT = dt("outT", [D, T], F32, kind="ExternalOutput").ap()

    xT_d = dscr("xT_d", [D, T], F32)
    yT_d = dscr("yT_d", [D, T], F32)
    qT_d = dscr("qT_d", [D, T], BF16)
    kT_d = dscr("kT_d", [D, T], BF16)
    ktm_d = dscr("ktm_d", [T, D], BF16)
    vtm_d = dscr("vtm_d", [T, 2 * D], BF16)
    gtm_d = dscr("gtm_d", [T, 2 * D], BF16)
    zT_d = dscr("zT_d", [2 * D, T], BF16)
    uT_d = dscr("uT_d", [DFF, T], BF16)
    gate_d = dscr("gate_d", [T, 16], F32)
    cosT_d = dscr("cosT_d", [128, T], F32)
    sinT_d = dscr("sinT_d", [128, T], F32)
    costm_d = dscr("costm_d", [128, NCH, 128], F32)
    sintm_d = dscr("sintm_d", [128, NCH, 128], F32)

    st = ExitStack()
    with st:
        tr = Tracker(nc, st)
        sb = lambda name, shape, dtype: st.enter_context(nc.sbuf_tensor(name, list(shape), dtype))

        K = P()
        K.ones_f = sb("ones_f", [128, 128], F32)
        K.ident_f = sb("ident_f", [128, 128], F32)
        K.ident_b = sb("ident_b", [128, 128], BF16)
        K.gcol = sb("gcol", [128, 16, DC], F32)
        K.eps = sb("eps_t", [128, 1], F32)
        K.negpi = sb("negpi_t", [128, 1], F32)
        K.one = sb("one_t", [128, 1], F32)
        b_const = Buf("const")
        cch = tr.dma_chan("const")
        tr.dma("sp", cch, [(K.ones_f[:, :], cin["ones_f"]), (K.ident_f[:, :], cin["ident_f"]),
                           (K.ident_b[:, :], cin["ident_b"]), (K.gcol[:, :, :], gcol_in)], writes=[b_const])
        tr.op("dve", lambda e: e.memset(K.eps[:, :], EPS), writes=[b_const])
        tr.op("dve", lambda e: e.memset(K.negpi[:, :], -PI), writes=[b_const])
        tr.op("dve", lambda e: e.memset(K.one[:, :], 1.0), writes=[b_const])
        tr.barrier()

        DB = {n: Buf(n) for n in ["xT", "yT", "qT", "kT", "ktm", "vtm", "gtm", "zT", "uT", "gate", "tab", "out"]}

        psum_names = [f"ps{i}" for i in range(8)]

        def phase_tables():
            with ExitStack() as ph:
                t = lambda name, shape, dtype: ph.enter_context(nc.sbuf_tensor(name, list(shape), dtype))
                ch = tr.dma_chan("ph0")
                cho = tr.dma_chan("ph1")
                invc = t("invc", [128, 1], F32)
                invr = t("invr", [128, 128], F32)
                b0 = Buf()
                tr.dma("sp", ch, [(invc[:, :], cin["invf_col"]), (invr[:, :], cin["invf_rep"])], writes=[b0])
                pi_ = t("tb_pi", [128, 512], I32)
                pf = t("tb_pf", [128, 512], F32)
                an = t("tb_an", [128, 512], F32)
                rs = t("tb_rs", [128, 512], F32)
                so = t("tb_so", [128, 512], F32)
                co = t("tb_co", [128, 512], F32)
                bpi, bpf, ban, brs, bso, bco = (Buf() for _ in range(6))
                ki = t("tb_ki", [128, 512], I32)
                mm_ = t("tb_m", [128, 512], F32)
                bki, bmm = Buf(), Buf()

                def wrap_clamp():
                    tr.op("dve", lambda e: e.tensor_scalar(out=mm_[:, :], in0=rs[:, :], scalar1=PI, scalar2=-2 * PI,
                                                           op0=ALU.is_gt, op1=ALU.mult), reads=[brs], writes=[bmm])
                    tr.op("dve", lambda e: e.tensor_tensor(out=rs[:, :], in0=rs[:, :], in1=mm_[:, :], op=ALU.add),
                          reads=[brs, bmm], writes=[brs])
                    tr.op("dve", lambda e: e.tensor_scalar(out=rs[:, :], in0=rs[:, :], scalar1=-PI, scalar2=PI,
                                                           op0=ALU.max, op1=ALU.min), reads=[brs], writes=[brs])

                def sincos():
                    tr.op("dve", lambda e: e.tensor_scalar(out=mm_[:, :], in0=an[:, :], scalar1=1.0 / (2 * PI), scalar2=None,
                                                           op0=ALU.mult), reads=[ban], writes=[bmm])
                    tr.op("dve", lambda e: e.tensor_copy(out=ki[:, :], in_=mm_[:, :]), reads=[bmm], writes=[bki])
                    tr.op("dve", lambda e: e.tensor_copy(out=mm_[:, :], in_=ki[:, :]), reads=[bki], writes=[bmm])
                    tr.op("dve", lambda e: e.scalar_tensor_tensor(out=rs[:, :], in0=mm_[:, :], scalar=-2 * PI, in1=an[:, :],
                                                                  op0=ALU.mult, op1=ALU.add), reads=[bmm, ban], writes=[brs])
                    wrap_clamp()
                    tr.op("act", lambda e: e.activation(out=so[:, :], in_=rs[:, :], func=AF.Sin), reads=[brs], writes=[bso])
                    tr.op("dve", lambda e: e.tensor_scalar(out=rs[:, :], in0=rs[:, :], scalar1=0.5 * PI, scalar2=None,
                                                           op0=ALU.add), reads=[brs], writes=[brs])
                    wrap_clamp()
                    tr.op("act", lambda e: e.activation(out=co[:, :], in_=rs[:, :], func=AF.Sin), reads=[brs], writes=[bco])
                for i in range(T // 512):
                    sl = slice(i * 512, (i + 1) * 512)
                    tr.dma("sp", ch, [(pi_[:, :], pos_in[:, sl].partition_broadcast(128))], writes=[bpi])
                    tr.op("dve", lambda e: e.tensor_copy(out=pf[:, :], in_=pi_[:, :]), reads=[bpi], writes=[bpf])
                    tr.op("dve", lambda e: e.tensor_scalar(out=an[:, :], in0=pf[:, :], scalar1=invc[:, 0:1], scalar2=None,
                                                           op0=ALU.mult), reads=[bpf, b0], writes=[ban])
                    sincos()
                    tr.dma("sp", cho, [(sinT_d[:, sl], so[:, :])], reads=[bso], writes=[DB["tab"]])
                    tr.dma("sp", cho, [(cosT_d[:, sl], co[:, :])], reads=[bco], writes=[DB["tab"]])
                pti = t("tb_pti", [128, NCH], I32)
                ptf = t("tb_ptf", [128, NCH], F32)
                bpt = Buf()
                tr.dma("sp", ch, [(pti[:, :], postm_in)], writes=[bpt])
                tr.op("dve", lambda e: e.tensor_copy(out=ptf[:, :], in_=pti[:, :]), reads=[bpt], writes=[bpt])
                for i in range(NCH // 4):
                    for j in range(4):
                        c = i * 4 + j
                        tr.op("dve", lambda e: e.tensor_scalar(out=an[:, j * 128:(j + 1) * 128], in0=invr[:, :],
                                                               scalar1=ptf[:, c:c + 1], scalar2=None, op0=ALU.mult),
                              reads=[bpt, b0], writes=[ban])
                    sincos()
                    tr.dma("sp", cho, [(sintm_d[:, i * 4:(i + 1) * 4, :], so[:, :].rearrange("p (c f) -> p c f", c=4))],
                           reads=[bso], writes=[DB["tab"]])
                    tr.dma("sp", cho, [(costm_d[:, i * 4:(i + 1) * 4, :], co[:, :].rearrange("p (c f) -> p c f", c=4))],
                           reads=[bco], writes=[DB["tab"]])
                tr.barrier()

        def phase_norm(g, hT, src_x, y_src, gpost, gnext, dst_x):
            with ExitStack() as ph:
                t = lambda name, shape, dtype: ph.enter_context(nc.sbuf_tensor(name, list(shape), dtype))
                xs = t("n_xs", [128, DC, 512], F32)
                ys = t("n_ys", [128, DC, 512], F32) if y_src is not None else None
                sq = t("n_sq", [128, DC, 512], F32)
                rstd = t("n_rstd", [128, 512], F32)
                tmp = [t(f"n_tmp{i}", [128, 512], F32) for i in range(2)]
                ps = ph.enter_context(nc.psum_tensor("n_ps", [128, 512], F32))
                bx, by, bsq, brs, bps = Buf("xs"), Buf("ys"), Buf("sq"), Buf("rstd"), Buf("nps")
                btmp = [Buf(), Buf()]
                chx, chy, cho = tr.dma_chan("ph0"), tr.dma_chan("ph1"), tr.dma_chan("ph2")
                b_h = hT.buf if hT is not None else None

                def stats(src, bsrc):
                    for q4 in range(4):
                        tr.op("act", lambda e: e.activation(out=sq[:, q4 * 4:(q4 + 1) * 4, :], in_=src[:, q4 * 4:(q4 + 1) * 4, :],
                                                            func=AF.Square), reads=[bsrc], writes=[bsq])
                    for dc in range(DC):
                        tr.op("pe", lambda e: e.matmul(ps[:, :], K.ones_f[:, :], sq[:, dc, :], start=(dc == 0),
                                                       stop=(dc == DC - 1)), reads=[bsq, b_const], writes=[bps],
                              signal=(dc == DC - 1))
                    tr.op("act", lambda e: e.activation(out=rstd[:, :], in_=ps[:, :], func=AF.Sqrt, scale=1.0 / D,
                                                        bias=K.eps[:, 0:1]), reads=[bps, b_const], writes=[brs])
                    tr.op("dve", lambda e: e.reciprocal(out=rstd[:, :], in_=rstd[:, :]), reads=[brs], writes=[brs])

                for tb in range(TG // 512):
                    sl = slice(g * TG + tb * 512, g * TG + (tb + 1) * 512)
                    tr.dma("sp", chx, [(xs[:, :, :], src_x[:, sl].rearrange("(dc p) t -> p dc t", p=128))],
                           reads=[DB["xT"]], writes=[bx])
                    if y_src is not None:
                        tr.dma("sp", chy, [(ys[:, :, :], y_src[:, sl].rearrange("(dc p) t -> p dc t", p=128))],
                               reads=[DB["yT"]], writes=[by])
                        stats(ys, by)
                        for dc in range(DC):
                            tt = dc % 2
                            tr.op("dve", lambda e: e.scalar_tensor_tensor(out=tmp[tt][:, :], in0=ys[:, dc, :],
                                                                          scalar=K.gcol[:, gpost, dc:dc + 1], in1=rstd[:, :],
                                                                          op0=ALU.mult, op1=ALU.mult),
                                  reads=[by, brs, b_const], writes=[btmp[tt]])
                            tr.op("pool", lambda e: e.tensor_tensor(out=xs[:, dc, :], in0=xs[:, dc, :], in1=tmp[tt][:, :],
                                                                    op=ALU.add), reads=[bx, btmp[tt]], writes=[bx])
                    if dst_x is not None:
                        tr.dma("sp", cho, [(dst_x[:, sl].rearrange("(dc p) t -> p dc t", p=128), xs[:, :, :])],
                               reads=[bx], writes=[DB["out"] if dst_x is outT else DB["xT"]])
                    if gnext is not None:
                        stats(xs, bx)
                        for dc in range(DC):
                            tr.op("dve", lambda e: e.scalar_tensor_tensor(out=hT.t[:, dc, tb * 512:(tb + 1) * 512],
                                                                          in0=xs[:, dc, :], scalar=K.gcol[:, gnext, dc:dc + 1],
                                                                          in1=rstd[:, :], op0=ALU.mult, op1=ALU.mult),
                                  reads=[bx, brs, b_const], writes=[b_h])
                tr.barrier()

        def load_w_panel(wt, bw, chw, W, c0, ncols, KC):
            Wv = W.rearrange("(kc p) n -> p kc n", p=128)
            pairs = []
            for k0 in range(0, KC, 16):
                k1 = min(KC, k0 + 16)
                pairs.append((wt[:, k0:k1, 0:ncols], Wv[:, k0:k1, c0:c0 + ncols]))
            tr.dma("pool", chw, pairs, writes=[bw])

        def gemm_fm(g, hT, W, panels, epilogue, tag):
            with ExitStack() as ph:
                t = lambda name, shape, dtype: ph.enter_context(nc.sbuf_tensor(name, list(shape), dtype))
                wt = [t(f"gw{i}", [128, DC, 256], BF16) for i in range(2)]
                bw = [Buf(), Buf()]
                chw = [tr.dma_chan("w0"), tr.dma_chan("w1")]
                ps = [ph.enter_context(nc.psum_tensor(f"gps{i}", [128, 512], F32)) for i in range(8)]
                bps = [Buf() for _ in range(8)]
                NTB = TG // 512
                env = epilogue("init", ph, t)
                load_w_panel(wt[0], bw[0], chw[0], W, panels[0], 256, DC)
                for pi, c0 in enumerate(panels):
                    s = pi % 2
                    if pi + 1 < len(panels):
                        load_w_panel(wt[1 - s], bw[1 - s], chw[1 - s], W, panels[pi + 1], 256, DC)
                    for tb in range(NTB):
                        for fb in range(2):
                            bank = (tb % 4) * 2 + fb
                            for kc in range(DC):
                                tr.op("pe", lambda e: e.matmul(ps[bank][:, :], wt[s][:, kc, fb * 128:(fb + 1) * 128],
                                                               hT.t[:, kc, tb * 512:(tb + 1) * 512], start=(kc == 0),
                                                               stop=(kc == DC - 1)),
                                      reads=[bw[s], hT.buf], writes=[bps[bank]], signal=(kc == DC - 1))
                        b0, b1 = (tb % 4) * 2, (tb % 4) * 2 + 1
                        epilogue("ep", env, (pi, c0, g * TG + tb * 512, ps[b0], bps[b0], ps[b1], bps[b1]))
                tr.barrier()

        def gemm_tm(g, hT, W, panels, epilogue):
            with ExitStack() as ph:
                t = lambda name, shape, dtype: ph.enter_context(nc.sbuf_tensor(name, list(shape), dtype))
                wt = [t(f"gw{i}", [128, DC, 512], BF16) for i in range(2)]
                bw = [Buf(), Buf()]
                chw = [tr.dma_chan("w0"), tr.dma_chan("w1")]
                ps = [ph.enter_context(nc.psum_tensor(f"gps{i}", [128, 512], F32)) for i in range(8)]
                bps = [Buf() for _ in range(8)]
                NTT = TG // 128
                env = epilogue("init", ph, t)
                load_w_panel(wt[0], bw[0], chw[0], W, panels[0][0], panels[0][1], DC)
                for pi, (c0, ncol) in enumerate(panels):
                    s = pi % 2
                    if pi + 1 < len(panels):
                        load_w_panel(wt[1 - s], bw[1 - s], chw[1 - s], W, panels[pi + 1][0], panels[pi + 1][1], DC)
                    for tt in range(NTT):
                        bank = tt % 8
                        for kc in range(DC):
                            tr.op("pe", lambda e: e.matmul(ps[bank][:, 0:ncol], hT.t[:, kc, tt * 128:(tt + 1) * 128],
                                                           wt[s][:, kc, 0:ncol], start=(kc == 0), stop=(kc == DC - 1)),
                                  reads=[bw[s], hT.buf], writes=[bps[bank]], signal=(kc == DC - 1))
                        epilogue("ep", env, (pi, c0, ncol, g * TG + tt * 128, ps[bank], bps[bank]))
                tr.barrier()

        def gemm_stream(g, aT_d, b_a, KC, W, y_d, b_y):
            with ExitStack() as ph:
                t = lambda name, shape, dtype: ph.enter_context(nc.sbuf_tensor(name, list(shape), dtype))
                wt = [t(f"sw{i}", [128, KC, 256], BF16) for i in range(2)]
                bw = [Buf(), Buf()]
                chw = [tr.dma_chan("w0"), tr.dma_chan("w1")]
                NA = 4
                at = [t(f"sa{i}", [128, TG], BF16) for i in range(NA)]
                ba = [Buf() for _ in range(NA)]
                cha = [tr.dma_chan(f"a{i}") for i in range(NA)]
                ot = [t(f"so{i}", [128, 512], F32) for i in range(4)]
                bo = [Buf() for _ in range(4)]
                cho = [tr.dma_chan(f"o{i}") for i in range(4)]
                ps = [ph.enter_context(nc.psum_tensor(f"gps{i}", [128, 512], F32)) for i in range(8)]
                bps = [Buf() for _ in range(8)]
                NTB = TG // 512
                av = aT_d.rearrange("(kc p) t -> p kc t", p=128)
                npan = D // 256
                load_w_panel(wt[0], bw[0], chw[0], W, 0, 256, KC)
                ai = 0
                oi = 0
                for pi in range(npan):
                    s = pi % 2
                    if pi + 1 < npan:
                        load_w_panel(wt[1 - s], bw[1 - s], chw[1 - s], W, (pi + 1) * 256, 256, KC)
                    for kc in range(KC):
                        a = ai % NA
                        ai += 1
                        tr.dma("sp", cha[a], [(at[a][:, :], av[:, kc, g * TG:(g + 1) * TG])], reads=[b_a], writes=[ba[a]])
                        for fb in range(2):
                            for tb in range(NTB):
                                bank = fb * 4 + tb
                                tr.op("pe", lambda e: e.matmul(ps[bank][:, :], wt[s][:, kc, fb * 128:(fb + 1) * 128],
                                                               at[a][:, tb * 512:(tb + 1) * 512], start=(kc == 0),
                                                               stop=(kc == KC - 1)),
                                      reads=[bw[s], ba[a]], writes=[bps[bank]],
                                      signal=(kc == KC - 1 or (fb == 1 and tb == NTB - 1)))
                    for fb in range(2):
                        for tb in range(NTB):
                            bank = fb * 4 + tb
                            o = oi % 4
                            oi += 1
                            if o % 2 == 0:
                                tr.op("act", lambda e: e.activation(out=ot[o][:, :], in_=ps[bank][:, :], func=AF.Copy),
                                      reads=[bps[bank]], writes=[bo[o]])
                            else:
                                tr.op("dve", lambda e: e.tensor_copy(out=ot[o][:, :], in_=ps[bank][:, :]),
                                      reads=[bps[bank]], writes=[bo[o]])
                            r0 = pi * 256 + fb * 128
                            c0 = g * TG + tb * 512
                            tr.dma("sp", cho[o], [(y_d[r0:r0 + 128, c0:c0 + 512], ot[o][:, :])], reads=[bo[o]], writes=[b_y])
                tr.barrier()

        def ret_inproj(g, hT, W):
            def ep_fm(mode, env, a):
                if mode == "init":
                    ph, t = env, a
                    E = P()
                    E.cos = t("r_cos", [128, TG], F32)
                    E.sin = t("r_sin", [128, TG], F32)
                    E.btab = Buf()
                    ch = tr.dma_chan("ph0")
                    tr.dma("sp", ch, [(E.cos[:, :], cosT_d[:, g * TG:(g + 1) * TG]), (E.sin[:, :], sinT_d[:, g * TG:(g + 1) * TG])],
                           reads=[DB["tab"]], writes=[E.btab])
                    E.tmp = [t(f"r_t{i}", [128, 512], F32) for i in range(4)]
                    E.bt = [Buf() for _ in range(4)]
                    E.o = [t(f"r_o{i}", [128, 512], BF16) for i in range(4)]
                    E.bo = [Buf() for _ in range(4)]
                    E.cho = [tr.dma_chan(f"o{i}") for i in range(4)]
                    E.i = 0
                    return E
                E = env
                pi, c0, tok0, ps0, bp0, ps1, bp1 = a
                isk = c0 >= D
                scl = (RET_DK ** -0.5) if isk else 1.0
                dst = kT_d if isk else qT_d
                bdst = DB["kT"] if isk else DB["qT"]
                r0 = c0 - D if isk else c0
                lt = slice(tok0 - g * TG, tok0 - g * TG + 512)
                cs, sn = E.cos[:, lt], E.sin[:, lt]
                A, B_, C, Dd = E.tmp
                bA, bB, bC, bD = E.bt
                o1, o2 = E.o[(E.i * 2) % 4], E.o[(E.i * 2 + 1) % 4]
                bo1, bo2 = E.bo[(E.i * 2) % 4], E.bo[(E.i * 2 + 1) % 4]
                c1, c2 = E.cho[(E.i * 2) % 4], E.cho[(E.i * 2 + 1) % 4]
                E.i += 1
                stt = lambda out, in0, in1: (lambda e: e.scalar_tensor_tensor(out=out, in0=in0, scalar=scl, in1=in1,
                                                                              op0=ALU.mult, op1=ALU.mult))
                tr.op("dve", stt(A[:, :], ps0[:, :], cs), reads=[bp0, E.btab], writes=[bA])
                tr.op("dve", stt(B_[:, :], ps1[:, :], sn), reads=[bp1, E.btab], writes=[bB])
                tr.op("dve", stt(C[:, :], ps1[:, :], cs), reads=[bp1, E.btab], writes=[bC])
                tr.op("dve", stt(Dd[:, :], ps0[:, :], sn), reads=[bp0, E.btab], writes=[bD])
                tr.op("pool", lambda e: e.tensor_tensor(out=o1[:, :], in0=A[:, :], in1=B_[:, :], op=ALU.subtract),
                      reads=[bA, bB], writes=[bo1])
                tr.op("pool", lambda e: e.tensor_tensor(out=o2[:, :], in0=C[:, :], in1=Dd[:, :], op=ALU.add),
                      reads=[bC, bD], writes=[bo2])
                tr.dma("sp", c1, [(dst[r0:r0 + 128, tok0:tok0 + 512], o1[:, :])], reads=[bo1], writes=[bdst])
                tr.dma("sp", c2, [(dst[r0 + 128:r0 + 256, tok0:tok0 + 512], o2[:, :])], reads=[bo2], writes=[bdst])

            gemm_fm(g, hT, W, [h * 256 for h in range(16)], ep_fm, "ret")

            def ep_tm(mode, env, a):
                if mode == "init":
                    ph, t = env, a
                    E = P()
                    NCG = TG // 128
                    E.cos = t("r_cos", [128, NCG, 128], F32)
                    E.sin = t("r_sin", [128, NCG, 128], F32)
                    E.btab = Buf()
                    ch = tr.dma_chan("ph0")
                    tr.dma("sp", ch, [(E.cos[:, :, :], costm_d[:, g * NCG:(g + 1) * NCG, :]),
                                      (E.sin[:, :, :], sintm_d[:, g * NCG:(g + 1) * NCG, :])],
                           reads=[DB["tab"]], writes=[E.btab])
                    E.tmp = [t(f"r_t{i}", [128, 2, 128], F32) for i in range(4)]
                    E.bt = [Buf() for _ in range(4)]
                    E.o = [t(f"r_o{i}", [128, 512], BF16) for i in range(4)]
                    E.bo = [Buf() for _ in range(4)]
                    E.cho = [tr.dma_chan(f"o{i}") for i in range(4)]
                    E.i = 0
                    return E
                E = env
                pi, c0, ncol, tok0, ps, bp = a
                o = E.o[E.i % 4]
                bo = E.bo[E.i % 4]
                co = E.cho[E.i % 4]
                par = E.i % 2
                E.i += 1
                if c0 < 2 * D:
                    lc = (tok0 - g * TG) // 128
                    scl = RET_DK ** -0.5
                    pv = ps[:, :].rearrange("p (h two f) -> p h two f", h=2, two=2)
                    ov = o[:, :].rearrange("p (h two f) -> p h two f", h=2, two=2)
                    cs = E.cos[:, lc, :].unsqueeze(1).to_broadcast([128, 2, 128])
                    sn = E.sin[:, lc, :].unsqueeze(1).to_broadcast([128, 2, 128])
                    A, B_, C, Dd = E.tmp
                    bA, bB, bC, bD = E.bt
                    stt = lambda out, in0, in1: (lambda e: e.scalar_tensor_tensor(out=out, in0=in0, scalar=scl, in1=in1,
                                                                                  op0=ALU.mult, op1=ALU.mult))
                    tr.op("dve", stt(A[:, :, :], pv[:, :, 0, :], cs), reads=[bp, E.btab], writes=[bA])
                    tr.op("dve", stt(B_[:, :, :], pv[:, :, 1, :], sn), reads=[bp, E.btab], writes=[bB])
                    tr.op("dve", stt(C[:, :, :], pv[:, :, 1, :], cs), reads=[bp, E.btab], writes=[bC])
                    tr.op("dve", stt(Dd[:, :, :], pv[:, :, 0, :], sn), reads=[bp, E.btab], writes=[bD])
                    tr.op("pool", lambda e: e.tensor_tensor(out=ov[:, :, 0, :], in0=A[:, :, :], in1=B_[:, :, :], op=ALU.subtract),
                          reads=[bA, bB], writes=[bo])
                    tr.op("pool", lambda e: e.tensor_tensor(out=ov[:, :, 1, :], in0=C[:, :, :], in1=Dd[:, :, :], op=ALU.add),
                          reads=[bC, bD], writes=[bo])
                    tr.dma("sp", co, [(ktm_d[tok0:tok0 + 128, c0 - D:c0 - D + 512], o[:, :])], reads=[bo], writes=[DB["ktm"]])
                elif c0 < 4 * D:
                    if par == 0:
                        tr.op("act", lambda e: e.activation(out=o[:, :], in_=ps[:, :], func=AF.Copy), reads=[bp], writes=[bo])
                    else:
                        tr.op("dve", lambda e: e.tensor_copy(out=o[:, :], in_=ps[:, :]), reads=[bp], writes=[bo])
                    tr.dma("sp", co, [(vtm_d[tok0:tok0 + 128, c0 - 2 * D:c0 - 2 * D + 512], o[:, :])], reads=[bo], writes=[DB["vtm"]])
                else:
                    tr.op("act", lambda e: e.activation(out=o[:, :], in_=ps[:, :], func=AF.Silu), reads=[bp], writes=[bo])
                    tr.dma("sp", co, [(gtm_d[tok0:tok0 + 128, c0 - 4 * D:c0 - 4 * D + 512], o[:, :])], reads=[bo], writes=[DB["gtm"]])

            gemm_tm(g, hT, W, [(D + i * 512, 512) for i in range(20)], ep_tm)

        def ret_mixer(state):
            with ExitStack() as ph:
                t = lambda name, shape, dtype: ph.enter_context(nc.sbuf_tensor(name, list(shape), dtype))
                decT = t("m_dec", [128, RET_H, 128], F32)
                xir = t("m_xi", [128, RET_H, 128], F32)
                zet = t("m_zeta", [128, RET_H], F32)
                bc = Buf()
                ch = tr.dma_chan("ph0")
                tr.dma("sp", ch, [(decT[:, :, :], cin["decayT"]), (xir[:, :, :], cin["xirep"]), (zet[:, :], cin["zeta"])], writes=[bc])
                S = t("m_S", [128, RET_H, 2, RET_DV], F32)
                Sb = t("m_Sb", [128, RET_H, 2, RET_DV], BF16)
                bS = [Buf() for _ in range(RET_H)]
                bSb = [Buf() for _ in range(RET_H)]
                tr.op("pool", lambda e: e.memset(S[:, :, :, :], 0.0), writes=bS)
                tr.op("pool", lambda e: e.memset(Sb[:, :, :, :], 0.0), writes=bSb)
                NB = 2
                qt2 = [t(f"m_q{i}", [128, RET_H, 2, 256], BF16) for i in range(NB)]
                kt2 = [t(f"m_k{i}", [128, RET_H, 2, 256], BF16) for i in range(NB)]
                bqk = [Buf() for _ in range(NB)]
                chqk = [tr.dma_chan(f"qk{i}") for i in range(NB)]
                km = [t(f"m_km{i}", [128, D], BF16) for i in range(NB)]
                vm = [t(f"m_v{i}", [128, 2 * D], BF16) for i in range(NB)]
                gm = [t(f"m_g{i}", [128, 2 * D], BF16) for i in range(NB)]
                bin_ = [Buf() for _ in range(NB)]
                chin = [tr.dma_chan(f"a{i}") for i in range(NB)]
                qx = [t(f"m_qx{i}", [128, 2, 128], BF16) for i in range(2)]
                bqx = [Buf(), Buf()]
                kz = [t(f"m_kz{i}", [128, 256], BF16) for i in range(2)]
                bkz = [Buf(), Buf()]
                sT = [t(f"m_sT{i}", [128, 128], BF16) for i in range(2)]
                bsT = [Buf(), Buf()]
                yn = [t(f"m_yn{i}", [128, RET_DV], F32) for i in range(2)]
                byn = [Buf(), Buf()]
                z = [t(f"m_z{i}", [128, RET_DV], BF16) for i in range(2)]
                bz = [Buf(), Buf()]
                stats = [t(f"m_st{i}", [128, 6], F32) for i in range(2)]
                mv = [t(f"m_mv{i}", [128, 2], F32) for i in range(2)]
                rs = [t(f"m_rs{i}", [128, 2], F32) for i in range(2)]
                bst = [Buf(), Buf()]
                zT = [t(f"m_zT{i}", [128, 32, 512], BF16) for i in range(1)]
                bzT = Buf()
                chz = tr.dma_chan("o0")
                ps_y = [ph.enter_context(nc.psum_tensor(f"mps_y{i}", [128, 512], F32)) for i in range(2)]
                ps_u = [ph.enter_context(nc.psum_tensor(f"mps_u{i}", [128, 512], F32)) for i in range(2)]
                ps_t = [ph.enter_context(nc.psum_tensor(f"mps_t{i}", [128, 4, 128], BF16)) for i in range(2)]
                ps_s = [ph.enter_context(nc.psum_tensor(f"mps_s{i}", [128, 128], F32)) for i in range(2)]
                bps_s, bps_y, bps_u, bps_t = ([Buf(), Buf()] for _ in range(4))

                def load(c):
                    i = c % NB
                    sl = slice(c * 128, (c + 1) * 128)
                    if c % 2 == 0:
                        i2 = (c // 2) % NB
                        s2 = slice(c * 128, (c + 2) * 128)
                        tr.dma("sp", chqk[i2], [
                            (qt2[i2][:, :, :, :], qT_d[:, s2].rearrange("(h two p) t -> p h two t", two=2, p=128)),
                            (kt2[i2][:, :, :, :], kT_d[:, s2].rearrange("(h two p) t -> p h two t", two=2, p=128))],
                            reads=[DB["qT"], DB["kT"]], writes=[bqk[i2]])
                    tr.dma("sp", chin[i], [
                        (km[i][:, :], ktm_d[sl, :]), (vm[i][:, :], vtm_d[sl, :]), (gm[i][:, :], gtm_d[sl, :])],
                        reads=[DB["ktm"], DB["vtm"], DB["gtm"]], writes=[bin_[i]])

                load(0)
                it = 0
                for c in range(NCH):
                    i = c % NB
                    if c + 1 < NCH:
                        load(c + 1)
                    c4 = c % 4
                    i2 = (c // 2) % NB
                    co = (c % 2) * 128
                    for h in range(RET_H):
                        j = it % 2
                        it += 1
                        for dcc in range(2):
                            tr.op("pe", lambda e: e.matmul(ps_s[j][:, :], kt2[i2][:, h, dcc, co:co + 128], qt2[i2][:, h, dcc, co:co + 128],
                                                           start=(dcc == 0), stop=(dcc == 1)),
                                  reads=[bqk[i2]], writes=[bps_s[j]], signal=(dcc == 1))
                        tr.op("dve", lambda e: e.tensor_tensor(out=sT[j][:, :], in0=ps_s[j][:, :], in1=decT[:, h, :], op=ALU.mult),
                              reads=[bps_s[j], bc], writes=[bsT[j]])
                        tr.op("pool", lambda e: e.tensor_tensor(out=qx[j][:, :, :], in0=qt2[i2][:, h, :, co:co + 128],
                                                                in1=xir[:, h, :].unsqueeze(1).to_broadcast([128, 2, 128]),
                                                                op=ALU.mult), reads=[bqk[i2], bc], writes=[bqx[j]])
                        tr.op("pe", lambda e: e.matmul(ps_y[j][:, :], sT[j][:, :], vm[i][:, h * 512:(h + 1) * 512],
                                                       start=True, stop=False),
                              reads=[bsT[j], bin_[i]], writes=[bps_y[j]], signal=False)
                        for dcc in range(2):
                            tr.op("pe", lambda e: e.matmul(ps_y[j][:, :], qx[j][:, dcc, :], Sb[:, h, dcc, :],
                                                           start=False, stop=(dcc == 1)),
                                  reads=[bqx[j], bSb[h]], writes=[bps_y[j]], signal=(dcc == 1))
                        tr.op("pool", lambda e: e.tensor_tensor(out=kz[j][:, :], in0=km[i][:, h * 256:(h + 1) * 256],
                                                                in1=zet[:, h:h + 1].to_broadcast([128, 256]), op=ALU.mult),
                              reads=[bin_[i], bc], writes=[bkz[j]])
                        for dcc in range(2):
                            tr.op("pe", lambda e: e.matmul(ps_u[dcc][:, :], kz[j][:, dcc * 128:(dcc + 1) * 128],
                                                           vm[i][:, h * 512:(h + 1) * 512], start=True, stop=True),
                                  reads=[bkz[j], bin_[i]], writes=[bps_u[dcc]])
                        for dcc in range(2):
                            tr.op("dve", lambda e: e.scalar_tensor_tensor(out=S[:, h, dcc, :], in0=S[:, h, dcc, :],
                                                                          scalar=HC["gchunk"][h], in1=ps_u[dcc][:, :],
                                                                          op0=ALU.mult, op1=ALU.add),
                                  reads=[bps_u[dcc], bS[h]], writes=[bS[h]])
                        tr.op("act", lambda e: e.activation(out=Sb[:, h, :, :], in_=S[:, h, :, :], func=AF.Copy),
                              reads=[bS[h]], writes=[bSb[h]])
                        tr.op("dve", lambda e: e.bn_stats(out=stats[j][:, :], in_=ps_y[j][:, :]), reads=[bps_y[j]], writes=[bst[j]])
                        tr.op("dve", lambda e: e.bn_aggr(out=mv[j][:, :], in_=stats[j][:, :]), reads=[bst[j]], writes=[bst[j]])
                        tr.op("act", lambda e: e.activation(out=rs[j][:, 0:1], in_=mv[j][:, 1:2], func=AF.Sqrt,
                                                            bias=K.eps[:, 0:1]), reads=[bst[j], b_const], writes=[bst[j]])
                        tr.op("dve", lambda e: e.reciprocal(out=rs[j][:, 0:1], in_=rs[j][:, 0:1]), reads=[bst[j]], writes=[bst[j]])
                        tr.op("dve", lambda e: e.scalar_tensor_tensor(out=rs[j][:, 1:2], in0=mv[j][:, 0:1], scalar=-1.0,
                                                                      in1=rs[j][:, 0:1], op0=ALU.mult, op1=ALU.mult),
                              reads=[bst[j]], writes=[bst[j]])
                        tr.op("act", lambda e: e.activation(out=yn[j][:, :], in_=ps_y[j][:, :], func=AF.Identity,
                                                            bias=rs[j][:, 1:2], scale=rs[j][:, 0:1]),
                              reads=[bps_y[j], bst[j]], writes=[byn[j]])
                        tr.op("pool", lambda e: e.tensor_tensor(out=z[j][:, :], in0=yn[j][:, :], in1=gm[i][:, h * 512:(h + 1) * 512],
                                                                op=ALU.mult), reads=[byn[j], bin_[i]], writes=[bz[j]])
                        for q4 in range(4):
                            tr.op("pe", lambda e: e.transpose(out=ps_t[j][:, q4, :], in_=z[j][:, q4 * 128:(q4 + 1) * 128],
                                                              identity=K.ident_b[:, :]),
                                  reads=[bz[j], b_const], writes=[bps_t[j]], signal=(q4 == 3))
                        tr.op("act", lambda e: e.activation(out=zT[0][:, h * 4:(h + 1) * 4, c4 * 128:(c4 + 1) * 128],
                                                            in_=ps_t[j][:, :, :], func=AF.Copy),
                              reads=[bps_t[j]], writes=[bzT])
                    if c4 == 3 or c == NCH - 1:
                        nt = (c4 + 1) * 128
                        t0 = (c - c4) * 128
                        tr.dma("sp", chz, [(zT_d[:, t0:t0 + nt].rearrange("(b p) t -> p b t", p=128), zT[0][:, :, 0:nt])],
                               reads=[bzT], writes=[DB["zT"]])
                tr.barrier()

        def ml_inproj(g, hT, W):
            def ep_fm(mode, env, a):
                if mode == "init":
                    ph, t = env, a
                    E = P()
                    E.o = [t(f"l_o{i}", [128, 512], BF16) for i in range(4)]
                    E.bo = [Buf() for _ in range(4)]
                    E.cho = [tr.dma_chan(f"o{i}") for i in range(4)]
                    E.i = 0
                    return E
                E = env
                pi, c0, tok0, ps0, bp0, ps1, bp1 = a
                isk = c0 >= 1024
                scl = (ML_DQK ** -0.5) if isk else 1.0
                dst = kT_d if isk else qT_d
                bdst = DB["kT"] if isk else DB["qT"]
                r0 = c0 - 1024 if isk else c0
                for fb, (ps, bp) in enumerate(((ps0, bp0), (ps1, bp1))):
                    k = E.i % 4
                    E.i += 1
                    if fb == 0:
                        tr.op("act", lambda e: e.activation(out=E.o[k][:, :], in_=ps[:, :], func=AF.Copy, scale=scl),
                              reads=[bp], writes=[E.bo[k]])
                    else:
                        tr.op("dve", lambda e: e.tensor_scalar(out=E.o[k][:, :], in0=ps[:, :], scalar1=scl, scalar2=None,
                                                               op0=ALU.mult), reads=[bp], writes=[E.bo[k]])
                    tr.dma("sp", E.cho[k], [(dst[r0 + fb * 128:r0 + fb * 128 + 128, tok0:tok0 + 512], E.o[k][:, :])],
                           reads=[E.bo[k]], writes=[bdst])

            gemm_fm(g, hT, W, [i * 256 for i in range(8)], ep_fm, "ml")

            def ep_tm(mode, env, a):
                if mode == "init":
                    ph, t = env, a
                    E = P()
                    E.o = [t(f"l_o{i}", [128, 512], BF16) for i in range(4)]
                    E.bo = [Buf() for _ in range(4)]
                    E.cho = [tr.dma_chan(f"o{i}") for i in range(4)]
                    E.og = [t(f"l_og{i}", [128, 16], F32) for i in range(2)]
                    E.bog = [Buf() for _ in range(2)]
                    E.chg = [tr.dma_chan(f"a{i}") for i in range(2)]
                    E.i = 0
                    return E
                E = env
                pi, c0, ncol, tok0, ps, bp = a
                k = E.i % 4
                E.i += 1
                o, bo, co = E.o[k], E.bo[k], E.cho[k]
                if c0 < 2048:
                    scl = ML_DQK ** -0.5
                    tr.op("act", lambda e: e.activation(out=o[:, :], in_=ps[:, :], func=AF.Copy, scale=scl), reads=[bp], writes=[bo])
                    tr.dma("sp", co, [(ktm_d[tok0:tok0 + 128, c0 - 1024:c0 - 1024 + 512], o[:, :])], reads=[bo], writes=[DB["ktm"]])
                elif c0 < 4096:
                    tr.op("dve", lambda e: e.tensor_copy(out=o[:, :], in_=ps[:, :]), reads=[bp], writes=[bo])
                    tr.dma("sp", co, [(vtm_d[tok0:tok0 + 128, c0 - 2048:c0 - 2048 + 512], o[:, :])], reads=[bo], writes=[DB["vtm"]])
                elif c0 < 6144:
                    tr.op("act", lambda e: e.activation(out=o[:, :], in_=ps[:, :], func=AF.Sigmoid), reads=[bp], writes=[bo])
                    tr.dma("sp", co, [(gtm_d[tok0:tok0 + 128, c0 - 4096:c0 - 4096 + 512], o[:, :])], reads=[bo], writes=[DB["gtm"]])
                else:
                    kk = E.i % 2
                    tr.op("dve", lambda e: e.tensor_copy(out=E.og[kk][:, :], in_=ps[:, 0:16]), reads=[bp], writes=[E.bog[kk]])
                    tr.dma("sp", E.chg[kk], [(gate_d[tok0:tok0 + 128, :], E.og[kk][:, :])], reads=[E.bog[kk]], writes=[DB["gate"]])

            gemm_tm(g, hT, W, [(1024 + i * 512, 512) for i in range(10)] + [(6144, 16)], ep_tm)

        def ml_mixer(jl):
            with ExitStack() as ph:
                t = lambda name, shape, dtype: ph.enter_context(nc.sbuf_tensor(name, list(shape), dtype))
                tri = t("x_tri", [128, 128], F32)
                negm = t("x_negm", [128, 128], F32)
                bgt = t("x_bg", [128, 16], F32)
                ngt = t("x_ng", [128, D], F32)
                onesb = t("x_1b", [128, 1], BF16)
                bc = Buf()
                ch = tr.dma_chan("ph0")
                tr.dma("sp", ch, [(tri[:, :], cin["tri_f"]), (negm[:, :], cin["negm_f"]), (bgt[:, :], ml_bg[:, jl, :]),
                                  (ngt[:, :], ml_ng[:, jl, :])], writes=[bc])
                tr.op("dve", lambda e: e.memset(onesb[:, :], 1.0), writes=[bc])
                NCH8 = NCH * 8
                gts = t("x_gts", [128, NCH, 16], F32)
                ilog = t("x_il", [128, NCH, 8], F32)
                flog = t("x_fl", [128, NCH, 8], F32)
                cS = t("x_cS", [128, NCH, 8], F32)
                wS = t("x_wS", [128, NCH, 8], F32)
                eBt = t("x_eBt", [128, NCH, 8], F32)
                bg_ = Buf()
                tr.dma("sp", tr.dma_chan("ph1"), [(gts[:, :, :], gate_d.rearrange("(c p) k -> p c k", p=128))], reads=[DB["gate"]], writes=[bg_])
                tr.op("dve", lambda e: e.tensor_tensor(out=gts[:, :, :], in0=gts[:, :, :],
                                                       in1=bgt[:, :].unsqueeze(1).to_broadcast([128, NCH, 16]), op=ALU.add),
                      reads=[bg_, bc], writes=[bg_])
                tr.op("act", lambda e: e.activation(out=gts[:, :, :], in_=gts[:, :, :], func=AF.Tanh, scale=1.0 / SOFTCAP),
                      reads=[bg_], writes=[bg_])
                bil, bfl = Buf(), Buf()
                tr.op("dve", lambda e: e.tensor_scalar(out=ilog[:, :, :], in0=gts[:, :, 0:8], scalar1=SOFTCAP, scalar2=None,
                                                       op0=ALU.mult), reads=[bg_], writes=[bil])
                tr.op("act", lambda e: e.activation(out=flog[:, :, :], in_=gts[:, :, 8:16], func=AF.Exp, scale=-SOFTCAP),
                      reads=[bg_], writes=[bfl])
                tr.op("act", lambda e: e.activation(out=flog[:, :, :], in_=flog[:, :, :], func=AF.Ln, bias=K.one[:, 0:1]),
                      reads=[bfl, b_const], writes=[bfl])
                tr.op("dve", lambda e: e.tensor_scalar(out=flog[:, :, :], in0=flog[:, :, :], scalar1=-1.0, scalar2=None,
                                                       op0=ALU.mult), reads=[bfl], writes=[bfl])
                ps = [ph.enter_context(nc.psum_tensor(f"xps{i}", [128, 512], F32)) for i in range(8)]
                bps = [Buf() for _ in range(8)]
                pB, pDl, pSc, pN0, pN1, pU0, pU1, pT = ps
                bB, bDl, bSc, bN0, bN1, bU0, bU1, bT = bps
                pTb = pT[:, :].bitcast(BF16)
                fl2 = flog[:, :, :].rearrange("p c h -> p (c h)")
                tr.op("pe", lambda e: e.matmul(pB[:, 0:NCH8], tri[:, :], fl2, start=True, stop=True), reads=[bc, bfl], writes=[bB])
                tr.op("pe", lambda e: e.matmul(pDl[:, 0:NCH8], K.ones_f[:, :], fl2, start=True, stop=True),
                      reads=[b_const, bfl], writes=[bDl])
                bcS, bwS, beBt = Buf(), Buf(), Buf()
                tr.op("dve", lambda e: e.tensor_tensor(out=cS[:, :, :].rearrange("p c h -> p (c h)"),
                                                       in0=ilog[:, :, :].rearrange("p c h -> p (c h)"), in1=pB[:, 0:NCH8],
                                                       op=ALU.subtract), reads=[bil, bB], writes=[bcS])
                tr.op("dve", lambda e: e.tensor_tensor(out=wS[:, :, :].rearrange("p c h -> p (c h)"),
                                                       in0=cS[:, :, :].rearrange("p c h -> p (c h)"), in1=pDl[:, 0:NCH8],
                                                       op=ALU.add), reads=[bcS, bDl], writes=[bwS])
                tr.op("act", lambda e: e.activation(out=wS[:, :, :], in_=wS[:, :, :], func=AF.Exp), reads=[bwS], writes=[bwS])
                tr.op("act", lambda e: e.activation(out=eBt[:, :, :].rearrange("p c h -> p (c h)"), in_=pDl[:, 0:NCH8], func=AF.Exp),
                      reads=[bDl], writes=[beBt])
                C = t("x_C", [128, ML_H, ML_DV], F32)
                Cb = t("x_Cb", [128, ML_H, ML_DV], BF16)
                nv = t("x_n", [128, ML_H], F32)
                nb = t("x_nb", [128, ML_H], BF16)
                bC = [Buf(), Buf()]
                bCb = [Buf(), Buf()]
                tr.op("pool", lambda e: e.memset(C[:, :, :], 0.0), writes=bC)
                tr.op("pool", lambda e: e.memset(Cb[:, :, :], 0.0), writes=bCb)
                tr.op("pool", lambda e: e.memset(nv[:, :], 0.0), writes=bC)
                tr.op("pool", lambda e: e.memset(nb[:, :], 0.0), writes=bCb)
                NB = 2
                qt2 = [t(f"x_q{i}", [128, ML_H, 256], BF16) for i in range(NB)]
                kt2 = [t(f"x_k{i}", [128, ML_H, 256], BF16) for i in range(NB)]
                bqk = [Buf() for _ in range(NB)]
                chqk = [tr.dma_chan(f"qk{i}") for i in range(NB)]
                km = [t(f"x_km{i}", [128, 1024], BF16) for i in range(NB)]
                vm = [t(f"x_vm{i}", [128, D], BF16) for i in range(NB)]
                om = [t(f"x_om{i}", [128, D], BF16) for i in range(NB)]
                bin_ = [Buf() for _ in range(NB)]
                chin = [tr.dma_chan(f"a{i}") for i in range(NB)]
                Ftri = [t(f"x_F{i}", [128, 4, 128], F32) for i in range(2)]
                R2 = [t(f"x_R{i}", [128, 4, 128], F32) for i in range(2)]
                eB = [t(f"x_eB{i}", [128, 4, 128], F32) for i in range(2)]
                dw = [t(f"x_dw{i}", [128, 4, 128], F32) for i in range(2)]
                PT = [t(f"x_PT{i}", [128, 4, 128], BF16) for i in range(2)]
                qd = [t(f"x_qd{i}", [128, 4, 128], BF16) for i in range(2)]
                kw = [t(f"x_kw{i}", [128, 4, 128], BF16) for i in range(2)]
                gso = [t(f"x_gso{i}", [128, 1024], F32) for i in range(2)]
                zz = [t(f"x_z{i}", [128, 1024], BF16) for i in range(2)]
                junk = t("x_junk", [128, 256], F32)
                sm = [t(f"x_sm{i}", [128, 16], F32) for i in range(2)]
                bF, bR, beB, bdw, bPT, bqd, bkw, bgso, bzz, bsm = ([Buf(), Buf()] for _ in range(10))
                bjunk = Buf()
                zT = t("x_zT", [128, 16, 512], BF16)
                bzT = Buf()
                chz = tr.dma_chan("o0")

                def load(c):
                    i = c % NB
                    sl = slice(c * 128, (c + 1) * 128)
                    if c % 2 == 0:
                        i2 = (c // 2) % NB
                        s2 = slice(c * 128, (c + 2) * 128)
                        tr.dma("sp", chqk[i2], [
                            (qt2[i2][:, :, :], qT_d[0:1024, s2].rearrange("(h p) t -> p h t", p=128)),
                            (kt2[i2][:, :, :], kT_d[0:1024, s2].rearrange("(h p) t -> p h t", p=128))],
                            reads=[DB["qT"], DB["kT"]], writes=[bqk[i2]])
                    tr.dma("sp", chin[i], [(km[i][:, :], ktm_d[sl, 0:1024]), (vm[i][:, :], vtm_d[sl, 0:D]), (om[i][:, :], gtm_d[sl, 0:D])],
                           reads=[DB["ktm"], DB["vtm"], DB["gtm"]], writes=[bin_[i]])

                load(0)
                it = 0
                for c in range(NCH):
                    i = c % NB
                    if c + 1 < NCH:
                        load(c + 1)
                    c4 = c % 4
                    i2 = (c // 2) % NB
                    co = (c % 2) * 128
                    for hh in range(2):
                        j = it % 2
                        it += 1
                        h0 = hh * 4
                        tr.op("dve", lambda e: e.tensor_tensor(out=Ftri[j][:, :, :], in0=tri[:, :].unsqueeze(1).to_broadcast([128, 4, 128]),
                                                               in1=flog[:, c, h0:h0 + 4].unsqueeze(2).to_broadcast([128, 4, 128]),
                                                               op=ALU.mult), reads=[bc, bfl], writes=[bF[j]])
                        tr.op("pool", lambda e: e.tensor_tensor(out=R2[j][:, :, :], in0=negm[:, :].unsqueeze(1).to_broadcast([128, 4, 128]),
                                                                in1=cS[:, c, h0:h0 + 4].unsqueeze(2).to_broadcast([128, 4, 128]),
                                                                op=ALU.add), reads=[bc, bcS], writes=[bR[j]])
                        F2 = Ftri[j][:, :, :].rearrange("p h n -> p (h n)")
                        R22 = R2[j][:, :, :].rearrange("p h n -> p (h n)")
                        tr.op("pe", lambda e: e.matmul(pB[:, :], K.ones_f[:, :], F2, start=True, stop=True),
                              reads=[b_const, bF[j]], writes=[bB])
                        tr.op("pe", lambda e: e.matmul(pDl[:, :], K.ones_f[:, :], F2, start=True, stop=False),
                              reads=[b_const, bF[j]], writes=[bDl], signal=False)
                        tr.op("pe", lambda e: e.matmul(pDl[:, :], K.ident_f[:, :], R22, start=False, stop=True),
                              reads=[b_const, bR[j]], writes=[bDl])
                        tr.op("act", lambda e: e.activation(out=eB[j][:, :, :].rearrange("p h n -> p (h n)"), in_=pB[:, :], func=AF.Exp),
                              reads=[bB], writes=[beB[j]])
                        tr.op("act", lambda e: e.activation(out=dw[j][:, :, :].rearrange("p h n -> p (h n)"), in_=pDl[:, :], func=AF.Exp),
                              reads=[bDl], writes=[bdw[j]])
                        for h in range(4):
                            tr.op("pe", lambda e: e.matmul(pSc[:, h * 128:(h + 1) * 128], kt2[i2][:, h0 + h, co:co + 128],
                                                           qt2[i2][:, h0 + h, co:co + 128], start=True, stop=True),
                                  reads=[bqk[i2]], writes=[bSc], signal=(h == 3))
                        tr.op("dve", lambda e: e.tensor_tensor(out=PT[j][:, :, :].rearrange("p h n -> p (h n)"), in0=pSc[:, :],
                                                               in1=dw[j][:, :, :].rearrange("p h n -> p (h n)"), op=ALU.mult),
                              reads=[bSc, bdw[j]], writes=[bPT[j]])
                        tr.op("pool", lambda e: e.tensor_tensor(out=qd[j][:, :, :], in0=qt2[i2][:, h0:h0 + 4, co:co + 128],
                                                                in1=eB[j][:, :, :], op=ALU.mult), reads=[bqk[i2], beB[j]], writes=[bqd[j]])
                        for h in range(4):
                            pn, bn_ = (pN0, bN0) if h < 2 else (pN1, bN1)
                            cs = (h % 2) * 256
                            tr.op("pe", lambda e: e.matmul(pn[:, cs:cs + 256], PT[j][:, h, :], vm[i][:, (h0 + h) * 256:(h0 + h + 1) * 256],
                                                           start=True, stop=False), reads=[bPT[j], bin_[i]], writes=[bn_], signal=False)
                            tr.op("pe", lambda e: e.matmul(pn[:, cs:cs + 256], qd[j][:, h, :], Cb[:, h0 + h, :], start=False, stop=True),
                                  reads=[bqd[j], bCb[hh]], writes=[bn_], signal=(h % 2 == 1))
                        for h in range(4):
                            tr.op("pe", lambda e: e.matmul(pDl[:, h:h + 1], PT[j][:, h, :], onesb[:, 0:1], start=True, stop=False),
                                  reads=[bPT[j], bc], writes=[bDl], signal=False)
                            tr.op("pe", lambda e: e.matmul(pDl[:, h:h + 1], qd[j][:, h, :], nb[:, h0 + h:h0 + h + 1], start=False, stop=True),
                                  reads=[bqd[j], bCb[hh]], writes=[bDl], signal=(h == 3))
                        s_ = sm[j]
                        tr.op("act", lambda e: e.activation(out=s_[:, 0:4], in_=pDl[:, 0:4], func=AF.Abs), reads=[bDl], writes=[bsm[j]])
                        tr.op("dve", lambda e: e.tensor_scalar(out=s_[:, 0:4], in0=s_[:, 0:4], scalar1=1.0, scalar2=None,
                                                               op0=ALU.max), reads=[bsm[j]], writes=[bsm[j]])
                        tr.op("dve", lambda e: e.reciprocal(out=s_[:, 4:8], in_=s_[:, 0:4]), reads=[bsm[j]], writes=[bsm[j]])
                        for h in range(4):
                            pn, bn_ = (pN0, bN0) if h < 2 else (pN1, bN1)
                            cs = (h % 2) * 256
                            tr.op("act", lambda e: e.activation(out=junk[:, :], in_=pn[:, cs:cs + 256], func=AF.Square,
                                                                scale=s_[:, 4 + h:5 + h], accum_out=s_[:, 8 + h:9 + h]),
                                  reads=[bn_, bsm[j]], writes=[bjunk, bsm[j]])
                        tr.op("act", lambda e: e.activation(out=s_[:, 8:12], in_=s_[:, 8:12], func=AF.Sqrt, scale=1.0 / ML_DV,
                                                            bias=K.eps[:, 0:1]), reads=[bsm[j], b_const], writes=[bsm[j]])
                        tr.op("dve", lambda e: e.reciprocal(out=s_[:, 8:12], in_=s_[:, 8:12]), reads=[bsm[j]], writes=[bsm[j]])
                        tr.op("dve", lambda e: e.tensor_tensor(out=s_[:, 12:16], in0=s_[:, 8:12], in1=s_[:, 4:8], op=ALU.mult),
                              reads=[bsm[j]], writes=[bsm[j]])
                        tr.op("pool", lambda e: e.tensor_tensor(out=gso[j][:, :], in0=om[i][:, h0 * 256:h0 * 256 + 1024],
                                                                in1=ngt[:, h0 * 256:h0 * 256 + 1024], op=ALU.mult),
                              reads=[bin_[i], bc], writes=[bgso[j]])
                        for h in range(4):
                            pn, bn_ = (pN0, bN0) if h < 2 else (pN1, bN1)
                            cs = (h % 2) * 256
                            tr.op("dve", lambda e: e.scalar_tensor_tensor(out=zz[j][:, h * 256:(h + 1) * 256], in0=pn[:, cs:cs + 256],
                                                                          scalar=s_[:, 12 + h:13 + h], in1=gso[j][:, h * 256:(h + 1) * 256],
                                                                          op0=ALU.mult, op1=ALU.mult),
                                  reads=[bn_, bsm[j], bgso[j]], writes=[bzz[j]])
                        for k8 in range(8):
                            tr.op("pe", lambda e: e.transpose(out=pTb[:, k8 * 128:(k8 + 1) * 128], in_=zz[j][:, k8 * 128:(k8 + 1) * 128],
                                                              identity=K.ident_b[:, :]), reads=[bzz[j], b_const], writes=[bT], signal=(k8 == 7))
                        tr.op("act", lambda e: e.activation(out=zT[:, h0 * 2:h0 * 2 + 8, c4 * 128:(c4 + 1) * 128],
                                                            in_=pTb.rearrange("p (k n) -> p k n", k=8), func=AF.Copy),
                              reads=[bT], writes=[bzT])
                        tr.op("pool", lambda e: e.tensor_tensor(out=kw[j][:, :, :],
                                                                in0=km[i][:, h0 * 128:(h0 + 4) * 128].rearrange("p (h d) -> p h d", h=4),
                                                                in1=wS[:, c, h0:h0 + 4].unsqueeze(2).to_broadcast([128, 4, 128]),
                                                                op=ALU.mult), reads=[bin_[i], bwS], writes=[bkw[j]])
                        for h in range(4):
                            pu, bu = (pU0, bU0) if h < 2 else (pU1, bU1)
                            cs = (h % 2) * 256
                            tr.op("pe", lambda e: e.matmul(pu[:, cs:cs + 256], kw[j][:, h, :], vm[i][:, (h0 + h) * 256:(h0 + h + 1) * 256],
                                                           start=True, stop=True), reads=[bkw[j], bin_[i]], writes=[bu], signal=(h % 2 == 1))
                        for h in range(4):
                            tr.op("pe", lambda e: e.matmul(pSc[:, h:h + 1], kw[j][:, h, :], onesb[:, 0:1], start=True, stop=True),
                                  reads=[bkw[j], bc], writes=[bSc], signal=(h == 3))
                        for h in range(4):
                            pu, bu = (pU0, bU0) if h < 2 else (pU1, bU1)
                            cs = (h % 2) * 256
                            tr.op("dve", lambda e: e.scalar_tensor_tensor(out=C[:, h0 + h, :], in0=C[:, h0 + h, :],
                                                                          scalar=eBt[:, c, h0 + h:h0 + h + 1], in1=pu[:, cs:cs + 256],
                                                                          op0=ALU.mult, op1=ALU.add),
                                  reads=[bu, beBt, bC[hh]], writes=[bC[hh]])
                        tr.op("dve", lambda e: e.tensor_tensor(out=nv[:, h0:h0 + 4], in0=nv[:, h0:h0 + 4], in1=eBt[:, c, h0:h0 + 4],
                                                               op=ALU.mult), reads=[beBt, bC[hh]], writes=[bC[hh]])
                        tr.op("dve", lambda e: e.tensor_tensor(out=nv[:, h0:h0 + 4], in0=nv[:, h0:h0 + 4], in1=pSc[:, 0:4], op=ALU.add),
                              reads=[bSc, bC[hh]], writes=[bC[hh]])
                        tr.op("act", lambda e: e.activation(out=Cb[:, h0:h0 + 4, :], in_=C[:, h0:h0 + 4, :], func=AF.Copy),
                              reads=[bC[hh]], writes=[bCb[hh]])
                        tr.op("act", lambda e: e.activation(out=nb[:, h0:h0 + 4], in_=nv[:, h0:h0 + 4], func=AF.Copy),
                              reads=[bC[hh]], writes=[bCb[hh]])
                    if c4 == 3 or c == NCH - 1:
                        nt = (c4 + 1) * 128
                        t0 = (c - c4) * 128
                        tr.dma("sp", chz, [(zT_d[0:D, t0:t0 + nt].rearrange("(b p) t -> p b t", p=128), zT[:, :, 0:nt])],
                               reads=[bzT], writes=[DB["zT"]])
                tr.barrier()

        def mlp1(g, hT, W):
            def ep(mode, env, a):
                if mode == "init":
                    ph, t = env, a
                    E = P()
                    E.tmp = [t(f"u_t{i}", [128, 512], F32) for i in range(4)]
                    E.bt = [Buf() for _ in range(4)]
                    E.o = [t(f"u_o{i}", [128, 512], BF16) for i in range(4)]
                    E.bo = [Buf() for _ in range(4)]
                    E.cho = [tr.dma_chan(f"o{i}") for i in range(4)]
                    E.i = 0
                    return E
                E = env
                pi, c0, tok0, ps0, bp0, ps1, bp1 = a
                for fb, (ps, bp) in enumerate(((ps0, bp0), (ps1, bp1))):
                    k = E.i % 4
                    E.i += 1
                    tr.op("act", lambda e: e.activation(out=E.tmp[k][:, :], in_=ps[:, :], func=AF.Relu), reads=[bp], writes=[E.bt[k]])
                    tr.op("pool", lambda e: e.tensor_tensor(out=E.o[k][:, :], in0=E.tmp[k][:, :], in1=E.tmp[k][:, :], op=ALU.mult),
                          reads=[E.bt[k]], writes=[E.bo[k]])
                    r0 = c0 + fb * 128
                    tr.dma("sp", E.cho[k], [(uT_d[r0:r0 + 128, tok0:tok0 + 512], E.o[k][:, :])], reads=[E.bo[k]], writes=[DB["uT"]])
            gemm_fm(g, hT, W, [i * 256 for i in range(DFF // 256)], ep, "mlp")

        phase_tables()
        class HT:
            def __enter__(self):
                self.cm = nc.sbuf_tensor("hT", [128, DC, TG], BF16)
                self.t = self.cm.__enter__()
                self.buf = Buf("hT")
                return self

            def __exit__(self, *a):
                return self.cm.__exit__(*a)

        if True:
            src_x = xT_in
            for l in range(n_layers):
                last = (l == n_layers - 1)
                if l % 2 == 0:
                    j = l // 2
                    for g in range(NG):
                        with HT() as hT:
                            phase_norm(g, hT, src_x, None, None, l * 4 + 0, xT_d if l == 0 else None)
                            ret_inproj(g, hT, ret_w_in[j])
                    ret_mixer(None)
                    KCo = 32
                    Wo = ret_w_out[j]
                else:
                    j = l // 2
                    for g in range(NG):
                        with HT() as hT:
                            phase_norm(g, hT, src_x, None, None, l * 4 + 0, xT_d if l == 0 else None)
                            ml_inproj(g, hT, ml_w_in[j])
                    ml_mixer(j)
                    KCo = 16
                    Wo = ml_w_out[j]
                src_x = xT_d
                for g in range(NG):
                    gemm_stream(g, zT_d, DB["zT"], KCo, Wo, yT_d, DB["yT"])
                    with HT() as hT:
                        phase_norm(g, hT, xT_d, yT_d, l * 4 + 1, l * 4 + 2, xT_d)
                        mlp1(g, hT, w1_in[l])
                    gemm_stream(g, uT_d, DB["uT"], DFF // 128, w2_in[l], yT_d, DB["yT"])
                    if last:
                        phase_norm(g, None, xT_d, yT_d, l * 4 + 3, None, outT)
                    else:
                        phase_norm(g, None, xT_d, yT_d, l * 4 + 3, None, xT_d)
        tr.barrier()
        print("ninst", tr.ninst, "nwait", tr.nwait, flush=True)
    return nc_real


def make_in_maps(inputs, T, cores):
    HC = host_consts()
    x = np.asarray(inputs["x"], dtype=np.float32)
    pos = np.asarray(inputs["positions"]).astype(np.int32)
    ng = np.asarray(inputs["norm_g"], dtype=np.float32)
    L = ng.shape[0]
    gcol = np.zeros((128, 16, DC), np.float32)
    gcol[:, :L * 4, :] = ng.reshape(L * 4, DC, 128).transpose(2, 0, 1)
    shared = {
        "gcol": gcol,
        "ret_w_in": np.ascontiguousarray(inputs["ret_w_in"], dtype=np.float32),
        "ret_w_out": np.ascontiguousarray(inputs["ret_w_out"], dtype=np.float32),
        "ml_w_in": np.ascontiguousarray(inputs["mlstm_w_in"], dtype=np.float32),
        "ml_bg": np.ascontiguousarray(np.broadcast_to(np.asarray(inputs["mlstm_b_gate"], np.float32)[None], (128, 2, 16))),
        "ml_ng": np.ascontiguousarray(np.broadcast_to(np.asarray(inputs["mlstm_norm_g"], np.float32)[None], (128, 2, D))),
        "ml_w_out": np.ascontiguousarray(inputs["mlstm_w_out"], dtype=np.float32),
        "mlp_w1": np.ascontiguousarray(inputs["mlp_w1"], dtype=np.float32),
        "mlp_w2": np.ascontiguousarray(inputs["mlp_w2"], dtype=np.float32),
    }
    for n in CONST_NAMES:
        shared["c_" + n] = HC[n]
    maps = []
    for (b, t0) in cores:
        m = dict(shared)
        m["xT"] = np.ascontiguousarray(x[b, t0:t0 + T, :].T)
        m["pos"] = np.ascontiguousarray(pos[b, t0:t0 + T].reshape(1, T))
        m["pos_tm"] = np.ascontiguousarray(pos[b, t0:t0 + T].reshape(T // 128, 128).T)
        maps.append(m)
    return maps


def kernel(**inputs):
    x = np.asarray(inputs["x"])
    B, S, _ = x.shape
    T = S
    cores = [(b, 0) for b in range(B)]
    nc = build(T)
    in_maps = make_in_maps(inputs, T, cores)
    res = run_bass_kernel_spmd(nc, in_maps, core_ids=list(range(len(cores))))
    out = np.zeros((B, S, D), np.float32)
    for i, (b, t0) in enumerate(cores):
        out[b, t0:t0 + T, :] = np.asarray(res.results[i]["outT"]).T
    return out
```

```python
import math
from contextlib import ExitStack
import numpy as np
import ml_dtypes
import concourse.bass as bass
import concourse.mybir as mybir
from concourse.bass_utils import run_bass_kernel_spmd

F32 = mybir.dt.float32
BF16 = mybir.dt.bfloat16
I32 = mybir.dt.int32
AF = mybir.ActivationFunctionType
ALU = mybir.AluOpType
AX = mybir.AxisListType

D = 2048
DC = 16
DFF = 8192
EPS = 1e-6
RET_H = 8
RET_DK = 256
RET_DV = 512
RET_IN = 12288
ML_H = 8
ML_DQK = 128
ML_DV = 256
ML_IN = 6160
SOFTCAP = 15.0
PI = math.pi


class Buf:
    __slots__ = ("name", "w", "r")

    def __init__(self, name=""):
        self.name = name
        self.w = None
        self.r = {}


class Chan:
    def __init__(self, name, sem, step):
        self.name = name
        self.sem = sem
        self.step = step
        self.cnt = 0


class Tracker:
    def __init__(self, nc, stack):
        self.nc = nc
        self.stack = stack
        self.engs = {"pe": nc.tensor, "act": nc.scalar, "dve": nc.vector,
                     "pool": nc.gpsimd, "sp": nc.sync}
        self.chan = {}
        for n in self.engs:
            sem = stack.enter_context(nc.semaphore("s_" + n))
            self.chan[n] = Chan(n, sem, 1)
        self.waited = {n: {} for n in self.engs}
        self.ninst = {n: 0 for n in self.engs}
        self.nwait = 0
        self.dma_pool = {}

    def dma_chan(self, name):
        if name in self.dma_pool:
            return self.dma_pool[name]
        sem = self.stack.enter_context(self.nc.semaphore("d_" + name))
        c = Chan("d_" + name, sem, 16)
        self.chan[c.name] = c
        self.dma_pool[name] = c
        return c

    def _deps(self, eng, reads, writes):
        deps = {}

        def add(c, n):
            if deps.get(c, 0) < n:
                deps[c] = n
        for b in reads:
            if b.w is not None:
                add(*b.w)
        for b in writes:
            if b.w is not None and b.w[0] != eng:
                add(*b.w)
            for c, n in b.r.items():
                if c != eng:
                    add(c, n)
        return deps

    def _emit_waits(self, eng, deps):
        e = self.engs[eng]
        wd = self.waited[eng]
        for c, n in deps.items():
            if c == eng and eng == "pe":
                continue
            if self.chan[c].step == 16:
                n = max(n, self.chan[c].cnt)
            if wd.get(c, 0) >= n:
                continue
            e.wait_ge(self.chan[c].sem, n)
            wd[c] = n
            self.nwait += 1

    def op(self, eng, fn, reads=(), writes=(), signal=True):
        deps = self._deps(eng, reads, writes)
        self._emit_waits(eng, deps)
        ins = fn(self.engs[eng])
        ch = self.chan[eng]
        self.ninst[eng] += 1
        if signal:
            ch.cnt += 1
            ins.then_inc(ch.sem, 1)
            tag = (eng, ch.cnt)
        else:
            tag = (eng, ch.cnt + 1)
        for b in writes:
            b.w = tag
            b.r = {}
        for b in reads:
            if b.r.get(eng, 0) < tag[1]:
                b.r[eng] = tag[1]
        return ins

    def dma(self, q, ch, pairs, reads=(), writes=(), **kw):
        deps = self._deps("__dma__", reads, writes)
        self._emit_waits(q, deps)
        e = self.engs[q]
        for (o, i) in pairs:
            e.dma_start(out=o, in_=i, **kw).then_inc(ch.sem, 16)
            ch.cnt += 16
            self.ninst[q] += 1
        tag = (ch.name, ch.cnt)
        for b in writes:
            b.w = tag
            b.r = {}
        for b in reads:
            if b.r.get(ch.name, 0) < tag[1]:
                b.r[ch.name] = tag[1]

    def collective(self, src, dst, nranks, reads=(), writes=()):
        if "cc" not in self.chan:
            sem = self.stack.enter_context(self.nc.semaphore("cc_sem"))
            self.chan["cc"] = Chan("cc", sem, 1)
        ch = self.chan["cc"]
        deps = self._deps("__cc__", reads, writes)
        self._emit_waits("pool", deps)
        ins = self.nc.gpsimd.collective_compute("AllGather", ALU.bypass, replica_groups=[list(range(nranks))],
                                                ins=[src.opt()], outs=[dst.opt()])
        ins.then_inc(ch.sem)
        ch.cnt += 1
        self.ninst["pool"] += 1
        tag = ("cc", ch.cnt)
        for b in writes:
            b.w = tag
            b.r = {}
        for b in reads:
            if b.r.get("cc", 0) < tag[1]:
                b.r["cc"] = tag[1]

    def barrier(self):
        for eng in self.engs:
            deps = {}
            for c in self.chan.values():
                if c.cnt > 0 and c.name != eng:
                    deps[c.name] = c.cnt
            self._emit_waits(eng, deps)


def host_consts(T=2048):
    c = {}
    idx = np.arange(128)
    c["ones_f"] = np.ones((128, 128), np.float32)
    c["ident_f"] = np.eye(128, dtype=np.float32)
    c["ident_b"] = np.eye(128, dtype=np.float32).astype(ml_dtypes.bfloat16)
    c["tri_f"] = (idx[:, None] <= idx[None, :]).astype(np.float32)
    c["negm_f"] = np.where(idx[:, None] <= idx[None, :], 0.0, -30000.0).astype(np.float32)
    lg = np.log1p(-np.power(2.0, -5.0 - np.arange(RET_H, dtype=np.float64)))
    rel = (idx[None, :] - idx[:, None]).astype(np.float64)
    decT = np.where(rel[None] >= 0, np.exp(np.maximum(rel[None], 0) * lg[:, None, None]), 0.0)
    c["decayT"] = np.ascontiguousarray(decT.transpose(1, 0, 2)).astype(np.float32)
    xi = np.exp((idx[None, :] + 1.0) * lg[:, None])
    c["xirep"] = np.ascontiguousarray(np.broadcast_to(xi[None], (128, RET_H, 128))).astype(np.float32)
    zeta = np.exp((127.0 - idx[:, None]) * lg[None, :])
    c["zeta"] = zeta.astype(np.float32)
    c["gchunk"] = [float(np.exp(128.0 * v)) for v in lg]
    nch = T // 128
    tpos = (np.arange(nch)[None, :, None] * 128 + idx[:, None, None]).astype(np.float64)
    c["zetaF"] = np.exp((T - 1.0 - tpos) * lg[None, None, :]).astype(np.float32)
    invf = np.power(np.float32(10000.0), -np.linspace(0.0, 1.0, 128, dtype=np.float32)).astype(np.float32)
    c["invf_col"] = invf.reshape(128, 1).copy()
    c["invf_rep"] = np.ascontiguousarray(np.broadcast_to(invf[None, :], (128, 128))).astype(np.float32)
    return c


CONST_NAMES = ["ones_f", "ident_f", "ident_b", "tri_f", "negm_f", "decayT", "xirep", "zeta",
               "invf_col", "invf_rep", "zetaF"]


class P:
    pass


def build(T, n_layers=4, exch=0):
    HC = host_consts(T)
    TG = min(T, 2048)
    NG = T // TG
    NCH = T // 128
    nc_real = bass.Bass("TRN2", target_bir_lowering=False)

    class NCW:
        def __init__(self, n):
            self._n = n
            self._uid = 0

        def __getattr__(self, a):
            return getattr(self._n, a)

        def sbuf_tensor(self, name, shape, dtype):
            self._uid += 1
            return self._n.sbuf_tensor(f"{name}_u{self._uid}", shape, dtype)

        def psum_tensor(self, name, shape, dtype):
            self._uid += 1
            return self._n.psum_tensor(f"{name}_u{self._uid}", shape, dtype)

    nc = NCW(nc_real)
    dt = nc.dram_tensor

    def din(name, shape, dtype):
        return dt(name, list(shape), dtype, kind="ExternalInput").ap()

    def dscr(name, shape, dtype):
        return dt(name, list(shape), dtype, kind="Internal").ap()

    xT_in = din("xT", [D, T], F32)
    pos_in = din("pos", [1, T], I32)
    postm_in = din("pos_tm", [128, NCH], I32)
    gcol_in = din("gcol", [128, 16, DC], F32)
    ret_w_in = din("ret_w_in", [2, D, RET_IN], F32)
    ret_w_out = din("ret_w_out", [2, 2 * D, D], F32)
    ml_w_in = din("ml_w_in", [2, D, ML_IN], F32)
    ml_bg = din("ml_bg", [128, 2, 16], F32)
    ml_ng = din("ml_ng", [128, 2, D], F32)
    ml_w_out = din("ml_w_out", [2, D, D], F32)
    w1_in = din("mlp_w1", [4, D, DFF], F32)
    w2_in = din("mlp_w2", [4, DFF, D], F32)
    cin = {n: din("c_" + n, HC[n].shape, BF16 if n == "ident_b" else F32) for n in CONST_NAMES}
    outT = dt("outT", [D, T], F32, kind="ExternalOutput").ap()
    if exch:
        selw_in = din("selw", [128, 8], F32)
        Sx_d = dscr("Sx_d", [D, 512], F32)
        G_d = dscr("G_d", [exch * D, 512], F32)
        Cx_d = dscr("Cx_d", [1152, 256], F32)
        Gc_d = dscr("Gc_d", [exch * 1152, 256], F32)

    xT_d = dscr("xT_d", [D, T], F32)
    yT_d = dscr("yT_d", [D, T], F32)
    qT_d = dscr("qT_d", [D, T], BF16)
    kT_d = dscr("kT_d", [D, T], BF16)
    ktm_d = dscr("ktm_d", [T, D], BF16)
    vtm_d = dscr("vtm_d", [T, 2 * D], BF16)
    gtm_d = dscr("gtm_d", [T, 2 * D], BF16)
    zT_d = dscr("zT_d", [2 * D, T], BF16)
    uT_d = dscr("uT_d", [DFF, T], BF16)
    gate_d = dscr("gate_d", [T, 16], F32)
    cosT_d = dscr("cosT_d", [128, T], F32)
    sinT_d = dscr("sinT_d", [128, T], F32)
    costm_d = dscr("costm_d", [128, NCH, 128], F32)
    sintm_d = dscr("sintm_d", [128, NCH, 128], F32)

    st = ExitStack()
    with st:
        tr = Tracker(nc, st)
        sb = lambda name, shape, dtype: st.enter_context(nc.sbuf_tensor(name, list(shape), dtype))

        K = P()
        K.ones_f = sb("ones_f", [128, 128], F32)
        K.ident_f = sb("ident_f", [128, 128], F32)
        K.ident_b = sb("ident_b", [128, 128], BF16)
        K.ones_b = sb("ones_b", [128, 128], BF16)
        K.gcol = sb("gcol", [128, 16, DC], F32)
        K.eps = sb("eps_t", [128, 1], F32)
        K.negpi = sb("negpi_t", [128, 1], F32)
        K.one = sb("one_t", [128, 1], F32)
        b_const = Buf("const")
        cch = tr.dma_chan("const")
        tr.dma("sp", cch, [(K.ones_f[:, :], cin["ones_f"]), (K.ident_f[:, :], cin["ident_f"]),
                           (K.ident_b[:, :], cin["ident_b"]), (K.gcol[:, :, :], gcol_in)], writes=[b_const])
        tr.op("dve", lambda e: e.memset(K.eps[:, :], EPS), writes=[b_const])
        tr.op("dve", lambda e: e.memset(K.negpi[:, :], -PI), writes=[b_const])
        tr.op("dve", lambda e: e.memset(K.one[:, :], 1.0), writes=[b_const])
        tr.op("dve", lambda e: e.memset(K.ones_b[:, :], 1.0), writes=[b_const])
        tr.barrier()

        DB = {n: Buf(n) for n in ["xT", "yT", "qT", "kT", "ktm", "vtm", "gtm", "zT", "uT", "gate", "tab", "out"]}
        DBX = {n: Buf(n) for n in ["Sx", "G", "Cx", "Gc"]}

        psum_names = [f"ps{i}" for i in range(8)]

        def phase_tables():
            with ExitStack() as ph:
                t = lambda name, shape, dtype: ph.enter_context(nc.sbuf_tensor(name, list(shape), dtype))
                ch = tr.dma_chan("ph0")
                cho = tr.dma_chan("ph1")
                invc = t("invc", [128, 1], F32)
                invr = t("invr", [128, 128], F32)
                b0 = Buf()
                tr.dma("sp", ch, [(invc[:, :], cin["invf_col"]), (invr[:, :], cin["invf_rep"])], writes=[b0])
                pi_ = t("tb_pi", [128, 512], I32)
                pf = t("tb_pf", [128, 512], F32)
                an = t("tb_an", [128, 512], F32)
                rs = t("tb_rs", [128, 512], F32)
                so = t("tb_so", [128, 512], F32)
                co = t("tb_co", [128, 512], F32)
                bpi, bpf, ban, brs, bso, bco = (Buf() for _ in range(6))
                ki = t("tb_ki", [128, 512], I32)
                mm_ = t("tb_m", [128, 512], F32)
                bki, bmm = Buf(), Buf()

                def wrap_clamp():
                    tr.op("dve", lambda e: e.tensor_scalar(out=mm_[:, :], in0=rs[:, :], scalar1=PI, scalar2=-2 * PI,
                                                           op0=ALU.is_gt, op1=ALU.mult), reads=[brs], writes=[bmm])
                    tr.op("dve", lambda e: e.tensor_tensor(out=rs[:, :], in0=rs[:, :], in1=mm_[:, :], op=ALU.add),
                          reads=[brs, bmm], writes=[brs])
                    tr.op("dve", lambda e: e.tensor_scalar(out=rs[:, :], in0=rs[:, :], scalar1=-PI, scalar2=PI,
                                                           op0=ALU.max, op1=ALU.min), reads=[brs], writes=[brs])

                def sincos():
                    tr.op("dve", lambda e: e.tensor_scalar(out=mm_[:, :], in0=an[:, :], scalar1=1.0 / (2 * PI), scalar2=None,
                                                           op0=ALU.mult), reads=[ban], writes=[bmm])
                    tr.op("dve", lambda e: e.tensor_copy(out=ki[:, :], in_=mm_[:, :]), reads=[bmm], writes=[bki])
                    tr.op("dve", lambda e: e.tensor_copy(out=mm_[:, :], in_=ki[:, :]), reads=[bki], writes=[bmm])
                    tr.op("dve", lambda e: e.scalar_tensor_tensor(out=rs[:, :], in0=mm_[:, :], scalar=-2 * PI, in1=an[:, :],
                                                                  op0=ALU.mult, op1=ALU.add), reads=[bmm, ban], writes=[brs])
                    wrap_clamp()
                    tr.op("act", lambda e: e.activation(out=so[:, :], in_=rs[:, :], func=AF.Sin), reads=[brs], writes=[bso])
                    tr.op("dve", lambda e: e.tensor_scalar(out=rs[:, :], in0=rs[:, :], scalar1=0.5 * PI, scalar2=None,
                                                           op0=ALU.add), reads=[brs], writes=[brs])
                    wrap_clamp()
                    tr.op("act", lambda e: e.activation(out=co[:, :], in_=rs[:, :], func=AF.Sin), reads=[brs], writes=[bco])
                for i in range(T // 512):
                    sl = slice(i * 512, (i + 1) * 512)
                    tr.dma("sp", ch, [(pi_[:, :], pos_in[:, sl].partition_broadcast(128))], writes=[bpi])
                    tr.op("dve", lambda e: e.tensor_copy(out=pf[:, :], in_=pi_[:, :]), reads=[bpi], writes=[bpf])
                    tr.op("dve", lambda e: e.tensor_scalar(out=an[:, :], in0=pf[:, :], scalar1=invc[:, 0:1], scalar2=None,
                                                           op0=ALU.mult), reads=[bpf, b0], writes=[ban])
                    sincos()
                    tr.dma("sp", cho, [(sinT_d[:, sl], so[:, :])], reads=[bso], writes=[DB["tab"]])
                    tr.dma("sp", cho, [(cosT_d[:, sl], co[:, :])], reads=[bco], writes=[DB["tab"]])
                pti = t("tb_pti", [128, NCH], I32)
                ptf = t("tb_ptf", [128, NCH], F32)
                bpt = Buf()
                tr.dma("sp", ch, [(pti[:, :], postm_in)], writes=[bpt])
                tr.op("dve", lambda e: e.tensor_copy(out=ptf[:, :], in_=pti[:, :]), reads=[bpt], writes=[bpt])
                for i in range(NCH // 4):
                    for j in range(4):
                        c = i * 4 + j
                        tr.op("dve", lambda e: e.tensor_scalar(out=an[:, j * 128:(j + 1) * 128], in0=invr[:, :],
                                                               scalar1=ptf[:, c:c + 1], scalar2=None, op0=ALU.mult),
                              reads=[bpt, b0], writes=[ban])
                    sincos()
                    tr.dma("sp", cho, [(sintm_d[:, i * 4:(i + 1) * 4, :], so[:, :].rearrange("p (c f) -> p c f", c=4))],
                           reads=[bso], writes=[DB["tab"]])
                    tr.dma("sp", cho, [(costm_d[:, i * 4:(i + 1) * 4, :], co[:, :].rearrange("p (c f) -> p c f", c=4))],
                           reads=[bco], writes=[DB["tab"]])
                tr.barrier()

        def phase_norm(g, hT, src_x, y_src, gpost, gnext, dst_x):
            with ExitStack() as ph:
                t = lambda name, shape, dtype: ph.enter_context(nc.sbuf_tensor(name, list(shape), dtype))
                xs = t("n_xs", [128, DC, 512], F32)
                ys = t("n_ys", [128, DC, 512], F32) if y_src is not None else None
                sq = t("n_sq", [128, DC, 512], BF16)
                rstd = t("n_rstd", [128, 512], F32)
                tmp = [t(f"n_tmp{i}", [128, 512], F32) for i in range(2)]
                ps = ph.enter_context(nc.psum_tensor("n_ps", [128, 512], F32))
                bx, by, bsq, brs, bps = Buf("xs"), Buf("ys"), Buf("sq"), Buf("rstd"), Buf("nps")
                btmp = [Buf(), Buf()]
                chx, chy, cho = tr.dma_chan("ph0"), tr.dma_chan("ph1"), tr.dma_chan("ph2")
                b_h = hT.buf if hT is not None else None

                def stats(src, bsrc):
                    for q4 in range(4):
                        sl4 = slice(q4 * 4, (q4 + 1) * 4)
                        if q4 % 2 == 0:
                            tr.op("act", lambda e: e.activation(out=sq[:, sl4, :], in_=src[:, sl4, :], func=AF.Square),
                                  reads=[bsrc], writes=[bsq])
                        else:
                            tr.op("pool", lambda e: e.tensor_tensor(out=sq[:, sl4, :], in0=src[:, sl4, :], in1=src[:, sl4, :],
                                                                    op=ALU.mult), reads=[bsrc], writes=[bsq])
                    for dc in range(DC):
                        tr.op("pe", lambda e: e.matmul(ps[:, :], K.ones_b[:, :], sq[:, dc, :], start=(dc == 0),
                                                       stop=(dc == DC - 1)), reads=[bsq, b_const], writes=[bps],
                              signal=(dc == DC - 1))
                    tr.op("act", lambda e: e.activation(out=rstd[:, :], in_=ps[:, :], func=AF.Sqrt, scale=1.0 / D,
                                                        bias=K.eps[:, 0:1]), reads=[bps, b_const], writes=[brs])
                    tr.op("dve", lambda e: e.reciprocal(out=rstd[:, :], in_=rstd[:, :]), reads=[brs], writes=[brs])

                for tb in range(TG // 512):
                    sl = slice(g * TG + tb * 512, g * TG + (tb + 1) * 512)
                    tr.dma("sp", chx, [(xs[:, :, :], src_x[:, sl].rearrange("(dc p) t -> p dc t", p=128))],
                           reads=[DB["xT"]], writes=[bx])
                    if y_src is not None:
                        tr.dma("sp", chy, [(ys[:, :, :], y_src[:, sl].rearrange("(dc p) t -> p dc t", p=128))],
                               reads=[DB["yT"]], writes=[by])
                        stats(ys, by)
                        for dc in range(DC):
                            tt = dc % 2
                            tr.op("dve", lambda e: e.scalar_tensor_tensor(out=tmp[tt][:, :], in0=ys[:, dc, :],
                                                                          scalar=K.gcol[:, gpost, dc:dc + 1], in1=rstd[:, :],
                                                                          op0=ALU.mult, op1=ALU.mult),
                                  reads=[by, brs, b_const], writes=[btmp[tt]])
                            tr.op("pool", lambda e: e.tensor_tensor(out=xs[:, dc, :], in0=xs[:, dc, :], in1=tmp[tt][:, :],
                                                                    op=ALU.add), reads=[bx, btmp[tt]], writes=[bx])
                    if dst_x is not None:
                        tr.dma("sp", cho, [(dst_x[:, sl].rearrange("(dc p) t -> p dc t", p=128), xs[:, :, :])],
                               reads=[bx], writes=[DB["out"] if dst_x is outT else DB["xT"]])
                    if gnext is not None:
                        stats(xs, bx)
                        for dc in range(DC):
                            tr.op("dve", lambda e: e.scalar_tensor_tensor(out=hT.t[:, dc, tb * 512:(tb + 1) * 512],
                                                                          in0=xs[:, dc, :], scalar=K.gcol[:, gnext, dc:dc + 1],
                                                                          in1=rstd[:, :], op0=ALU.mult, op1=ALU.mult),
                                  reads=[bx, brs, b_const], writes=[b_h])
                tr.barrier()

        def load_w_panel(wt, bw, chw, W, c0, ncols, KC):
            Wv = W.rearrange("(kc p) n -> p kc n", p=128)
            pairs = []
            for k0 in range(0, KC, 16):
                k1 = min(KC, k0 + 16)
                pairs.append((wt[:, k0:k1, 0:ncols], Wv[:, k0:k1, c0:c0 + ncols]))
            tr.dma("pool", chw, pairs, writes=[bw])

        def gemm_fm(g, hT, W, panels, epilogue, tag):
            with ExitStack() as ph:
                t = lambda name, shape, dtype: ph.enter_context(nc.sbuf_tensor(name, list(shape), dtype))
                wt = [t(f"gw{i}", [128, DC, 256], BF16) for i in range(2)]
                bw = [Buf(), Buf()]
                chw = [tr.dma_chan("w0"), tr.dma_chan("w1")]
                ps = [ph.enter_context(nc.psum_tensor(f"gps{i}", [128, 512], F32)) for i in range(8)]
                bps = [Buf() for _ in range(8)]
                NTB = TG // 512
                env = epilogue("init", ph, t)
                load_w_panel(wt[0], bw[0], chw[0], W, panels[0], 256, DC)
                for pi, c0 in enumerate(panels):
                    s = pi % 2
                    if pi + 1 < len(panels):
                        load_w_panel(wt[1 - s], bw[1 - s], chw[1 - s], W, panels[pi + 1], 256, DC)
                    for tb in range(NTB):
                        for fb in range(2):
                            bank = (tb % 4) * 2 + fb
                            for kc in range(DC):
                                tr.op("pe", lambda e: e.matmul(ps[bank][:, :], wt[s][:, kc, fb * 128:(fb + 1) * 128],
                                                               hT.t[:, kc, tb * 512:(tb + 1) * 512], start=(kc == 0),
                                                               stop=(kc == DC - 1)),
                                      reads=[bw[s], hT.buf], writes=[bps[bank]], signal=(kc == DC - 1))
                        b0, b1 = (tb % 4) * 2, (tb % 4) * 2 + 1
                        epilogue("ep", env, (pi, c0, g * TG + tb * 512, ps[b0], bps[b0], ps[b1], bps[b1]))
                tr.barrier()

        def gemm_tm(g, hT, W, panels, epilogue):
            with ExitStack() as ph:
                t = lambda name, shape, dtype: ph.enter_context(nc.sbuf_tensor(name, list(shape), dtype))
                wt = [t(f"gw{i}", [128, DC, 512], BF16) for i in range(2)]
                bw = [Buf(), Buf()]
                chw = [tr.dma_chan("w0"), tr.dma_chan("w1")]
                ps = [ph.enter_context(nc.psum_tensor(f"gps{i}", [128, 512], F32)) for i in range(8)]
                bps = [Buf() for _ in range(8)]
                NTT = TG // 128
                env = epilogue("init", ph, t)
                load_w_panel(wt[0], bw[0], chw[0], W, panels[0][0], panels[0][1], DC)
                for pi, (c0, ncol) in enumerate(panels):
                    s = pi % 2
                    if pi + 1 < len(panels):
                        load_w_panel(wt[1 - s], bw[1 - s], chw[1 - s], W, panels[pi + 1][0], panels[pi + 1][1], DC)
                    for tt in range(NTT):
                        bank = tt % 8
                        for kc in range(DC):
                            tr.op("pe", lambda e: e.matmul(ps[bank][:, 0:ncol], hT.t[:, kc, tt * 128:(tt + 1) * 128],
                                                           wt[s][:, kc, 0:ncol], start=(kc == 0), stop=(kc == DC - 1)),
                                  reads=[bw[s], hT.buf], writes=[bps[bank]], signal=(kc == DC - 1))
                        epilogue("ep", env, (pi, c0, ncol, g * TG + tt * 128, ps[bank], bps[bank]))
                tr.barrier()

        def gemm_stream(g, aT_d, b_a, KC, W, y_d, b_y):
            with ExitStack() as ph:
                t = lambda name, shape, dtype: ph.enter_context(nc.sbuf_tensor(name, list(shape), dtype))
                NW = 4
                wt = [t(f"sw{i}", [128, KC, 256], BF16) for i in range(NW)]
                bw = [Buf() for _ in range(NW)]
                chw = [tr.dma_chan(f"w{i}") for i in range(NW)]
                NTH = 2 if TG >= 1024 else 1
                TH = TG // NTH
                NTB = TH // 512
                NA = 4
                at = [t(f"sa{i}", [128, TH], BF16) for i in range(NA)]
                ba = [Buf() for _ in range(NA)]
                cha = [tr.dma_chan(f"a{i}") for i in range(NA)]
                ot = [t(f"so{i}", [128, 512], F32) for i in range(4)]
                bo = [Buf() for _ in range(4)]
                cho = [tr.dma_chan(f"o{i}") for i in range(4)]
                ps = [ph.enter_context(nc.psum_tensor(f"gps{i}", [128, 512], F32)) for i in range(8)]
                bps = [Buf() for _ in range(8)]
                av = aT_d.rearrange("(kc p) t -> p kc t", p=128)
                npair = D // 512

                def load_pair(pp):
                    for q in range(2):
                        k = (pp % 2) * 2 + q
                        load_w_panel(wt[k], bw[k], chw[k], W, pp * 512 + q * 256, 256, KC)

                load_pair(0)
                ai = 0
                oi = 0
                for pp in range(npair):
                    if pp + 1 < npair:
                        load_pair(pp + 1)
                    for th in range(NTH):
                        t0 = g * TG + th * TH
                        for kc in range(KC):
                            a = ai % NA
                            ai += 1
                            tr.dma("sp", cha[a], [(at[a][:, :], av[:, kc, t0:t0 + TH])], reads=[b_a], writes=[ba[a]])
                            for fb4 in range(4):
                                k = (pp % 2) * 2 + fb4 // 2
                                fb = fb4 % 2
                                for tb in range(NTB):
                                    bank = fb4 * 2 + tb
                                    tr.op("pe", lambda e: e.matmul(ps[bank][:, :], wt[k][:, kc, fb * 128:(fb + 1) * 128],
                                                                   at[a][:, tb * 512:(tb + 1) * 512], start=(kc == 0),
                                                                   stop=(kc == KC - 1)),
                                          reads=[bw[k], ba[a]], writes=[bps[bank]],
                                          signal=(kc == KC - 1 or (fb4 == 3 and tb == NTB - 1)))
                        for fb4 in range(4):
                            for tb in range(NTB):
                                bank = fb4 * 2 + tb
                                o = oi % 4
                                oi += 1
                                if o % 2 == 0:
                                    tr.op("act", lambda e: e.activation(out=ot[o][:, :], in_=ps[bank][:, :], func=AF.Copy),
                                          reads=[bps[bank]], writes=[bo[o]])
                                else:
                                    tr.op("dve", lambda e: e.tensor_copy(out=ot[o][:, :], in_=ps[bank][:, :]),
                                          reads=[bps[bank]], writes=[bo[o]])
                                r0 = pp * 512 + fb4 * 128
                                c0 = t0 + tb * 512
                                tr.dma("sp", cho[o], [(y_d[r0:r0 + 128, c0:c0 + 512], ot[o][:, :])], reads=[bo[o]], writes=[b_y])
                tr.barrier()

        def ret_inproj(g, hT, W):
            def ep_fm(mode, env, a):
                if mode == "init":
                    ph, t = env, a
                    E = P()
                    E.cos = t("r_cos", [128, TG], F32)
                    E.sin = t("r_sin", [128, TG], F32)
                    E.btab = Buf()
                    ch = tr.dma_chan("ph0")
                    tr.dma("sp", ch, [(E.cos[:, :], cosT_d[:, g * TG:(g + 1) * TG]), (E.sin[:, :], sinT_d[:, g * TG:(g + 1) * TG])],
                           reads=[DB["tab"]], writes=[E.btab])
                    E.tmp = [t(f"r_t{i}", [128, 512], F32) for i in range(4)]
                    E.bt = [Buf() for _ in range(4)]
                    E.o = [t(f"r_o{i}", [128, 512], BF16) for i in range(4)]
                    E.bo = [Buf() for _ in range(4)]
                    E.cho = [tr.dma_chan(f"o{i}") for i in range(4)]
                    E.i = 0
                    return E
                E = env
                pi, c0, tok0, ps0, bp0, ps1, bp1 = a
                isk = c0 >= D
                scl = (RET_DK ** -0.5) if isk else 1.0
                dst = kT_d if isk else qT_d
                bdst = DB["kT"] if isk else DB["qT"]
                r0 = c0 - D if isk else c0
                lt = slice(tok0 - g * TG, tok0 - g * TG + 512)
                cs, sn = E.cos[:, lt], E.sin[:, lt]
                A, B_, C, Dd = E.tmp
                bA, bB, bC, bD = E.bt
                o1, o2 = E.o[(E.i * 2) % 4], E.o[(E.i * 2 + 1) % 4]
                bo1, bo2 = E.bo[(E.i * 2) % 4], E.bo[(E.i * 2 + 1) % 4]
                c1, c2 = E.cho[(E.i * 2) % 4], E.cho[(E.i * 2 + 1) % 4]
                E.i += 1
                stt = lambda out, in0, in1: (lambda e: e.scalar_tensor_tensor(out=out, in0=in0, scalar=scl, in1=in1,
                                                                              op0=ALU.mult, op1=ALU.mult))
                tr.op("dve", stt(A[:, :], ps0[:, :], cs), reads=[bp0, E.btab], writes=[bA])
                tr.op("dve", stt(B_[:, :], ps1[:, :], sn), reads=[bp1, E.btab], writes=[bB])
                tr.op("dve", stt(C[:, :], ps1[:, :], cs), reads=[bp1, E.btab], writes=[bC])
                tr.op("dve", stt(Dd[:, :], ps0[:, :], sn), reads=[bp0, E.btab], writes=[bD])
                tr.op("pool", lambda e: e.tensor_tensor(out=o1[:, :], in0=A[:, :], in1=B_[:, :], op=ALU.subtract),
                      reads=[bA, bB], writes=[bo1])
                tr.op("pool", lambda e: e.tensor_tensor(out=o2[:, :], in0=C[:, :], in1=Dd[:, :], op=ALU.add),
                      reads=[bC, bD], writes=[bo2])
                tr.dma("sp", c1, [(dst[r0:r0 + 128, tok0:tok0 + 512], o1[:, :])], reads=[bo1], writes=[bdst])
                tr.dma("sp", c2, [(dst[r0 + 128:r0 + 256, tok0:tok0 + 512], o2[:, :])], reads=[bo2], writes=[bdst])

            gemm_fm(g, hT, W, [h * 256 for h in range(16)], ep_fm, "ret")

            def ep_tm(mode, env, a):
                if mode == "init":
                    ph, t = env, a
                    E = P()
                    NCG = TG // 128
                    E.cos = t("r_cos", [128, NCG, 128], F32)
                    E.sin = t("r_sin", [128, NCG, 128], F32)
                    E.btab = Buf()
                    ch = tr.dma_chan("ph0")
                    tr.dma("sp", ch, [(E.cos[:, :, :], costm_d[:, g * NCG:(g + 1) * NCG, :]),
                                      (E.sin[:, :, :], sintm_d[:, g * NCG:(g + 1) * NCG, :])],
                           reads=[DB["tab"]], writes=[E.btab])
                    E.tmp = [t(f"r_t{i}", [128, 2, 128], F32) for i in range(4)]
                    E.bt = [Buf() for _ in range(4)]
                    E.o = [t(f"r_o{i}", [128, 512], BF16) for i in range(4)]
                    E.bo = [Buf() for _ in range(4)]
                    E.cho = [tr.dma_chan(f"o{i}") for i in range(4)]
                    E.i = 0
                    return E
                E = env
                pi, c0, ncol, tok0, ps, bp = a
                o = E.o[E.i % 4]
                bo = E.bo[E.i % 4]
                co = E.cho[E.i % 4]
                par = E.i % 2
                E.i += 1
                if c0 < 2 * D:
                    lc = (tok0 - g * TG) // 128
                    scl = RET_DK ** -0.5
                    pv = ps[:, :].rearrange("p (h two f) -> p h two f", h=2, two=2)
                    ov = o[:, :].rearrange("p (h two f) -> p h two f", h=2, two=2)
                    cs = E.cos[:, lc, :].unsqueeze(1).to_broadcast([128, 2, 128])
                    sn = E.sin[:, lc, :].unsqueeze(1).to_broadcast([128, 2, 128])
                    A, B_, C, Dd = E.tmp
                    bA, bB, bC, bD = E.bt
                    stt = lambda out, in0, in1: (lambda e: e.scalar_tensor_tensor(out=out, in0=in0, scalar=scl, in1=in1,
                                                                                  op0=ALU.mult, op1=ALU.mult))
                    tr.op("dve", stt(A[:, :, :], pv[:, :, 0, :], cs), reads=[bp, E.btab], writes=[bA])
                    tr.op("dve", stt(B_[:, :, :], pv[:, :, 1, :], sn), reads=[bp, E.btab], writes=[bB])
                    tr.op("dve", stt(C[:, :, :], pv[:, :, 1, :], cs), reads=[bp, E.btab], writes=[bC])
                    tr.op("dve", stt(Dd[:, :, :], pv[:, :, 0, :], sn), reads=[bp, E.btab], writes=[bD])
                    tr.op("pool", lambda e: e.tensor_tensor(out=ov[:, :, 0, :], in0=A[:, :, :], in1=B_[:, :, :], op=ALU.subtract),
                          reads=[bA, bB], writes=[bo])
                    tr.op("pool", lambda e: e.tensor_tensor(out=ov[:, :, 1, :], in0=C[:, :, :], in1=Dd[:, :, :], op=ALU.add),
                          reads=[bC, bD], writes=[bo])
                    tr.dma("sp", co, [(ktm_d[tok0:tok0 + 128, c0 - D:c0 - D + 512], o[:, :])], reads=[bo], writes=[DB["ktm"]])
                elif c0 < 4 * D:
                    if par == 0:
                        tr.op("act", lambda e: e.activation(out=o[:, :], in_=ps[:, :], func=AF.Copy), reads=[bp], writes=[bo])
                    else:
                        tr.op("dve", lambda e: e.tensor_copy(out=o[:, :], in_=ps[:, :]), reads=[bp], writes=[bo])
                    tr.dma("sp", co, [(vtm_d[tok0:tok0 + 128, c0 - 2 * D:c0 - 2 * D + 512], o[:, :])], reads=[bo], writes=[DB["vtm"]])
                else:
                    tr.op("act", lambda e: e.activation(out=o[:, :], in_=ps[:, :], func=AF.Silu), reads=[bp], writes=[bo])
                    tr.dma("sp", co, [(gtm_d[tok0:tok0 + 128, c0 - 4 * D:c0 - 4 * D + 512], o[:, :])], reads=[bo], writes=[DB["gtm"]])

            gemm_tm(g, hT, W, [(D + i * 512, 512) for i in range(20)], ep_tm)

        def ret_mixer(state):
            with ExitStack() as ph:
                t = lambda name, shape, dtype: ph.enter_context(nc.sbuf_tensor(name, list(shape), dtype))
                NJ = 4
                decT = t("m_dec", [128, RET_H, 128], F32)
                xir = t("m_xi", [128, RET_H, 128], F32)
                zet = t("m_zeta", [128, RET_H], F32)
                bc = Buf()
                ch = tr.dma_chan("ph0")
                tr.dma("sp", ch, [(decT[:, :, :], cin["decayT"]), (xir[:, :, :], cin["xirep"]), (zet[:, :], cin["zeta"])], writes=[bc])
                S = t("m_S", [128, RET_H, 2, RET_DV], F32)
                Sb = t("m_Sb", [128, RET_H, 2, RET_DV], BF16)
                bS = [Buf() for _ in range(RET_H)]
                bSb = [Buf() for _ in range(RET_H)]
                tr.op("pool", lambda e: e.memset(S[:, :, :, :], 0.0), writes=bS)
                tr.op("pool", lambda e: e.memset(Sb[:, :, :, :], 0.0), writes=bSb)
                NB = 2
                qt2 = [t(f"m_q{i}", [128, RET_H, 2, 256], BF16) for i in range(NB)]
                kt2 = [t(f"m_k{i}", [128, RET_H, 2, 256], BF16) for i in range(NB)]
                bqk = [Buf() for _ in range(NB)]
                chqk = [tr.dma_chan(f"qk{i}") for i in range(NB)]
                km = [t(f"m_km{i}", [128, D], BF16) for i in range(NB)]
                vm = [t(f"m_v{i}", [128, 2 * D], BF16) for i in range(NB)]
                gm = [t(f"m_g{i}", [128, 2 * D], BF16) for i in range(NB)]
                bin_ = [Buf() for _ in range(NB)]
                chin = [tr.dma_chan(f"a{i}") for i in range(NB)]
                qx = [t(f"m_qx{i}", [128, 2, 128], BF16) for i in range(NJ)]
                bqx = [Buf() for _ in range(NJ)]
                kz = [t(f"m_kz{i}", [128, 256], BF16) for i in range(NJ)]
                bkz = [Buf() for _ in range(NJ)]
                sT = [t(f"m_sT{i}", [128, 128], BF16) for i in range(NJ)]
                bsT = [Buf() for _ in range(NJ)]
                yn = [t(f"m_yn{i}", [128, RET_DV], F32) for i in range(NJ)]
                byn = [Buf() for _ in range(NJ)]
                z = [t(f"m_z{i}", [128, RET_DV], BF16) for i in range(NJ)]
                bz = [Buf() for _ in range(NJ)]
                stats = [t(f"m_st{i}", [128, 6], F32) for i in range(NJ)]
                mv = [t(f"m_mv{i}", [128, 2], F32) for i in range(NJ)]
                rs = [t(f"m_rs{i}", [128, 2], F32) for i in range(NJ)]
                bst = [Buf() for _ in range(NJ)]
                zT = [t(f"m_zT{i}", [128, 32, 512], BF16) for i in range(1)]
                bzT = Buf()
                chz = tr.dma_chan("o0")
                ps_y = [ph.enter_context(nc.psum_tensor(f"mps_y{i}", [128, 512], F32)) for i in range(NJ)]
                ps_u = [ph.enter_context(nc.psum_tensor(f"mps_u{i}", [128, 512], F32)) for i in range(2)]
                ps_t_all = ph.enter_context(nc.psum_tensor("mps_t", [128, 2, 4, 128], BF16))
                ps_s_all = ph.enter_context(nc.psum_tensor("mps_s", [128, 2, 128], F32))
                ps_t = [ps_t_all[:, i, :, :] for i in range(2)]
                ps_s = [ps_s_all[:, i, :] for i in range(2)]
                bps_s, bps_y, bps_u, bps_t = ([Buf() for _ in range(NJ)] for _ in range(4))

                def load(c):
                    i = c % NB
                    sl = slice(c * 128, (c + 1) * 128)
                    if c % 2 == 0:
                        i2 = (c // 2) % NB
                        s2 = slice(c * 128, (c + 2) * 128)
                        tr.dma("sp", chqk[i2], [
                            (qt2[i2][:, :, :, :], qT_d[:, s2].rearrange("(h two p) t -> p h two t", two=2, p=128)),
                            (kt2[i2][:, :, :, :], kT_d[:, s2].rearrange("(h two p) t -> p h two t", two=2, p=128))],
                            reads=[DB["qT"], DB["kT"]], writes=[bqk[i2]])
                    tr.dma("sp", chin[i], [
                        (km[i][:, :], ktm_d[sl, :]), (vm[i][:, :], vtm_d[sl, :]), (gm[i][:, :], gtm_d[sl, :])],
                        reads=[DB["ktm"], DB["vtm"], DB["gtm"]], writes=[bin_[i]])

                if exch:
                    zF = t("m_zF", [128, NCH, RET_H], F32)
                    selw = t("m_selw", [128, 8], F32)
                    kzp = [t(f"m_kzp{i}", [128, 2, 256], BF16) for i in range(2)]
                    bkzp = [Buf(), Buf()]
                    bzf = Buf()
                    tr.dma("sp", tr.dma_chan("ph1"), [(zF[:, :, :], cin["zetaF"]), (selw[:, :], selw_in)], writes=[bzf])
                    accs = [(ps_y[0], bps_y[0]), (ps_y[1], bps_y[1]), (ps_u[0], bps_u[0]), (ps_u[1], bps_u[1])]
                    li = 0
                    cho2 = [tr.dma_chan("o1"), tr.dma_chan("o2")]
                    for hp in range(RET_H // 2):
                        for c in range(NCH):
                            i = li % NB
                            li += 1
                            sl = slice(c * 128, (c + 1) * 128)
                            tr.dma("sp", chin[i], [(km[i][:, 0:512], ktm_d[sl, hp * 512:(hp + 1) * 512]),
                                                   (vm[i][:, 0:1024], vtm_d[sl, hp * 1024:(hp + 1) * 1024])],
                                   reads=[DB["ktm"], DB["vtm"]], writes=[bin_[i]])
                            kk = c % 2
                            tr.op("pool", lambda e: e.tensor_tensor(out=kzp[kk][:, :, :],
                                                                    in0=km[i][:, 0:512].rearrange("p (h d) -> p h d", h=2),
                                                                    in1=zF[:, c, hp * 2:hp * 2 + 2].unsqueeze(2).to_broadcast([128, 2, 256]),
                                                                    op=ALU.mult), reads=[bin_[i], bzf], writes=[bkzp[kk]])
                            for hl in range(2):
                                for dcc in range(2):
                                    pa, bpa = accs[hl * 2 + dcc]
                                    tr.op("pe", lambda e: e.matmul(pa[:, :], kzp[kk][:, hl, dcc * 128:(dcc + 1) * 128],
                                                                   vm[i][:, hl * 512:(hl + 1) * 512], start=(c == 0), stop=(c == NCH - 1)),
                                          reads=[bkzp[kk], bin_[i]], writes=[bpa], signal=(c == NCH - 1 or (hl == 1 and dcc == 1)))
                        for hl in range(2):
                            for dcc in range(2):
                                pa, bpa = accs[hl * 2 + dcc]
                                k2 = (hl * 2 + dcc) % 2
                                tr.op("act", lambda e: e.activation(out=yn[k2][:, :], in_=pa[:, :], func=AF.Copy), reads=[bpa], writes=[byn[k2]])
                                r0 = ((hp * 2 + hl) * 2 + dcc) * 128
                                tr.dma("sp", cho2[k2], [(Sx_d[r0:r0 + 128, :], yn[k2][:, :])], reads=[byn[k2]], writes=[DBX["Sx"]])
                    tr.collective(Sx_d, G_d, exch, reads=[DBX["Sx"]], writes=[DBX["G"]])
                    Gv = G_d.rearrange("(r b p) e -> r b p e", r=exch, p=128)
                    li = 0
                    for r in range(exch):
                        for b16 in range(16):
                            k2 = li % 2
                            li += 1
                            h_, dcc = b16 // 2, b16 % 2
                            tr.dma("sp", cho2[k2], [(yn[k2][:, :], Gv[r, b16, :, :])], reads=[DBX["G"]], writes=[byn[k2]])
                            tr.op("dve", lambda e: e.scalar_tensor_tensor(out=S[:, h_, dcc, :], in0=yn[k2][:, :], scalar=selw[:, r:r + 1],
                                                                          in1=S[:, h_, dcc, :], op0=ALU.mult, op1=ALU.add),
                                  reads=[byn[k2], bzf, bS[h_]], writes=[bS[h_]])
                    for h_ in range(RET_H):
                        tr.op("act", lambda e: e.activation(out=Sb[:, h_, :, :], in_=S[:, h_, :, :], func=AF.Copy),
                              reads=[bS[h_]], writes=[bSb[h_]])
                load(0)
                it = 0
                for c in range(NCH):
                    i = c % NB
                    if c + 1 < NCH:
                        load(c + 1)
                    c4 = c % 4
                    i2 = (c // 2) % NB
                    co = (c % 2) * 128
                    for h in range(RET_H):
                        j = it % NJ
                        j2 = it % 2
                        it += 1
                        for dcc in range(2):
                            tr.op("pe", lambda e: e.matmul(ps_s[j2][:, :], kt2[i2][:, h, dcc, co:co + 128], qt2[i2][:, h, dcc, co:co + 128],
                                                           start=(dcc == 0), stop=(dcc == 1)),
                                  reads=[bqk[i2]], writes=[bps_s[j2]], signal=(dcc == 1))
                        tr.op("dve", lambda e: e.tensor_tensor(out=sT[j][:, :], in0=ps_s[j2][:, :], in1=decT[:, h, :], op=ALU.mult),
                              reads=[bps_s[j2], bc], writes=[bsT[j]])
                        tr.op("pool", lambda e: e.tensor_tensor(out=qx[j][:, :, :], in0=qt2[i2][:, h, :, co:co + 128],
                                                                in1=xir[:, h, :].unsqueeze(1).to_broadcast([128, 2, 128]),
                                                                op=ALU.mult), reads=[bqk[i2], bc], writes=[bqx[j]])
                        tr.op("pe", lambda e: e.matmul(ps_y[j][:, :], sT[j][:, :], vm[i][:, h * 512:(h + 1) * 512],
                                                       start=True, stop=False),
                              reads=[bsT[j], bin_[i]], writes=[bps_y[j]], signal=False)
                        for dcc in range(2):
                            tr.op("pe", lambda e: e.matmul(ps_y[j][:, :], qx[j][:, dcc, :], Sb[:, h, dcc, :],
                                                           start=False, stop=(dcc == 1)),
                                  reads=[bqx[j], bSb[h]], writes=[bps_y[j]], signal=(dcc == 1))
                        tr.op("pool", lambda e: e.tensor_tensor(out=kz[j][:, :], in0=km[i][:, h * 256:(h + 1) * 256],
                                                                in1=zet[:, h:h + 1].to_broadcast([128, 256]), op=ALU.mult),
                              reads=[bin_[i], bc], writes=[bkz[j]])
                        for dcc in range(2):
                            tr.op("pe", lambda e: e.matmul(ps_u[dcc][:, :], kz[j][:, dcc * 128:(dcc + 1) * 128],
                                                           vm[i][:, h * 512:(h + 1) * 512], start=True, stop=True),
                                  reads=[bkz[j], bin_[i]], writes=[bps_u[dcc]])
                        for dcc in range(2):
                            tr.op("dve", lambda e: e.scalar_tensor_tensor(out=S[:, h, dcc, :], in0=S[:, h, dcc, :],
                                                                          scalar=HC["gchunk"][h], in1=ps_u[dcc][:, :],
                                                                          op0=ALU.mult, op1=ALU.add),
                                  reads=[bps_u[dcc], bS[h]], writes=[bS[h]])
                        tr.op("act", lambda e: e.activation(out=Sb[:, h, :, :], in_=S[:, h, :, :], func=AF.Copy),
                              reads=[bS[h]], writes=[bSb[h]])
                        tr.op("dve", lambda e: e.bn_stats(out=stats[j][:, :], in_=ps_y[j][:, :]), reads=[bps_y[j]], writes=[bst[j]])
                        tr.op("dve", lambda e: e.bn_aggr(out=mv[j][:, :], in_=stats[j][:, :]), reads=[bst[j]], writes=[bst[j]])
                        tr.op("act", lambda e: e.activation(out=rs[j][:, 0:1], in_=mv[j][:, 1:2], func=AF.Sqrt,
                                                            bias=K.eps[:, 0:1]), reads=[bst[j], b_const], writes=[bst[j]])
                        tr.op("dve", lambda e: e.reciprocal(out=rs[j][:, 0:1], in_=rs[j][:, 0:1]), reads=[bst[j]], writes=[bst[j]])
                        tr.op("dve", lambda e: e.scalar_tensor_tensor(out=rs[j][:, 1:2], in0=mv[j][:, 0:1], scalar=-1.0,
                                                                      in1=rs[j][:, 0:1], op0=ALU.mult, op1=ALU.mult),
                              reads=[bst[j]], writes=[bst[j]])
                        tr.op("act", lambda e: e.activation(out=yn[j][:, :], in_=ps_y[j][:, :], func=AF.Identity,
                                                            bias=rs[j][:, 1:2], scale=rs[j][:, 0:1]),
                              reads=[bps_y[j], bst[j]], writes=[byn[j]])
                        tr.op("pool", lambda e: e.tensor_tensor(out=z[j][:, :], in0=yn[j][:, :], in1=gm[i][:, h * 512:(h + 1) * 512],
                                                                op=ALU.mult), reads=[byn[j], bin_[i]], writes=[bz[j]])
                        for q4 in range(4):
                            tr.op("pe", lambda e: e.transpose(out=ps_t[j2][:, q4, :], in_=z[j][:, q4 * 128:(q4 + 1) * 128],
                                                              identity=K.ident_b[:, :]),
                                  reads=[bz[j], b_const], writes=[bps_t[j2]], signal=(q4 == 3))
                        tr.op("act", lambda e: e.activation(out=zT[0][:, h * 4:(h + 1) * 4, c4 * 128:(c4 + 1) * 128],
                                                            in_=ps_t[j2][:, :, :], func=AF.Copy),
                              reads=[bps_t[j2]], writes=[bzT])
                    if c4 == 3 or c == NCH - 1:
                        nt = (c4 + 1) * 128
                        t0 = (c - c4) * 128
                        tr.dma("sp", chz, [(zT_d[:, t0:t0 + nt].rearrange("(b p) t -> p b t", p=128), zT[0][:, :, 0:nt])],
                               reads=[bzT], writes=[DB["zT"]])
                tr.barrier()

        def ml_inproj(g, hT, W):
            def ep_fm(mode, env, a):
                if mode == "init":
                    ph, t = env, a
                    E = P()
                    E.o = [t(f"l_o{i}", [128, 512], BF16) for i in range(4)]
                    E.bo = [Buf() for _ in range(4)]
                    E.cho = [tr.dma_chan(f"o{i}") for i in range(4)]
                    E.i = 0
                    return E
                E = env
                pi, c0, tok0, ps0, bp0, ps1, bp1 = a
                isk = c0 >= 1024
                scl = (ML_DQK ** -0.5) if isk else 1.0
                dst = kT_d if isk else qT_d
                bdst = DB["kT"] if isk else DB["qT"]
                r0 = c0 - 1024 if isk else c0
                for fb, (ps, bp) in enumerate(((ps0, bp0), (ps1, bp1))):
                    k = E.i % 4
                    E.i += 1
                    if fb == 0:
                        tr.op("act", lambda e: e.activation(out=E.o[k][:, :], in_=ps[:, :], func=AF.Copy, scale=scl),
                              reads=[bp], writes=[E.bo[k]])
                    else:
                        tr.op("dve", lambda e: e.tensor_scalar(out=E.o[k][:, :], in0=ps[:, :], scalar1=scl, scalar2=None,
                                                               op0=ALU.mult), reads=[bp], writes=[E.bo[k]])
                    tr.dma("sp", E.cho[k], [(dst[r0 + fb * 128:r0 + fb * 128 + 128, tok0:tok0 + 512], E.o[k][:, :])],
                           reads=[E.bo[k]], writes=[bdst])

            gemm_fm(g, hT, W, [i * 256 for i in range(8)], ep_fm, "ml")

            def ep_tm(mode, env, a):
                if mode == "init":
                    ph, t = env, a
                    E = P()
                    E.o = [t(f"l_o{i}", [128, 512], BF16) for i in range(4)]
                    E.bo = [Buf() for _ in range(4)]
                    E.cho = [tr.dma_chan(f"o{i}") for i in range(4)]
                    E.og = [t(f"l_og{i}", [128, 16], F32) for i in range(2)]
                    E.bog = [Buf() for _ in range(2)]
                    E.chg = [tr.dma_chan(f"a{i}") for i in range(2)]
                    E.i = 0
                    return E
                E = env
                pi, c0, ncol, tok0, ps, bp = a
                k = E.i % 4
                E.i += 1
                o, bo, co = E.o[k], E.bo[k], E.cho[k]
                if c0 < 2048:
                    scl = ML_DQK ** -0.5
                    tr.op("act", lambda e: e.activation(out=o[:, :], in_=ps[:, :], func=AF.Copy, scale=scl), reads=[bp], writes=[bo])
                    tr.dma("sp", co, [(ktm_d[tok0:tok0 + 128, c0 - 1024:c0 - 1024 + 512], o[:, :])], reads=[bo], writes=[DB["ktm"]])
                elif c0 < 4096:
                    tr.op("dve", lambda e: e.tensor_copy(out=o[:, :], in_=ps[:, :]), reads=[bp], writes=[bo])
                    tr.dma("sp", co, [(vtm_d[tok0:tok0 + 128, c0 - 2048:c0 - 2048 + 512], o[:, :])], reads=[bo], writes=[DB["vtm"]])
                elif c0 < 6144:
                    tr.op("act", lambda e: e.activation(out=o[:, :], in_=ps[:, :], func=AF.Sigmoid), reads=[bp], writes=[bo])
                    tr.dma("sp", co, [(gtm_d[tok0:tok0 + 128, c0 - 4096:c0 - 4096 + 512], o[:, :])], reads=[bo], writes=[DB["gtm"]])
                else:
                    kk = E.i % 2
                    tr.op("dve", lambda e: e.tensor_copy(out=E.og[kk][:, :], in_=ps[:, 0:16]), reads=[bp], writes=[E.bog[kk]])
                    tr.dma("sp", E.chg[kk], [(gate_d[tok0:tok0 + 128, :], E.og[kk][:, :])], reads=[E.bog[kk]], writes=[DB["gate"]])

            gemm_tm(g, hT, W, [(1024 + i * 512, 512) for i in range(10)] + [(6144, 16)], ep_tm)

        def ml_mixer(jl):
            with ExitStack() as ph:
                t = lambda name, shape, dtype: ph.enter_context(nc.sbuf_tensor(name, list(shape), dtype))
                tri = t("x_tri", [128, 128], F32)
                negm = t("x_negm", [128, 128], F32)
                bgt = t("x_bg", [128, 16], F32)
                ngt = t("x_ng", [128, D], F32)
                onesb = t("x_1b", [128, 1], BF16)
                bc = Buf()
                ch = tr.dma_chan("ph0")
                tr.dma("sp", ch, [(tri[:, :], cin["tri_f"]), (negm[:, :], cin["negm_f"]), (bgt[:, :], ml_bg[:, jl, :]),
                                  (ngt[:, :], ml_ng[:, jl, :])], writes=[bc])
                tr.op("dve", lambda e: e.memset(onesb[:, :], 1.0), writes=[bc])
                NCH8 = NCH * 8
                gts = t("x_gts", [128, NCH, 16], F32)
                ilog = t("x_il", [128, NCH, 8], F32)
                flog = t("x_fl", [128, NCH, 8], F32)
                cS = t("x_cS", [128, NCH, 8], F32)
                wS = t("x_wS", [128, NCH, 8], F32)
                eBt = t("x_eBt", [128, NCH, 8], F32)
                bg_ = Buf()
                tr.dma("sp", tr.dma_chan("ph1"), [(gts[:, :, :], gate_d.rearrange("(c p) k -> p c k", p=128))], reads=[DB["gate"]], writes=[bg_])
                tr.op("dve", lambda e: e.tensor_tensor(out=gts[:, :, :], in0=gts[:, :, :],
                                                       in1=bgt[:, :].unsqueeze(1).to_broadcast([128, NCH, 16]), op=ALU.add),
                      reads=[bg_, bc], writes=[bg_])
                tr.op("act", lambda e: e.activation(out=gts[:, :, :], in_=gts[:, :, :], func=AF.Tanh, scale=1.0 / SOFTCAP),
                      reads=[bg_], writes=[bg_])
                bil, bfl = Buf(), Buf()
                tr.op("dve", lambda e: e.tensor_scalar(out=ilog[:, :, :], in0=gts[:, :, 0:8], scalar1=SOFTCAP, scalar2=None,
                                                       op0=ALU.mult), reads=[bg_], writes=[bil])
                tr.op("act", lambda e: e.activation(out=flog[:, :, :], in_=gts[:, :, 8:16], func=AF.Exp, scale=-SOFTCAP),
                      reads=[bg_], writes=[bfl])
                tr.op("act", lambda e: e.activation(out=flog[:, :, :], in_=flog[:, :, :], func=AF.Ln, bias=K.one[:, 0:1]),
                      reads=[bfl, b_const], writes=[bfl])
                tr.op("dve", lambda e: e.tensor_scalar(out=flog[:, :, :], in0=flog[:, :, :], scalar1=-1.0, scalar2=None,
                                                       op0=ALU.mult), reads=[bfl], writes=[bfl])
                ps = [ph.enter_context(nc.psum_tensor(f"xps{i}", [128, 512], F32)) for i in range(8)]
                bps = [Buf() for _ in range(8)]
                pB, pDl, pSc, pN0, pN1, pU0, pU1, pT = ps
                bB, bDl, bSc, bN0, bN1, bU0, bU1, bT = bps
                pTb = pT[:, :].bitcast(BF16)
                fl2 = flog[:, :, :].rearrange("p c h -> p (c h)")
                tr.op("pe", lambda e: e.matmul(pB[:, 0:NCH8], tri[:, :], fl2, start=True, stop=True), reads=[bc, bfl], writes=[bB])
                tr.op("pe", lambda e: e.matmul(pDl[:, 0:NCH8], K.ones_f[:, :], fl2, start=True, stop=True),
                      reads=[b_const, bfl], writes=[bDl])
                bcS, bwS, beBt = Buf(), Buf(), Buf()
                tr.op("dve", lambda e: e.tensor_tensor(out=cS[:, :, :].rearrange("p c h -> p (c h)"),
                                                       in0=ilog[:, :, :].rearrange("p c h -> p (c h)"), in1=pB[:, 0:NCH8],
                                                       op=ALU.subtract), reads=[bil, bB], writes=[bcS])
                tr.op("dve", lambda e: e.tensor_tensor(out=wS[:, :, :].rearrange("p c h -> p (c h)"),
                                                       in0=cS[:, :, :].rearrange("p c h -> p (c h)"), in1=pDl[:, 0:NCH8],
                                                       op=ALU.add), reads=[bcS, bDl], writes=[bwS])
                tr.op("act", lambda e: e.activation(out=wS[:, :, :], in_=wS[:, :, :], func=AF.Exp), reads=[bwS], writes=[bwS])
                tr.op("act", lambda e: e.activation(out=eBt[:, :, :].rearrange("p c h -> p (c h)"), in_=pDl[:, 0:NCH8], func=AF.Exp),
                      reads=[bDl], writes=[beBt])
                C = t("x_C", [128, ML_H, ML_DV], F32)
                Cb = t("x_Cb", [128, ML_H, ML_DV], BF16)
                nv = t("x_n", [128, ML_H], F32)
                nb = t("x_nb", [128, ML_H], BF16)
                bC = [Buf(), Buf()]
                bCb = [Buf(), Buf()]
                tr.op("pool", lambda e: e.memset(C[:, :, :], 0.0), writes=bC)
                tr.op("pool", lambda e: e.memset(Cb[:, :, :], 0.0), writes=bCb)
                tr.op("pool", lambda e: e.memset(nv[:, :], 0.0), writes=bC)
                tr.op("pool", lambda e: e.memset(nb[:, :], 0.0), writes=bCb)
                NB = 2
                qt2 = [t(f"x_q{i}", [128, ML_H, 256], BF16) for i in range(NB)]
                kt2 = [t(f"x_k{i}", [128, ML_H, 256], BF16) for i in range(NB)]
                bqk = [Buf() for _ in range(NB)]
                chqk = [tr.dma_chan(f"qk{i}") for i in range(NB)]
                km = [t(f"x_km{i}", [128, 1024], BF16) for i in range(NB)]
                vm = [t(f"x_vm{i}", [128, D], BF16) for i in range(NB)]
                om = [t(f"x_om{i}", [128, D], BF16) for i in range(NB)]
                bin_ = [Buf() for _ in range(NB)]
                chin = [tr.dma_chan(f"a{i}") for i in range(NB)]
                Ftri = [t(f"x_F{i}", [128, 4, 128], F32) for i in range(2)]
                R2 = [t(f"x_R{i}", [128, 4, 128], F32) for i in range(2)]
                eB = [t(f"x_eB{i}", [128, 4, 128], F32) for i in range(2)]
                dw = [t(f"x_dw{i}", [128, 4, 128], F32) for i in range(2)]
                PT = [t(f"x_PT{i}", [128, 4, 128], BF16) for i in range(2)]
                qd = [t(f"x_qd{i}", [128, 4, 128], BF16) for i in range(2)]
                kw = [t(f"x_kw{i}", [128, 4, 128], BF16) for i in range(2)]
                gso = [t(f"x_gso{i}", [128, 1024], F32) for i in range(2)]
                zz = [t(f"x_z{i}", [128, 1024], BF16) for i in range(2)]
                junk = t("x_junk", [128, 256], F32)
                sm = [t(f"x_sm{i}", [128, 16], F32) for i in range(2)]
                bF, bR, beB, bdw, bPT, bqd, bkw, bgso, bzz, bsm = ([Buf(), Buf()] for _ in range(10))
                bjunk = Buf()
                zT = t("x_zT", [128, 16, 512], BF16)
                bzT = Buf()
                chz = tr.dma_chan("o0")

                def load(c):
                    i = c % NB
                    sl = slice(c * 128, (c + 1) * 128)
                    if c % 2 == 0:
                        i2 = (c // 2) % NB
                        s2 = slice(c * 128, (c + 2) * 128)
                        tr.dma("sp", chqk[i2], [
                            (qt2[i2][:, :, :], qT_d[0:1024, s2].rearrange("(h p) t -> p h t", p=128)),
                            (kt2[i2][:, :, :], kT_d[0:1024, s2].rearrange("(h p) t -> p h t", p=128))],
                            reads=[DB["qT"], DB["kT"]], writes=[bqk[i2]])
                    tr.dma("sp", chin[i], [(km[i][:, :], ktm_d[sl, 0:1024]), (vm[i][:, :], vtm_d[sl, 0:D]), (om[i][:, :], gtm_d[sl, 0:D])],
                           reads=[DB["ktm"], DB["vtm"], DB["gtm"]], writes=[bin_[i]])

                if exch:
                    selw = t("x_selw", [128, 8], F32)
                    bsel = Buf()
                    tr.dma("sp", tr.dma_chan("ph2"), [(selw[:, :], selw_in)], writes=[bsel])
                    Bt = t("x_Bt", [128, NCH, 8], F32)
                    Sfx = t("x_Sfx", [128, NCH, 8], F32)
                    wF = t("x_wF", [128, NCH, 8], F32)
                    bBt, bSfx, bwF = Buf(), Buf(), Buf()
                    tr.op("dve", lambda e: e.tensor_copy(out=Bt[:, :, :].rearrange("p c h -> p (c h)"), in_=pDl[:, 0:NCH8]),
                          reads=[bDl], writes=[bBt])
                    tr.op("dve", lambda e: e.memset(Sfx[:, NCH - 1, :], 0.0), writes=[bSfx])
                    for c in range(NCH - 2, -1, -1):
                        tr.op("dve", lambda e: e.tensor_tensor(out=Sfx[:, c, :], in0=Sfx[:, c + 1, :], in1=Bt[:, c + 1, :], op=ALU.add),
                              reads=[bSfx, bBt], writes=[bSfx])
                    tr.op("dve", lambda e: e.tensor_tensor(out=wF[:, :, :], in0=cS[:, :, :], in1=Bt[:, :, :], op=ALU.add),
                          reads=[bcS, bBt], writes=[bwF])
                    tr.op("dve", lambda e: e.tensor_tensor(out=wF[:, :, :], in0=wF[:, :, :], in1=Sfx[:, :, :], op=ALU.add),
                          reads=[bwF, bSfx], writes=[bwF])
                    tr.op("act", lambda e: e.activation(out=wF[:, :, :], in_=wF[:, :, :], func=AF.Exp), reads=[bwF], writes=[bwF])
                    kwp = [t(f"x_kwp{i}", [128, 8, 128], BF16) for i in range(2)]
                    bkwp = [Buf(), Buf()]
                    accC = [(pN0, bN0), (pN1, bN1), (pU0, bU0), (pU1, bU1)]
                    for c in range(NCH):
                        i = c % NB
                        sl = slice(c * 128, (c + 1) * 128)
                        tr.dma("sp", chin[i], [(km[i][:, :], ktm_d[sl, 0:1024]), (vm[i][:, :], vtm_d[sl, 0:D])],
                               reads=[DB["ktm"], DB["vtm"]], writes=[bin_[i]])
                        kk = c % 2
                        tr.op("pool", lambda e: e.tensor_tensor(out=kwp[kk][:, :, :], in0=km[i][:, :].rearrange("p (h d) -> p h d", h=8),
                                                                in1=wF[:, c, :].unsqueeze(2).to_broadcast([128, 8, 128]), op=ALU.mult),
                              reads=[bin_[i], bwF], writes=[bkwp[kk]])
                        for h in range(8):
                            pa, bpa = accC[h // 2]
                            cs = (h % 2) * 256
                            tr.op("pe", lambda e: e.matmul(pa[:, cs:cs + 256], kwp[kk][:, h, :], vm[i][:, h * 256:(h + 1) * 256],
                                                           start=(c == 0), stop=(c == NCH - 1)),
                                  reads=[bkwp[kk], bin_[i]], writes=[bpa], signal=False)
                            tr.op("pe", lambda e: e.matmul(pSc[:, h:h + 1], kwp[kk][:, h, :], onesb[:, 0:1],
                                                           start=(c == 0), stop=(c == NCH - 1)),
                                  reads=[bkwp[kk], bc], writes=[bSc], signal=(h == 7))
                    cho2 = [tr.dma_chan("o1"), tr.dma_chan("o2"), tr.dma_chan("o3")]
                    for b4, (pa, bpa) in enumerate(accC):
                        g_ = gso[b4 // 2]
                        off = (b4 % 2) * 512
                        tr.op("act", lambda e: e.activation(out=g_[:, off:off + 512], in_=pa[:, :], func=AF.Copy),
                              reads=[bpa], writes=[bgso[b4 // 2]])
                    for k in range(2):
                        tr.dma("sp", cho2[k], [(Cx_d[k * 512:(k + 1) * 512, :].rearrange("(h p) e -> p h e", p=128),
                                                gso[k][:, :].rearrange("p (h e) -> p h e", h=4))], reads=[bgso[k]], writes=[DBX["Cx"]])
                    tr.op("dve", lambda e: e.tensor_copy(out=sm[0][:, 0:8], in_=pSc[:, 0:8]), reads=[bSc], writes=[bsm[0]])
                    tr.dma("sp", cho2[2], [(Cx_d[1024:1152, 0:8], sm[0][:, 0:8])], reads=[bsm[0]], writes=[DBX["Cx"]])
                    tr.collective(Cx_d, Gc_d, exch, reads=[DBX["Cx"]], writes=[DBX["Gc"]])
                    for r in range(exch):
                        for k in range(2):
                            tr.dma("sp", cho2[k], [(gso[k][:, :].rearrange("p (h e) -> p h e", h=4),
                                                    Gc_d[r * 1152 + k * 512:r * 1152 + (k + 1) * 512, :].rearrange("(h p) e -> p h e", p=128))],
                                   reads=[DBX["Gc"]], writes=[bgso[k]])
                            tr.op("dve", lambda e: e.scalar_tensor_tensor(out=C[:, k * 4:(k + 1) * 4, :].rearrange("p h e -> p (h e)"),
                                                                          in0=gso[k][:, :], scalar=selw[:, r:r + 1],
                                                                          in1=C[:, k * 4:(k + 1) * 4, :].rearrange("p h e -> p (h e)"),
                                                                          op0=ALU.mult, op1=ALU.add),
                                  reads=[bgso[k], bsel, bC[k]], writes=[bC[k]])
                        tr.dma("sp", cho2[2], [(sm[1][:, 0:8], Gc_d[r * 1152 + 1024:r * 1152 + 1152, 0:8])], reads=[DBX["Gc"]], writes=[bsm[1]])
                        tr.op("dve", lambda e: e.scalar_tensor_tensor(out=nv[:, :], in0=sm[1][:, 0:8], scalar=selw[:, r:r + 1], in1=nv[:, :],
                                                                      op0=ALU.mult, op1=ALU.add),
                              reads=[bsm[1], bsel] + bC, writes=bC)
                    for k in range(2):
                        tr.op("act", lambda e: e.activation(out=Cb[:, k * 4:(k + 1) * 4, :], in_=C[:, k * 4:(k + 1) * 4, :], func=AF.Copy),
                              reads=[bC[k]], writes=[bCb[k]])
                    tr.op("act", lambda e: e.activation(out=nb[:, :], in_=nv[:, :], func=AF.Copy), reads=bC, writes=bCb)
                load(0)
                it = 0
                for c in range(NCH):
                    i = c % NB
                    if c + 1 < NCH:
                        load(c + 1)
                    c4 = c % 4
                    i2 = (c // 2) % NB
                    co = (c % 2) * 128
                    for hh in range(2):
                        j = it % 2
                        it += 1
                        h0 = hh * 4
                        tr.op("dve", lambda e: e.tensor_tensor(out=Ftri[j][:, :, :], in0=tri[:, :].unsqueeze(1).to_broadcast([128, 4, 128]),
                                                               in1=flog[:, c, h0:h0 + 4].unsqueeze(2).to_broadcast([128, 4, 128]),
                                                               op=ALU.mult), reads=[bc, bfl], writes=[bF[j]])
                        tr.op("pool", lambda e: e.tensor_tensor(out=R2[j][:, :, :], in0=negm[:, :].unsqueeze(1).to_broadcast([128, 4, 128]),
                                                                in1=cS[:, c, h0:h0 + 4].unsqueeze(2).to_broadcast([128, 4, 128]),
                                                                op=ALU.add), reads=[bc, bcS], writes=[bR[j]])
                        F2 = Ftri[j][:, :, :].rearrange("p h n -> p (h n)")
                        R22 = R2[j][:, :, :].rearrange("p h n -> p (h n)")
                        tr.op("pe", lambda e: e.matmul(pB[:, :], K.ones_f[:, :], F2, start=True, stop=True),
                              reads=[b_const, bF[j]], writes=[bB])
                        tr.op("pe", lambda e: e.matmul(pDl[:, :], K.ones_f[:, :], F2, start=True, stop=False),
                              reads=[b_const, bF[j]], writes=[bDl], signal=False)
                        tr.op("pe", lambda e: e.matmul(pDl[:, :], K.ident_f[:, :], R22, start=False, stop=True),
                              reads=[b_const, bR[j]], writes=[bDl])
                        tr.op("act", lambda e: e.activation(out=eB[j][:, :, :].rearrange("p h n -> p (h n)"), in_=pB[:, :], func=AF.Exp),
                              reads=[bB], writes=[beB[j]])
                        tr.op("act", lambda e: e.activation(out=dw[j][:, :, :].rearrange("p h n -> p (h n)"), in_=pDl[:, :], func=AF.Exp),
                              reads=[bDl], writes=[bdw[j]])
                        for h in range(4):
                            tr.op("pe", lambda e: e.matmul(pSc[:, h * 128:(h + 1) * 128], kt2[i2][:, h0 + h, co:co + 128],
                                                           qt2[i2][:, h0 + h, co:co + 128], start=True, stop=True),
                                  reads=[bqk[i2]], writes=[bSc], signal=(h == 3))
                        tr.op("dve", lambda e: e.tensor_tensor(out=PT[j][:, :, :].rearrange("p h n -> p (h n)"), in0=pSc[:, :],
                                                               in1=dw[j][:, :, :].rearrange("p h n -> p (h n)"), op=ALU.mult),
                              reads=[bSc, bdw[j]], writes=[bPT[j]])
                        tr.op("pool", lambda e: e.tensor_tensor(out=qd[j][:, :, :], in0=qt2[i2][:, h0:h0 + 4, co:co + 128],
                                                                in1=eB[j][:, :, :], op=ALU.mult), reads=[bqk[i2], beB[j]], writes=[bqd[j]])
                        for h in range(4):
                            pn, bn_ = (pN0, bN0) if h < 2 else (pN1, bN1)
                            cs = (h % 2) * 256
                            tr.op("pe", lambda e: e.matmul(pn[:, cs:cs + 256], PT[j][:, h, :], vm[i][:, (h0 + h) * 256:(h0 + h + 1) * 256],
                                                           start=True, stop=False), reads=[bPT[j], bin_[i]], writes=[bn_], signal=False)
                            tr.op("pe", lambda e: e.matmul(pn[:, cs:cs + 256], qd[j][:, h, :], Cb[:, h0 + h, :], start=False, stop=True),
                                  reads=[bqd[j], bCb[hh]], writes=[bn_], signal=(h % 2 == 1))
                        for h in range(4):
                            tr.op("pe", lambda e: e.matmul(pDl[:, h:h + 1], PT[j][:, h, :], onesb[:, 0:1], start=True, stop=False),
                                  reads=[bPT[j], bc], writes=[bDl], signal=False)
                            tr.op("pe", lambda e: e.matmul(pDl[:, h:h + 1], qd[j][:, h, :], nb[:, h0 + h:h0 + h + 1], start=False, stop=True),
                                  reads=[bqd[j], bCb[hh]], writes=[bDl], signal=(h == 3))
                        s_ = sm[j]
                        tr.op("act", lambda e: e.activation(out=s_[:, 0:4], in_=pDl[:, 0:4], func=AF.Abs), reads=[bDl], writes=[bsm[j]])
                        tr.op("dve", lambda e: e.tensor_scalar(out=s_[:, 0:4], in0=s_[:, 0:4], scalar1=1.0, scalar2=None,
                                                               op0=ALU.max), reads=[bsm[j]], writes=[bsm[j]])
                        tr.op("dve", lambda e: e.reciprocal(out=s_[:, 4:8], in_=s_[:, 0:4]), reads=[bsm[j]], writes=[bsm[j]])
                        for h in range(4):
                            pn, bn_ = (pN0, bN0) if h < 2 else (pN1, bN1)
                            cs = (h % 2) * 256
                            tr.op("act", lambda e: e.activation(out=junk[:, :], in_=pn[:, cs:cs + 256], func=AF.Square,
                                                                scale=s_[:, 4 + h:5 + h], accum_out=s_[:, 8 + h:9 + h]),
                                  reads=[bn_, bsm[j]], writes=[bjunk, bsm[j]])
                        tr.op("act", lambda e: e.activation(out=s_[:, 8:12], in_=s_[:, 8:12], func=AF.Sqrt, scale=1.0 / ML_DV,
                                                            bias=K.eps[:, 0:1]), reads=[bsm[j], b_const], writes=[bsm[j]])
                        tr.op("dve", lambda e: e.reciprocal(out=s_[:, 8:12], in_=s_[:, 8:12]), reads=[bsm[j]], writes=[bsm[j]])
                        tr.op("dve", lambda e: e.tensor_tensor(out=s_[:, 12:16], in0=s_[:, 8:12], in1=s_[:, 4:8], op=ALU.mult),
                              reads=[bsm[j]], writes=[bsm[j]])
                        tr.op("pool", lambda e: e.tensor_tensor(out=gso[j][:, :], in0=om[i][:, h0 * 256:h0 * 256 + 1024],
                                                                in1=ngt[:, h0 * 256:h0 * 256 + 1024], op=ALU.mult),
                              reads=[bin_[i], bc], writes=[bgso[j]])
                        for h in range(4):
                            pn, bn_ = (pN0, bN0) if h < 2 else (pN1, bN1)
                            cs = (h % 2) * 256
                            tr.op("dve", lambda e: e.scalar_tensor_tensor(out=zz[j][:, h * 256:(h + 1) * 256], in0=pn[:, cs:cs + 256],
                                                                          scalar=s_[:, 12 + h:13 + h], in1=gso[j][:, h * 256:(h + 1) * 256],
                                                                          op0=ALU.mult, op1=ALU.mult),
                                  reads=[bn_, bsm[j], bgso[j]], writes=[bzz[j]])
                        for k8 in range(8):
                            tr.op("pe", lambda e: e.transpose(out=pTb[:, k8 * 128:(k8 + 1) * 128], in_=zz[j][:, k8 * 128:(k8 + 1) * 128],
                                                              identity=K.ident_b[:, :]), reads=[bzz[j], b_const], writes=[bT], signal=(k8 == 7))
                        tr.op("act", lambda e: e.activation(out=zT[:, h0 * 2:h0 * 2 + 8, c4 * 128:(c4 + 1) * 128],
                                                            in_=pTb.rearrange("p (k n) -> p k n", k=8), func=AF.Copy),
                              reads=[bT], writes=[bzT])
                        tr.op("pool", lambda e: e.tensor_tensor(out=kw[j][:, :, :],
                                                                in0=km[i][:, h0 * 128:(h0 + 4) * 128].rearrange("p (h d) -> p h d", h=4),
                                                                in1=wS[:, c, h0:h0 + 4].unsqueeze(2).to_broadcast([128, 4, 128]),
                                                                op=ALU.mult), reads=[bin_[i], bwS], writes=[bkw[j]])
                        for h in range(4):
                            pu, bu = (pU0, bU0) if h < 2 else (pU1, bU1)
                            cs = (h % 2) * 256
                            tr.op("pe", lambda e: e.matmul(pu[:, cs:cs + 256], kw[j][:, h, :], vm[i][:, (h0 + h) * 256:(h0 + h + 1) * 256],
                                                           start=True, stop=True), reads=[bkw[j], bin_[i]], writes=[bu], signal=(h % 2 == 1))
                        for h in range(4):
                            tr.op("pe", lambda e: e.matmul(pSc[:, h:h + 1], kw[j][:, h, :], onesb[:, 0:1], start=True, stop=True),
                                  reads=[bkw[j], bc], writes=[bSc], signal=(h == 3))
                        for h in range(4):
                            pu, bu = (pU0, bU0) if h < 2 else (pU1, bU1)
                            cs = (h % 2) * 256
                            tr.op("dve", lambda e: e.scalar_tensor_tensor(out=C[:, h0 + h, :], in0=C[:, h0 + h, :],
                                                                          scalar=eBt[:, c, h0 + h:h0 + h + 1], in1=pu[:, cs:cs + 256],
                                                                          op0=ALU.mult, op1=ALU.add),
                                  reads=[bu, beBt, bC[hh]], writes=[bC[hh]])
                        tr.op("dve", lambda e: e.tensor_tensor(out=nv[:, h0:h0 + 4], in0=nv[:, h0:h0 + 4], in1=eBt[:, c, h0:h0 + 4],
                                                               op=ALU.mult), reads=[beBt, bC[hh]], writes=[bC[hh]])
                        tr.op("dve", lambda e: e.tensor_tensor(out=nv[:, h0:h0 + 4], in0=nv[:, h0:h0 + 4], in1=pSc[:, 0:4], op=ALU.add),
                              reads=[bSc, bC[hh]], writes=[bC[hh]])
                        tr.op("act", lambda e: e.activation(out=Cb[:, h0:h0 + 4, :], in_=C[:, h0:h0 + 4, :], func=AF.Copy),
                              reads=[bC[hh]], writes=[bCb[hh]])
                        tr.op("act", lambda e: e.activation(out=nb[:, h0:h0 + 4], in_=nv[:, h0:h0 + 4], func=AF.Copy),
                              reads=[bC[hh]], writes=[bCb[hh]])
                    if c4 == 3 or c == NCH - 1:
                        nt = (c4 + 1) * 128
                        t0 = (c - c4) * 128
                        tr.dma("sp", chz, [(zT_d[0:D, t0:t0 + nt].rearrange("(b p) t -> p b t", p=128), zT[:, :, 0:nt])],
                               reads=[bzT], writes=[DB["zT"]])
                tr.barrier()

        def mlp1(g, hT, W):
            def ep(mode, env, a):
                if mode == "init":
                    ph, t = env, a
                    E = P()
                    E.tmp = [t(f"u_t{i}", [128, 512], F32) for i in range(4)]
                    E.bt = [Buf() for _ in range(4)]
                    E.o = [t(f"u_o{i}", [128, 512], BF16) for i in range(4)]
                    E.bo = [Buf() for _ in range(4)]
                    E.cho = [tr.dma_chan(f"o{i}") for i in range(4)]
                    E.i = 0
                    return E
                E = env
                pi, c0, tok0, ps0, bp0, ps1, bp1 = a
                for fb, (ps, bp) in enumerate(((ps0, bp0), (ps1, bp1))):
                    k = E.i % 4
                    E.i += 1
                    tr.op("act", lambda e: e.activation(out=E.tmp[k][:, :], in_=ps[:, :], func=AF.Relu), reads=[bp], writes=[E.bt[k]])
                    tr.op("pool", lambda e: e.tensor_tensor(out=E.o[k][:, :], in0=E.tmp[k][:, :], in1=E.tmp[k][:, :], op=ALU.mult),
                          reads=[E.bt[k]], writes=[E.bo[k]])
                    r0 = c0 + fb * 128
                    tr.dma("sp", E.cho[k], [(uT_d[r0:r0 + 128, tok0:tok0 + 512], E.o[k][:, :])], reads=[E.bo[k]], writes=[DB["uT"]])
            gemm_fm(g, hT, W, [i * 256 for i in range(DFF // 256)], ep, "mlp")

        phase_tables()
        class HT:
            def __enter__(self):
                self.cm = nc.sbuf_tensor("hT", [128, DC, TG], BF16)
                self.t = self.cm.__enter__()
                self.buf = Buf("hT")
                return self

            def __exit__(self, *a):
                return self.cm.__exit__(*a)

        def sc(name):
            return nc.named_scope(name)

        if True:
            src_x = xT_in
            for l in range(n_layers):
                last = (l == n_layers - 1)
                j = l // 2
                is_ret = (l % 2 == 0)
                for g in range(NG):
                    with HT() as hT:
                        with sc(f"L{l}g{g}_norm0"):
                            phase_norm(g, hT, src_x, None, None, l * 4 + 0, xT_d if l == 0 else None)
                        with sc(f"L{l}g{g}_inproj"):
                            if is_ret:
                                ret_inproj(g, hT, ret_w_in[j])
                            else:
                                ml_inproj(g, hT, ml_w_in[j])
                with sc(f"L{l}_mixer"):
                    if is_ret:
                        ret_mixer(None)
                    else:
                        ml_mixer(j)
                KCo = 32 if is_ret else 16
                Wo = ret_w_out[j] if is_ret else ml_w_out[j]
                src_x = xT_d
                for g in range(NG):
                    with sc(f"L{l}g{g}_outproj"):
                        gemm_stream(g, zT_d, DB["zT"], KCo, Wo, yT_d, DB["yT"])
                    with HT() as hT:
                        with sc(f"L{l}g{g}_an1"):
                            phase_norm(g, hT, xT_d, yT_d, l * 4 + 1, l * 4 + 2, xT_d)
                        with sc(f"L{l}g{g}_mlp1"):
                            mlp1(g, hT, w1_in[l])
                    with sc(f"L{l}g{g}_mlp2"):
                        gemm_stream(g, uT_d, DB["uT"], DFF // 128, w2_in[l], yT_d, DB["yT"])
                    with sc(f"L{l}g{g}_an2"):
                        if last:
                            phase_norm(g, None, xT_d, yT_d, l * 4 + 3, None, outT)
                        else:
                            phase_norm(g, None, xT_d, yT_d, l * 4 + 3, None, xT_d)
        tr.barrier()
        print("ninst", tr.ninst, "nwait", tr.nwait, flush=True)
    return nc_real


def make_in_maps(inputs, T, cores, exch=0):
    HC = host_consts(T)
    x = np.asarray(inputs["x"], dtype=np.float32)
    pos = np.asarray(inputs["positions"]).astype(np.int32)
    ng = np.asarray(inputs["norm_g"], dtype=np.float32)
    L = ng.shape[0]
    gcol = np.zeros((128, 16, DC), np.float32)
    gcol[:, :L * 4, :] = ng.reshape(L * 4, DC, 128).transpose(2, 0, 1)
    shared = {
        "gcol": gcol,
        "ret_w_in": np.ascontiguousarray(inputs["ret_w_in"], dtype=np.float32),
        "ret_w_out": np.ascontiguousarray(inputs["ret_w_out"], dtype=np.float32),
        "ml_w_in": np.ascontiguousarray(inputs["mlstm_w_in"], dtype=np.float32),
        "ml_bg": np.ascontiguousarray(np.broadcast_to(np.asarray(inputs["mlstm_b_gate"], np.float32)[None], (128, 2, 16))),
        "ml_ng": np.ascontiguousarray(np.broadcast_to(np.asarray(inputs["mlstm_norm_g"], np.float32)[None], (128, 2, D))),
        "ml_w_out": np.ascontiguousarray(inputs["mlstm_w_out"], dtype=np.float32),
        "mlp_w1": np.ascontiguousarray(inputs["mlp_w1"], dtype=np.float32),
        "mlp_w2": np.ascontiguousarray(inputs["mlp_w2"], dtype=np.float32),
    }
    for n in CONST_NAMES:
        shared["c_" + n] = HC[n]
    maps = []
    for (b, t0) in cores:
        m = dict(shared)
        m["xT"] = np.ascontiguousarray(x[b, t0:t0 + T, :].T)
        m["pos"] = np.ascontiguousarray(pos[b, t0:t0 + T].reshape(1, T))
        m["pos_tm"] = np.ascontiguousarray(pos[b, t0:t0 + T].reshape(T // 128, 128).T)
        if exch:
            sw = np.zeros((128, 8), np.float32)
            if t0 > 0:
                sw[:, cores.index((b, t0 - T))] = 1.0
            m["selw"] = sw
        maps.append(m)
    return maps


def kernel(**inputs):
    x = np.asarray(inputs["x"])
    B, S, _ = x.shape
    T = S
    cores = [(b, 0) for b in range(B)]
    nc = build(T, exch=0)
    in_maps = make_in_maps(inputs, T, cores, exch=0)
    res = run_bass_kernel_spmd(nc, in_maps, core_ids=list(range(len(cores))))
    out = np.zeros((B, S, D), np.float32)
    for i, (b, t0) in enumerate(cores):
        out[b, t0:t0 + T, :] = np.asarray(res.results[i]["outT"]).T
    return out
```

```python
import math
from contextlib import ExitStack
import numpy as np
import ml_dtypes
import concourse.bass as bass
import concourse.mybir as mybir
from concourse.bass_utils import run_bass_kernel_spmd

F32 = mybir.dt.float32
BF16 = mybir.dt.bfloat16
I32 = mybir.dt.int32
AF = mybir.ActivationFunctionType
ALU = mybir.AluOpType
AX = mybir.AxisListType

D = 2048
DC = 16
DFF = 8192
EPS = 1e-6
RET_H = 8
RET_DK = 256
RET_DV = 512
RET_IN = 12288
ML_H = 8
ML_DQK = 128
ML_DV = 256
ML_IN = 6160
SOFTCAP = 15.0
PI = math.pi


class Buf:
    __slots__ = ("name", "w", "r")

    def __init__(self, name=""):
        self.name = name
        self.w = None
        self.r = {}


class Chan:
    def __init__(self, name, sem, step):
        self.name = name
        self.sem = sem
        self.step = step
        self.cnt = 0


class Tracker:
    def __init__(self, nc, stack):
        self.nc = nc
        self.stack = stack
        self.engs = {"pe": nc.tensor, "act": nc.scalar, "dve": nc.vector,
                     "pool": nc.gpsimd, "sp": nc.sync}
        self.chan = {}
        for n in self.engs:
            sem = stack.enter_context(nc.semaphore("s_" + n))
            self.chan[n] = Chan(n, sem, 1)
        self.waited = {n: {} for n in self.engs}
        self.ninst = {n: 0 for n in self.engs}
        self.nwait = 0
        self.dma_pool = {}
        self.dram_bufs = set()

    def dma_chan(self, name):
        if name in self.dma_pool:
            return self.dma_pool[name]
        sem = self.stack.enter_context(self.nc.semaphore("d_" + name))
        c = Chan("d_" + name, sem, 16)
        self.chan[c.name] = c
        self.dma_pool[name] = c
        return c

    def _deps(self, eng, reads, writes):
        deps = {}

        def add(c, n):
            if deps.get(c, 0) < n:
                deps[c] = n
        for b in reads:
            if b.w is not None:
                add(*b.w)
        for b in writes:
            if b.w is not None and b.w[0] != eng:
                add(*b.w)
            for c, n in b.r.items():
                if c != eng:
                    add(c, n)
        return deps

    def _emit_waits(self, eng, deps):
        e = self.engs[eng]
        wd = self.waited[eng]
        for c, n in deps.items():
            if c == eng and eng == "pe":
                continue
            if self.chan[c].step == 16:
                n = max(n, self.chan[c].cnt)
            if wd.get(c, 0) >= n:
                continue
            e.wait_ge(self.chan[c].sem, n)
            wd[c] = n
            self.nwait += 1

    def op(self, eng, fn, reads=(), writes=(), signal=True):
        deps = self._deps(eng, reads, writes)
        self._emit_waits(eng, deps)
        ins = fn(self.engs[eng])
        ch = self.chan[eng]
        self.ninst[eng] += 1
        if signal:
            ch.cnt += 1
            ins.then_inc(ch.sem, 1)
            tag = (eng, ch.cnt)
        else:
            tag = (eng, ch.cnt + 1)
        for b in writes:
            b.w = tag
            b.r = {}
        for b in reads:
            if b.r.get(eng, 0) < tag[1]:
                b.r[eng] = tag[1]
        return ins

    def dma(self, q, ch, pairs, reads=(), writes=(), **kw):
        if q == "sp" and any(id(b) in self.dram_bufs for b in writes):
            q = "act"
        deps = self._deps("__dma__", reads, writes)
        self._emit_waits(q, deps)
        e = self.engs[q]
        for (o, i) in pairs:
            e.dma_start(out=o, in_=i, **kw).then_inc(ch.sem, 16)
            ch.cnt += 16
            self.ninst[q] += 1
        tag = (ch.name, ch.cnt)
        for b in writes:
            b.w = tag
            b.r = {}
        for b in reads:
            if b.r.get(ch.name, 0) < tag[1]:
                b.r[ch.name] = tag[1]

    def collective(self, src, dst, nranks, reads=(), writes=()):
        if "cc" not in self.chan:
            sem = self.stack.enter_context(self.nc.semaphore("cc_sem"))
            self.chan["cc"] = Chan("cc", sem, 1)
        ch = self.chan["cc"]
        deps = self._deps("__cc__", reads, writes)
        self._emit_waits("pool", deps)
        ins = self.nc.gpsimd.collective_compute("AllGather", ALU.bypass, replica_groups=[list(range(nranks))],
                                                ins=[src.opt()], outs=[dst.opt()])
        ins.then_inc(ch.sem)
        ch.cnt += 1
        self.ninst["pool"] += 1
        tag = ("cc", ch.cnt)
        for b in writes:
            b.w = tag
            b.r = {}
        for b in reads:
            if b.r.get("cc", 0) < tag[1]:
                b.r["cc"] = tag[1]

    def barrier(self):
        for eng in self.engs:
            deps = {}
            for c in self.chan.values():
                if c.cnt > 0 and c.name != eng:
                    deps[c.name] = c.cnt
            self._emit_waits(eng, deps)


def host_consts(T=2048):
    c = {}
    idx = np.arange(128)
    c["ones_f"] = np.ones((128, 128), np.float32)
    c["ident_f"] = np.eye(128, dtype=np.float32)
    c["ident_b"] = np.eye(128, dtype=np.float32).astype(ml_dtypes.bfloat16)
    c["tri_f"] = (idx[:, None] <= idx[None, :]).astype(np.float32)
    c["negm_f"] = np.where(idx[:, None] <= idx[None, :], 0.0, -30000.0).astype(np.float32)
    lg = np.log1p(-np.power(2.0, -5.0 - np.arange(RET_H, dtype=np.float64)))
    rel = (idx[None, :] - idx[:, None]).astype(np.float64)
    decT = np.where(rel[None] >= 0, np.exp(np.maximum(rel[None], 0) * lg[:, None, None]), 0.0)
    c["decayT"] = np.ascontiguousarray(decT.transpose(1, 0, 2)).astype(np.float32)
    xi = np.exp((idx[None, :] + 1.0) * lg[:, None])
    c["xirep"] = np.ascontiguousarray(np.broadcast_to(xi[None], (128, RET_H, 128))).astype(np.float32)
    zeta = np.exp((127.0 - idx[:, None]) * lg[None, :])
    c["zeta"] = zeta.astype(np.float32)
    c["gchunk"] = [float(np.exp(128.0 * v)) for v in lg]
    nch = T // 128
    tpos = (np.arange(nch)[None, :, None] * 128 + idx[:, None, None]).astype(np.float64)
    c["zetaF"] = np.exp((T - 1.0 - tpos) * lg[None, None, :]).astype(np.float32)
    invf = np.power(np.float32(10000.0), -np.linspace(0.0, 1.0, 128, dtype=np.float32)).astype(np.float32)
    c["invf_col"] = invf.reshape(128, 1).copy()
    c["invf_rep"] = np.ascontiguousarray(np.broadcast_to(invf[None, :], (128, 128))).astype(np.float32)
    return c


CONST_NAMES = ["ones_f", "ident_f", "ident_b", "tri_f", "negm_f", "decayT", "xirep", "zeta",
               "invf_col", "invf_rep", "zetaF"]


class P:
    pass


def build(T, n_layers=4, exch=0):
    HC = host_consts(T)
    TG = min(T, 2048)
    NG = T // TG
    NCH = T // 128
    nc_real = bass.Bass("TRN2", target_bir_lowering=False)

    class NCW:
        def __init__(self, n):
            self._n = n
            self._uid = 0

        def __getattr__(self, a):
            return getattr(self._n, a)

        def sbuf_tensor(self, name, shape, dtype):
            self._uid += 1
            return self._n.sbuf_tensor(f"{name}_u{self._uid}", shape, dtype)

        def psum_tensor(self, name, shape, dtype):
            self._uid += 1
            return self._n.psum_tensor(f"{name}_u{self._uid}", shape, dtype)

    nc = NCW(nc_real)
    dt = nc.dram_tensor

    def din(name, shape, dtype):
        return dt(name, list(shape), dtype, kind="ExternalInput").ap()

    def dscr(name, shape, dtype):
        return dt(name, list(shape), dtype, kind="Internal").ap()

    xT_in = din("xT", [D, T], F32)
    pos_in = din("pos", [1, T], I32)
    postm_in = din("pos_tm", [128, NCH], I32)
    gcol_in = din("gcol", [128, 16, DC], F32)
    ret_w_in = din("ret_w_in", [2, D, RET_IN], F32)
    ret_w_out = din("ret_w_out", [2, 2 * D, D], F32)
    ml_w_in = din("ml_w_in", [2, D, ML_IN], F32)
    ml_bg = din("ml_bg", [128, 2, 16], F32)
    ml_ng = din("ml_ng", [128, 2, D], F32)
    ml_w_out = din("ml_w_out", [2, D, D], F32)
    w1_in = din("mlp_w1", [4, D, DFF], F32)
    w2_in = din("mlp_w2", [4, DFF, D], F32)
    cin = {n: din("c_" + n, HC[n].shape, BF16 if n == "ident_b" else F32) for n in CONST_NAMES}
    outT = dt("outT", [D, T], F32, kind="ExternalOutput").ap()
    if exch:
        selw_in = din("selw", [128, 8], F32)
        Sx_d = dscr("Sx_d", [D, 512], F32)
        G_d = dscr("G_d", [exch * D, 512], F32)
        Cx_d = dscr("Cx_d", [1152, 256], F32)
        Gc_d = dscr("Gc_d", [exch * 1152, 256], F32)

    xT_d = dscr("xT_d", [D, T], F32)
    yT_d = dscr("yT_d", [D, T], F32)
    qT_d = dscr("qT_d", [D, T], BF16)
    kT_d = dscr("kT_d", [D, T], BF16)
    ktm_d = dscr("ktm_d", [T, D], BF16)
    vtm_d = dscr("vtm_d", [T, 2 * D], BF16)
    gtm_d = dscr("gtm_d", [T, 2 * D], BF16)
    zT_d = dscr("zT_d", [2 * D, T], BF16)
    uT_d = dscr("uT_d", [DFF, T], BF16)
    gate_d = dscr("gate_d", [T, 16], F32)
    cosT_d = dscr("cosT_d", [128, T], F32)
    sinT_d = dscr("sinT_d", [128, T], F32)
    costm_d = dscr("costm_d", [128, NCH, 128], F32)
    sintm_d = dscr("sintm_d", [128, NCH, 128], F32)

    st = ExitStack()
    with st:
        tr = Tracker(nc, st)
        sb = lambda name, shape, dtype: st.enter_context(nc.sbuf_tensor(name, list(shape), dtype))

        K = P()
        K.ones_f = sb("ones_f", [128, 128], F32)
        K.ident_f = sb("ident_f", [128, 128], F32)
        K.ident_b = sb("ident_b", [128, 128], BF16)
        K.ones_b = sb("ones_b", [128, 128], BF16)
        K.gcol = sb("gcol", [128, 16, DC], F32)
        K.eps = sb("eps_t", [128, 1], F32)
        K.negpi = sb("negpi_t", [128, 1], F32)
        K.one = sb("one_t", [128, 1], F32)
        b_const = Buf("const")
        cch = tr.dma_chan("const")
        tr.dma("sp", cch, [(K.ones_f[:, :], cin["ones_f"]), (K.ident_f[:, :], cin["ident_f"]),
                           (K.ident_b[:, :], cin["ident_b"]), (K.gcol[:, :, :], gcol_in)], writes=[b_const])
        tr.op("dve", lambda e: e.memset(K.eps[:, :], EPS), writes=[b_const])
        tr.op("dve", lambda e: e.memset(K.negpi[:, :], -PI), writes=[b_const])
        tr.op("dve", lambda e: e.memset(K.one[:, :], 1.0), writes=[b_const])
        tr.op("dve", lambda e: e.memset(K.ones_b[:, :], 1.0), writes=[b_const])
        tr.barrier()

        DB = {n: Buf(n) for n in ["xT", "yT", "qT", "kT", "ktm", "vtm", "gtm", "zT", "uT", "gate", "tab", "out"]}
        DBX = {n: Buf(n) for n in ["Sx", "G", "Cx", "Gc"]}
        tr.dram_bufs = set(id(b) for b in list(DB.values()) + list(DBX.values()))

        psum_names = [f"ps{i}" for i in range(8)]

        def phase_tables():
            with ExitStack() as ph:
                t = lambda name, shape, dtype: ph.enter_context(nc.sbuf_tensor(name, list(shape), dtype))
                ch = tr.dma_chan("ph0")
                cho = tr.dma_chan("ph1")
                invc = t("invc", [128, 1], F32)
                invr = t("invr", [128, 128], F32)
                b0 = Buf()
                tr.dma("sp", ch, [(invc[:, :], cin["invf_col"]), (invr[:, :], cin["invf_rep"])], writes=[b0])
                pi_ = t("tb_pi", [128, 512], I32)
                pf = t("tb_pf", [128, 512], F32)
                an = t("tb_an", [128, 512], F32)
                rs = t("tb_rs", [128, 512], F32)
                so = t("tb_so", [128, 512], F32)
                co = t("tb_co", [128, 512], F32)
                bpi, bpf, ban, brs, bso, bco = (Buf() for _ in range(6))
                ki = t("tb_ki", [128, 512], I32)
                mm_ = t("tb_m", [128, 512], F32)
                bki, bmm = Buf(), Buf()

                def wrap_clamp():
                    tr.op("dve", lambda e: e.tensor_scalar(out=mm_[:, :], in0=rs[:, :], scalar1=PI, scalar2=-2 * PI,
                                                           op0=ALU.is_gt, op1=ALU.mult), reads=[brs], writes=[bmm])
                    tr.op("dve", lambda e: e.tensor_tensor(out=rs[:, :], in0=rs[:, :], in1=mm_[:, :], op=ALU.add),
                          reads=[brs, bmm], writes=[brs])
                    tr.op("dve", lambda e: e.tensor_scalar(out=rs[:, :], in0=rs[:, :], scalar1=-PI, scalar2=PI,
                                                           op0=ALU.max, op1=ALU.min), reads=[brs], writes=[brs])

                def sincos():
                    tr.op("dve", lambda e: e.tensor_scalar(out=mm_[:, :], in0=an[:, :], scalar1=1.0 / (2 * PI), scalar2=None,
                                                           op0=ALU.mult), reads=[ban], writes=[bmm])
                    tr.op("dve", lambda e: e.tensor_copy(out=ki[:, :], in_=mm_[:, :]), reads=[bmm], writes=[bki])
                    tr.op("dve", lambda e: e.tensor_copy(out=mm_[:, :], in_=ki[:, :]), reads=[bki], writes=[bmm])
                    tr.op("dve", lambda e: e.scalar_tensor_tensor(out=rs[:, :], in0=mm_[:, :], scalar=-2 * PI, in1=an[:, :],
                                                                  op0=ALU.mult, op1=ALU.add), reads=[bmm, ban], writes=[brs])
                    wrap_clamp()
                    tr.op("act", lambda e: e.activation(out=so[:, :], in_=rs[:, :], func=AF.Sin), reads=[brs], writes=[bso])
                    tr.op("dve", lambda e: e.tensor_scalar(out=rs[:, :], in0=rs[:, :], scalar1=0.5 * PI, scalar2=None,
                                                           op0=ALU.add), reads=[brs], writes=[brs])
                    wrap_clamp()
                    tr.op("act", lambda e: e.activation(out=co[:, :], in_=rs[:, :], func=AF.Sin), reads=[brs], writes=[bco])
                for i in range(T // 512):
                    sl = slice(i * 512, (i + 1) * 512)
                    tr.dma("sp", ch, [(pi_[:, :], pos_in[:, sl].partition_broadcast(128))], writes=[bpi])
                    tr.op("dve", lambda e: e.tensor_copy(out=pf[:, :], in_=pi_[:, :]), reads=[bpi], writes=[bpf])
                    tr.op("dve", lambda e: e.tensor_scalar(out=an[:, :], in0=pf[:, :], scalar1=invc[:, 0:1], scalar2=None,
                                                           op0=ALU.mult), reads=[bpf, b0], writes=[ban])
                    sincos()
                    tr.dma("sp", cho, [(sinT_d[:, sl], so[:, :])], reads=[bso], writes=[DB["tab"]])
                    tr.dma("sp", cho, [(cosT_d[:, sl], co[:, :])], reads=[bco], writes=[DB["tab"]])
                pti = t("tb_pti", [128, NCH], I32)
                ptf = t("tb_ptf", [128, NCH], F32)
                bpt = Buf()
                tr.dma("sp", ch, [(pti[:, :], postm_in)], writes=[bpt])
                tr.op("dve", lambda e: e.tensor_copy(out=ptf[:, :], in_=pti[:, :]), reads=[bpt], writes=[bpt])
                for i in range(NCH // 4):
                    for j in range(4):
                        c = i * 4 + j
                        tr.op("dve", lambda e: e.tensor_scalar(out=an[:, j * 128:(j + 1) * 128], in0=invr[:, :],
                                                               scalar1=ptf[:, c:c + 1], scalar2=None, op0=ALU.mult),
                              reads=[bpt, b0], writes=[ban])
                    sincos()
                    tr.dma("sp", cho, [(sintm_d[:, i * 4:(i + 1) * 4, :], so[:, :].rearrange("p (c f) -> p c f", c=4))],
                           reads=[bso], writes=[DB["tab"]])
                    tr.dma("sp", cho, [(costm_d[:, i * 4:(i + 1) * 4, :], co[:, :].rearrange("p (c f) -> p c f", c=4))],
                           reads=[bco], writes=[DB["tab"]])
                tr.barrier()

        def phase_norm(g, hT, src_x, y_src, gpost, gnext, dst_x):
            with ExitStack() as ph:
                t = lambda name, shape, dtype: ph.enter_context(nc.sbuf_tensor(name, list(shape), dtype))
                xs = t("n_xs", [128, DC, 512], F32)
                ys = t("n_ys", [128, DC, 512], F32) if y_src is not None else None
                sq = t("n_sq", [128, DC, 512], BF16)
                rstd = t("n_rstd", [128, 512], F32)
                tmp = [t(f"n_tmp{i}", [128, 512], F32) for i in range(2)]
                ps = ph.enter_context(nc.psum_tensor("n_ps", [128, 512], F32))
                bx, by, bsq, brs, bps = Buf("xs"), Buf("ys"), Buf("sq"), Buf("rstd"), Buf("nps")
                btmp = [Buf(), Buf()]
                chx, chy, cho = tr.dma_chan("ph0"), tr.dma_chan("ph1"), tr.dma_chan("ph2")
                b_h = hT.buf if hT is not None else None

                def stats(src, bsrc):
                    for q4 in range(4):
                        sl4 = slice(q4 * 4, (q4 + 1) * 4)
                        if q4 % 2 == 0:
                            tr.op("act", lambda e: e.activation(out=sq[:, sl4, :], in_=src[:, sl4, :], func=AF.Square),
                                  reads=[bsrc], writes=[bsq])
                        else:
                            tr.op("pool", lambda e: e.tensor_tensor(out=sq[:, sl4, :], in0=src[:, sl4, :], in1=src[:, sl4, :],
                                                                    op=ALU.mult), reads=[bsrc], writes=[bsq])
                    for dc in range(DC):
                        tr.op("pe", lambda e: e.matmul(ps[:, :], K.ones_b[:, :], sq[:, dc, :], start=(dc == 0),
                                                       stop=(dc == DC - 1)), reads=[bsq, b_const], writes=[bps],
                              signal=(dc == DC - 1))
                    tr.op("act", lambda e: e.activation(out=rstd[:, :], in_=ps[:, :], func=AF.Sqrt, scale=1.0 / D,
                                                        bias=K.eps[:, 0:1]), reads=[bps, b_const], writes=[brs])
                    tr.op("dve", lambda e: e.reciprocal(out=rstd[:, :], in_=rstd[:, :]), reads=[brs], writes=[brs])

                for tb in range(TG // 512):
                    sl = slice(g * TG + tb * 512, g * TG + (tb + 1) * 512)
                    tr.dma("sp", chx, [(xs[:, :, :], src_x[:, sl].rearrange("(dc p) t -> p dc t", p=128))],
                           reads=[DB["xT"]], writes=[bx])
                    if y_src is not None:
                        tr.dma("sp", chy, [(ys[:, :, :], y_src[:, sl].rearrange("(dc p) t -> p dc t", p=128))],
                               reads=[DB["yT"]], writes=[by])
                        stats(ys, by)
                        for dc in range(DC):
                            tt = dc % 2
                            tr.op("dve", lambda e: e.scalar_tensor_tensor(out=tmp[tt][:, :], in0=ys[:, dc, :],
                                                                          scalar=K.gcol[:, gpost, dc:dc + 1], in1=rstd[:, :],
                                                                          op0=ALU.mult, op1=ALU.mult),
                                  reads=[by, brs, b_const], writes=[btmp[tt]])
                            tr.op("pool", lambda e: e.tensor_tensor(out=xs[:, dc, :], in0=xs[:, dc, :], in1=tmp[tt][:, :],
                                                                    op=ALU.add), reads=[bx, btmp[tt]], writes=[bx])
                    if dst_x is not None:
                        tr.dma("sp", cho, [(dst_x[:, sl].rearrange("(dc p) t -> p dc t", p=128), xs[:, :, :])],
                               reads=[bx], writes=[DB["out"] if dst_x is outT else DB["xT"]])
                    if gnext is not None:
                        stats(xs, bx)
                        for dc in range(DC):
                            tr.op("dve", lambda e: e.scalar_tensor_tensor(out=hT.t[:, dc, tb * 512:(tb + 1) * 512],
                                                                          in0=xs[:, dc, :], scalar=K.gcol[:, gnext, dc:dc + 1],
                                                                          in1=rstd[:, :], op0=ALU.mult, op1=ALU.mult),
                                  reads=[bx, brs, b_const], writes=[b_h])
                tr.barrier()

        def load_w_panel(wt, bw, chw, W, c0, ncols, KC):
            Wv = W.rearrange("(kc p) n -> p kc n", p=128)
            pairs = []
            for k0 in range(0, KC, 16):
                k1 = min(KC, k0 + 16)
                pairs.append((wt[:, k0:k1, 0:ncols], Wv[:, k0:k1, c0:c0 + ncols]))
            tr.dma("pool", chw, pairs, writes=[bw])

        def gemm_fm(g, hT, W, panels, epilogue, tag):
            with ExitStack() as ph:
                t = lambda name, shape, dtype: ph.enter_context(nc.sbuf_tensor(name, list(shape), dtype))
                wt = [t(f"gw{i}", [128, DC, 512], BF16) for i in range(2)]
                bw = [Buf(), Buf()]
                chw = [tr.dma_chan("w0"), tr.dma_chan("w1")]
                ps = [ph.enter_context(nc.psum_tensor(f"gps{i}", [128, 512], F32)) for i in range(8)]
                bps = [Buf() for _ in range(8)]
                NTB = TG // 512
                env = epilogue("init", ph, t)
                assert len(panels) % 2 == 0 and all(panels[i + 1] == panels[i] + 256 for i in range(0, len(panels), 2))
                load_w_panel(wt[0], bw[0], chw[0], W, panels[0], 512, DC)
                for pi, c0 in enumerate(panels):
                    s = (pi // 2) % 2
                    po = (pi % 2) * 256
                    if pi % 2 == 0 and pi + 2 < len(panels):
                        load_w_panel(wt[1 - s], bw[1 - s], chw[1 - s], W, panels[pi + 2], 512, DC)
                    for tb in range(NTB):
                        for fb in range(2):
                            bank = (tb % 4) * 2 + fb
                            for kc in range(DC):
                                tr.op("pe", lambda e: e.matmul(ps[bank][:, :], wt[s][:, kc, po + fb * 128:po + (fb + 1) * 128],
                                                               hT.t[:, kc, tb * 512:(tb + 1) * 512], start=(kc == 0),
                                                               stop=(kc == DC - 1)),
                                      reads=[bw[s], hT.buf], writes=[bps[bank]], signal=(kc == DC - 1))
                        b0, b1 = (tb % 4) * 2, (tb % 4) * 2 + 1
                        epilogue("ep", env, (pi, c0, g * TG + tb * 512, ps[b0], bps[b0], ps[b1], bps[b1]))
                tr.barrier()

        def gemm_tm(g, hT, W, panels, epilogue):
            with ExitStack() as ph:
                t = lambda name, shape, dtype: ph.enter_context(nc.sbuf_tensor(name, list(shape), dtype))
                wt = [t(f"gw{i}", [128, DC, 512], BF16) for i in range(2)]
                bw = [Buf(), Buf()]
                chw = [tr.dma_chan("w0"), tr.dma_chan("w1")]
                ps = [ph.enter_context(nc.psum_tensor(f"gps{i}", [128, 512], F32)) for i in range(8)]
                bps = [Buf() for _ in range(8)]
                NTT = TG // 128
                env = epilogue("init", ph, t)
                load_w_panel(wt[0], bw[0], chw[0], W, panels[0][0], panels[0][1], DC)
                for pi, (c0, ncol) in enumerate(panels):
                    s = pi % 2
                    if pi + 1 < len(panels):
                        load_w_panel(wt[1 - s], bw[1 - s], chw[1 - s], W, panels[pi + 1][0], panels[pi + 1][1], DC)
                    for tt in range(NTT):
                        bank = tt % 8
                        for kc in range(DC):
                            tr.op("pe", lambda e: e.matmul(ps[bank][:, 0:ncol], hT.t[:, kc, tt * 128:(tt + 1) * 128],
                                                           wt[s][:, kc, 0:ncol], start=(kc == 0), stop=(kc == DC - 1)),
                                  reads=[bw[s], hT.buf], writes=[bps[bank]], signal=(kc == DC - 1))
                        epilogue("ep", env, (pi, c0, ncol, g * TG + tt * 128, ps[bank], bps[bank]))
                tr.barrier()

        def gemm_stream(g, aT_d, b_a, KC, W, y_d, b_y):
            with ExitStack() as ph:
                t = lambda name, shape, dtype: ph.enter_context(nc.sbuf_tensor(name, list(shape), dtype))
                NW = 2
                wt = [t(f"sw{i}", [128, KC, 512], BF16) for i in range(NW)]
                bw = [Buf() for _ in range(NW)]
                chw = [tr.dma_chan(f"w{i}") for i in range(NW)]
                NTH = 2 if TG >= 1024 else 1
                TH = TG // NTH
                NTB = TH // 512
                NA = 3
                KG = 8
                at = [t(f"sa{i}", [128, KG, TH], BF16) for i in range(NA)]
                ba = [Buf() for _ in range(NA)]
                cha = [tr.dma_chan(f"a{i}") for i in range(NA)]
                ot = [t(f"so{i}", [128, 512], F32) for i in range(4)]
                bo = [Buf() for _ in range(4)]
                cho = [tr.dma_chan(f"o{i}") for i in range(4)]
                ps = [ph.enter_context(nc.psum_tensor(f"gps{i}", [128, 512], F32)) for i in range(8)]
                bps = [Buf() for _ in range(8)]
                av = aT_d.rearrange("(kc p) t -> p kc t", p=128)
                npair = D // 512

                def load_pair(pp):
                    load_w_panel(wt[pp % 2], bw[pp % 2], chw[pp % 2], W, pp * 512, 512, KC)

                load_pair(0)
                ai = 0
                oi = 0
                for pp in range(npair):
                    if pp + 1 < npair:
                        load_pair(pp + 1)
                    for th in range(NTH):
                        t0 = g * TG + th * TH
                        for kg in range(KC // KG):
                            a = ai % NA
                            ai += 1
                            tr.dma("sp", cha[a], [(at[a][:, :, :], av[:, kg * KG:(kg + 1) * KG, t0:t0 + TH])], reads=[b_a], writes=[ba[a]])
                            for fb4 in range(4):
                                k = pp % 2
                                for tb in range(NTB):
                                    bank = fb4 * 2 + tb
                                    for kk in range(KG):
                                        kc = kg * KG + kk
                                        tr.op("pe", lambda e: e.matmul(ps[bank][:, :], wt[k][:, kc, fb4 * 128:(fb4 + 1) * 128],
                                                                       at[a][:, kk, tb * 512:(tb + 1) * 512], start=(kc == 0),
                                                                       stop=(kc == KC - 1)),
                                              reads=[bw[k], ba[a]], writes=[bps[bank]],
                                              signal=(kc == KC - 1 or (fb4 == 3 and tb == NTB - 1 and kk == KG - 1)))
                        for fb4 in range(4):
                            for tb in range(NTB):
                                bank = fb4 * 2 + tb
                                o = oi % 4
                                oi += 1
                                if o % 2 == 0:
                                    tr.op("act", lambda e: e.activation(out=ot[o][:, :], in_=ps[bank][:, :], func=AF.Copy),
                                          reads=[bps[bank]], writes=[bo[o]])
                                else:
                                    tr.op("dve", lambda e: e.tensor_copy(out=ot[o][:, :], in_=ps[bank][:, :]),
                                          reads=[bps[bank]], writes=[bo[o]])
                                r0 = pp * 512 + fb4 * 128
                                c0 = t0 + tb * 512
                                tr.dma("sp", cho[o], [(y_d[r0:r0 + 128, c0:c0 + 512], ot[o][:, :])], reads=[bo[o]], writes=[b_y])
                tr.barrier()

        def ret_inproj(g, hT, W):
            def ep_fm(mode, env, a):
                if mode == "init":
                    ph, t = env, a
                    E = P()
                    E.cos = t("r_cos", [128, TG], F32)
                    E.sin = t("r_sin", [128, TG], F32)
                    E.btab = Buf()
                    ch = tr.dma_chan("ph0")
                    tr.dma("sp", ch, [(E.cos[:, :], cosT_d[:, g * TG:(g + 1) * TG]), (E.sin[:, :], sinT_d[:, g * TG:(g + 1) * TG])],
                           reads=[DB["tab"]], writes=[E.btab])
                    E.tmp = [t(f"r_t{i}", [128, 512], F32) for i in range(4)]
                    E.bt = [Buf() for _ in range(4)]
                    E.o = [t(f"r_o{i}", [128, 512], BF16) for i in range(4)]
                    E.bo = [Buf() for _ in range(4)]
                    E.cho = [tr.dma_chan(f"o{i}") for i in range(4)]
                    E.i = 0
                    return E
                E = env
                pi, c0, tok0, ps0, bp0, ps1, bp1 = a
                isk = c0 >= D
                scl = (RET_DK ** -0.5) if isk else 1.0
                dst = kT_d if isk else qT_d
                bdst = DB["kT"] if isk else DB["qT"]
                r0 = c0 - D if isk else c0
                lt = slice(tok0 - g * TG, tok0 - g * TG + 512)
                cs, sn = E.cos[:, lt], E.sin[:, lt]
                A, B_, C, Dd = E.tmp
                bA, bB, bC, bD = E.bt
                o1, o2 = E.o[(E.i * 2) % 4], E.o[(E.i * 2 + 1) % 4]
                bo1, bo2 = E.bo[(E.i * 2) % 4], E.bo[(E.i * 2 + 1) % 4]
                c1, c2 = E.cho[(E.i * 2) % 4], E.cho[(E.i * 2 + 1) % 4]
                E.i += 1
                stt = lambda out, in0, in1: (lambda e: e.scalar_tensor_tensor(out=out, in0=in0, scalar=scl, in1=in1,
                                                                              op0=ALU.mult, op1=ALU.mult))
                tr.op("dve", stt(A[:, :], ps0[:, :], cs), reads=[bp0, E.btab], writes=[bA])
                tr.op("dve", stt(B_[:, :], ps1[:, :], sn), reads=[bp1, E.btab], writes=[bB])
                tr.op("dve", stt(C[:, :], ps1[:, :], cs), reads=[bp1, E.btab], writes=[bC])
                tr.op("dve", stt(Dd[:, :], ps0[:, :], sn), reads=[bp0, E.btab], writes=[bD])
                tr.op("pool", lambda e: e.tensor_tensor(out=o1[:, :], in0=A[:, :], in1=B_[:, :], op=ALU.subtract),
                      reads=[bA, bB], writes=[bo1])
                tr.op("pool", lambda e: e.tensor_tensor(out=o2[:, :], in0=C[:, :], in1=Dd[:, :], op=ALU.add),
                      reads=[bC, bD], writes=[bo2])
                tr.dma("sp", c1, [(dst[r0:r0 + 128, tok0:tok0 + 512], o1[:, :])], reads=[bo1], writes=[bdst])
                tr.dma("sp", c2, [(dst[r0 + 128:r0 + 256, tok0:tok0 + 512], o2[:, :])], reads=[bo2], writes=[bdst])

            gemm_fm(g, hT, W, [h * 256 for h in range(16)], ep_fm, "ret")

            def ep_tm(mode, env, a):
                if mode == "init":
                    ph, t = env, a
                    E = P()
                    NCG = TG // 128
                    E.cos = t("r_cos", [128, NCG, 128], F32)
                    E.sin = t("r_sin", [128, NCG, 128], F32)
                    E.btab = Buf()
                    ch = tr.dma_chan("ph0")
                    tr.dma("sp", ch, [(E.cos[:, :, :], costm_d[:, g * NCG:(g + 1) * NCG, :]),
                                      (E.sin[:, :, :], sintm_d[:, g * NCG:(g + 1) * NCG, :])],
                           reads=[DB["tab"]], writes=[E.btab])
                    E.tmp = [t(f"r_t{i}", [128, 2, 128], F32) for i in range(4)]
                    E.bt = [Buf() for _ in range(4)]
                    E.o = [t(f"r_o{i}", [128, 512], BF16) for i in range(4)]
                    E.bo = [Buf() for _ in range(4)]
                    E.cho = [tr.dma_chan(f"o{i}") for i in range(4)]
                    E.i = 0
                    return E
                E = env
                pi, c0, ncol, tok0, ps, bp = a
                o = E.o[E.i % 4]
                bo = E.bo[E.i % 4]
                co = E.cho[E.i % 4]
                par = E.i % 2
                E.i += 1
                if c0 < 2 * D:
                    lc = (tok0 - g * TG) // 128
                    scl = RET_DK ** -0.5
                    pv = ps[:, :].rearrange("p (h two f) -> p h two f", h=2, two=2)
                    ov = o[:, :].rearrange("p (h two f) -> p h two f", h=2, two=2)
                    cs = E.cos[:, lc, :].unsqueeze(1).to_broadcast([128, 2, 128])
                    sn = E.sin[:, lc, :].unsqueeze(1).to_broadcast([128, 2, 128])
                    A, B_, C, Dd = E.tmp
                    bA, bB, bC, bD = E.bt
                    stt = lambda out, in0, in1: (lambda e: e.scalar_tensor_tensor(out=out, in0=in0, scalar=scl, in1=in1,
                                                                                  op0=ALU.mult, op1=ALU.mult))
                    tr.op("dve", stt(A[:, :, :], pv[:, :, 0, :], cs), reads=[bp, E.btab], writes=[bA])
                    tr.op("dve", stt(B_[:, :, :], pv[:, :, 1, :], sn), reads=[bp, E.btab], writes=[bB])
                    tr.op("dve", stt(C[:, :, :], pv[:, :, 1, :], cs), reads=[bp, E.btab], writes=[bC])
                    tr.op("dve", stt(Dd[:, :, :], pv[:, :, 0, :], sn), reads=[bp, E.btab], writes=[bD])
                    tr.op("pool", lambda e: e.tensor_tensor(out=ov[:, :, 0, :], in0=A[:, :, :], in1=B_[:, :, :], op=ALU.subtract),
                          reads=[bA, bB], writes=[bo])
                    tr.op("pool", lambda e: e.tensor_tensor(out=ov[:, :, 1, :], in0=C[:, :, :], in1=Dd[:, :, :], op=ALU.add),
                          reads=[bC, bD], writes=[bo])
                    tr.dma("sp", co, [(ktm_d[tok0:tok0 + 128, c0 - D:c0 - D + 512], o[:, :])], reads=[bo], writes=[DB["ktm"]])
                elif c0 < 4 * D:
                    if par == 0:
                        tr.op("act", lambda e: e.activation(out=o[:, :], in_=ps[:, :], func=AF.Copy), reads=[bp], writes=[bo])
                    else:
                        tr.op("dve", lambda e: e.tensor_copy(out=o[:, :], in_=ps[:, :]), reads=[bp], writes=[bo])
                    tr.dma("sp", co, [(vtm_d[tok0:tok0 + 128, c0 - 2 * D:c0 - 2 * D + 512], o[:, :])], reads=[bo], writes=[DB["vtm"]])
                else:
                    tr.op("act", lambda e: e.activation(out=o[:, :], in_=ps[:, :], func=AF.Silu), reads=[bp], writes=[bo])
                    tr.dma("sp", co, [(gtm_d[tok0:tok0 + 128, c0 - 4 * D:c0 - 4 * D + 512], o[:, :])], reads=[bo], writes=[DB["gtm"]])

            gemm_tm(g, hT, W, [(D + i * 512, 512) for i in range(20)], ep_tm)

        def ret_mixer(state):
            with ExitStack() as ph:
                t = lambda name, shape, dtype: ph.enter_context(nc.sbuf_tensor(name, list(shape), dtype))
                NJ = 4
                decT = t("m_dec", [128, RET_H, 128], F32)
                xir = t("m_xi", [128, RET_H, 128], F32)
                zet = t("m_zeta", [128, RET_H], F32)
                bc = Buf()
                ch = tr.dma_chan("ph0")
                tr.dma("sp", ch, [(decT[:, :, :], cin["decayT"]), (xir[:, :, :], cin["xirep"]), (zet[:, :], cin["zeta"])], writes=[bc])
                S = t("m_S", [128, RET_H, 2, RET_DV], F32)
                Sb = t("m_Sb", [128, RET_H, 2, RET_DV], BF16)
                bS = [Buf() for _ in range(RET_H)]
                bSb = [Buf() for _ in range(RET_H)]
                tr.op("pool", lambda e: e.memset(S[:, :, :, :], 0.0), writes=bS)
                tr.op("pool", lambda e: e.memset(Sb[:, :, :, :], 0.0), writes=bSb)
                NB = 2
                qt2 = [t(f"m_q{i}", [128, RET_H, 2, 256], BF16) for i in range(NB)]
                kt2 = [t(f"m_k{i}", [128, RET_H, 2, 256], BF16) for i in range(NB)]
                bqk = [Buf() for _ in range(NB)]
                chqk = [tr.dma_chan(f"qk{i}") for i in range(NB)]
                km = [t(f"m_km{i}", [128, D], BF16) for i in range(NB)]
                vm = [t(f"m_v{i}", [128, 2 * D], BF16) for i in range(NB)]
                gm = [t(f"m_g{i}", [128, 2 * D], BF16) for i in range(NB)]
                bin_ = [Buf() for _ in range(NB)]
                chin = [tr.dma_chan(f"a{i}") for i in range(NB)]
                qx = [t(f"m_qx{i}", [128, 2, 128], BF16) for i in range(NJ)]
                bqx = [Buf() for _ in range(NJ)]
                kz = [t(f"m_kz{i}", [128, 256], BF16) for i in range(NJ)]
                bkz = [Buf() for _ in range(NJ)]
                sT = [t(f"m_sT{i}", [128, 128], BF16) for i in range(NJ)]
                bsT = [Buf() for _ in range(NJ)]
                yn = [t(f"m_yn{i}", [128, RET_DV], F32) for i in range(NJ)]
                byn = [Buf() for _ in range(NJ)]
                z = [t(f"m_z{i}", [128, RET_DV], BF16) for i in range(NJ)]
                bz = [Buf() for _ in range(NJ)]
                stats = [t(f"m_st{i}", [128, 6], F32) for i in range(NJ)]
                mv = [t(f"m_mv{i}", [128, 2], F32) for i in range(NJ)]
                rs = [t(f"m_rs{i}", [128, 2], F32) for i in range(NJ)]
                bst = [Buf() for _ in range(NJ)]
                zT = [t(f"m_zT{i}", [128, 32, 512], BF16) for i in range(1)]
                bzT = Buf()
                chz = tr.dma_chan("o0")
                ps_y = [ph.enter_context(nc.psum_tensor(f"mps_y{i}", [128, 512], F32)) for i in range(NJ)]
                ps_u = [ph.enter_context(nc.psum_tensor(f"mps_u{i}", [128, 512], F32)) for i in range(2)]
                ps_t_all = ph.enter_context(nc.psum_tensor("mps_t", [128, 2, 4, 128], BF16))
                ps_s_all = ph.enter_context(nc.psum_tensor("mps_s", [128, 2, 128], F32))
                ps_t = [ps_t_all[:, i, :, :] for i in range(2)]
                ps_s = [ps_s_all[:, i, :] for i in range(2)]
                bps_s, bps_y, bps_u, bps_t = ([Buf() for _ in range(NJ)] for _ in range(4))

                def load(c):
                    i = c % NB
                    sl = slice(c * 128, (c + 1) * 128)
                    if c % 2 == 0:
                        i2 = (c // 2) % NB
                        s2 = slice(c * 128, (c + 2) * 128)
                        tr.dma("sp", chqk[i2], [
                            (qt2[i2][:, :, :, :], qT_d[:, s2].rearrange("(h two p) t -> p h two t", two=2, p=128)),
                            (kt2[i2][:, :, :, :], kT_d[:, s2].rearrange("(h two p) t -> p h two t", two=2, p=128))],
                            reads=[DB["qT"], DB["kT"]], writes=[bqk[i2]])
                    tr.dma("sp", chin[i], [
                        (km[i][:, :], ktm_d[sl, :]), (vm[i][:, :], vtm_d[sl, :]), (gm[i][:, :], gtm_d[sl, :])],
                        reads=[DB["ktm"], DB["vtm"], DB["gtm"]], writes=[bin_[i]])

                if exch:
                    zF = t("m_zF", [128, NCH, RET_H], F32)
                    selw = t("m_selw", [128, 8], F32)
                    kzp = [t(f"m_kzp{i}", [128, 2, 256], BF16) for i in range(2)]
                    bkzp = [Buf(), Buf()]
                    bzf = Buf()
                    tr.dma("sp", tr.dma_chan("ph1"), [(zF[:, :, :], cin["zetaF"]), (selw[:, :], selw_in)], writes=[bzf])
                    accs = [(ps_y[0], bps_y[0]), (ps_y[1], bps_y[1]), (ps_u[0], bps_u[0]), (ps_u[1], bps_u[1])]
                    li = 0
                    cho2 = [tr.dma_chan("o1"), tr.dma_chan("o2")]
                    for hp in range(RET_H // 2):
                        for c in range(NCH):
                            i = li % NB
                            li += 1
                            sl = slice(c * 128, (c + 1) * 128)
                            tr.dma("sp", chin[i], [(km[i][:, 0:512], ktm_d[sl, hp * 512:(hp + 1) * 512]),
                                                   (vm[i][:, 0:1024], vtm_d[sl, hp * 1024:(hp + 1) * 1024])],
                                   reads=[DB["ktm"], DB["vtm"]], writes=[bin_[i]])
                            kk = c % 2
                            tr.op("pool", lambda e: e.tensor_tensor(out=kzp[kk][:, :, :],
                                                                    in0=km[i][:, 0:512].rearrange("p (h d) -> p h d", h=2),
                                                                    in1=zF[:, c, hp * 2:hp * 2 + 2].unsqueeze(2).to_broadcast([128, 2, 256]),
                                                                    op=ALU.mult), reads=[bin_[i], bzf], writes=[bkzp[kk]])
                            for hl in range(2):
                                for dcc in range(2):
                                    pa, bpa = accs[hl * 2 + dcc]
                                    tr.op("pe", lambda e: e.matmul(pa[:, :], kzp[kk][:, hl, dcc * 128:(dcc + 1) * 128],
                                                                   vm[i][:, hl * 512:(hl + 1) * 512], start=(c == 0), stop=(c == NCH - 1)),
                                          reads=[bkzp[kk], bin_[i]], writes=[bpa], signal=(c == NCH - 1 or (hl == 1 and dcc == 1)))
                        for hl in range(2):
                            for dcc in range(2):
                                pa, bpa = accs[hl * 2 + dcc]
                                k2 = (hl * 2 + dcc) % 2
                                tr.op("act", lambda e: e.activation(out=yn[k2][:, :], in_=pa[:, :], func=AF.Copy), reads=[bpa], writes=[byn[k2]])
                                r0 = ((hp * 2 + hl) * 2 + dcc) * 128
                                tr.dma("sp", cho2[k2], [(Sx_d[r0:r0 + 128, :], yn[k2][:, :])], reads=[byn[k2]], writes=[DBX["Sx"]])
                    tr.collective(Sx_d, G_d, exch, reads=[DBX["Sx"]], writes=[DBX["G"]])
                    Gv = G_d.rearrange("(r b p) e -> r b p e", r=exch, p=128)
                    li = 0
                    for r in range(exch):
                        for b16 in range(16):
                            k2 = li % 2
                            li += 1
                            h_, dcc = b16 // 2, b16 % 2
                            tr.dma("sp", cho2[k2], [(yn[k2][:, :], Gv[r, b16, :, :])], reads=[DBX["G"]], writes=[byn[k2]])
                            tr.op("dve", lambda e: e.scalar_tensor_tensor(out=S[:, h_, dcc, :], in0=yn[k2][:, :], scalar=selw[:, r:r + 1],
                                                                          in1=S[:, h_, dcc, :], op0=ALU.mult, op1=ALU.add),
                                  reads=[byn[k2], bzf, bS[h_]], writes=[bS[h_]])
                    for h_ in range(RET_H):
                        tr.op("act", lambda e: e.activation(out=Sb[:, h_, :, :], in_=S[:, h_, :, :], func=AF.Copy),
                              reads=[bS[h_]], writes=[bSb[h_]])
                iters = [(c, h) for c in range(NCH) for h in range(RET_H)]
                NIT = len(iters)

                def stage_a(n):
                    c, h = iters[n]
                    j, j2, i, i2, co = n % NJ, n % 2, c % NB, (c // 2) % NB, (c % 2) * 128
                    for dcc in range(2):
                        tr.op("pe", lambda e: e.matmul(ps_s[j2][:, :], kt2[i2][:, h, dcc, co:co + 128], qt2[i2][:, h, dcc, co:co + 128],
                                                       start=(dcc == 0), stop=(dcc == 1)),
                              reads=[bqk[i2]], writes=[bps_s[j2]], signal=(dcc == 1))
                    tr.op("dve", lambda e: e.tensor_tensor(out=sT[j][:, :], in0=ps_s[j2][:, :], in1=decT[:, h, :], op=ALU.mult),
                          reads=[bps_s[j2], bc], writes=[bsT[j]])
                    tr.op("pool", lambda e: e.tensor_tensor(out=qx[j][:, :, :], in0=qt2[i2][:, h, :, co:co + 128],
                                                            in1=xir[:, h, :].unsqueeze(1).to_broadcast([128, 2, 128]),
                                                            op=ALU.mult), reads=[bqk[i2], bc], writes=[bqx[j]])
                    tr.op("pool", lambda e: e.tensor_tensor(out=kz[j][:, :], in0=km[i][:, h * 256:(h + 1) * 256],
                                                            in1=zet[:, h:h + 1].to_broadcast([128, 256]), op=ALU.mult),
                          reads=[bin_[i], bc], writes=[bkz[j]])
                    tr.op("pe", lambda e: e.matmul(ps_y[j][:, :], sT[j][:, :], vm[i][:, h * 512:(h + 1) * 512],
                                                   start=True, stop=False),
                          reads=[bsT[j], bin_[i]], writes=[bps_y[j]], signal=False)
                    for dcc in range(2):
                        tr.op("pe", lambda e: e.matmul(ps_y[j][:, :], qx[j][:, dcc, :], Sb[:, h, dcc, :],
                                                       start=False, stop=(dcc == 1)),
                              reads=[bqx[j], bSb[h]], writes=[bps_y[j]], signal=(dcc == 1))
                    for dcc in range(2):
                        tr.op("pe", lambda e: e.matmul(ps_u[dcc][:, :], kz[j][:, dcc * 128:(dcc + 1) * 128],
                                                       vm[i][:, h * 512:(h + 1) * 512], start=True, stop=True),
                              reads=[bkz[j], bin_[i]], writes=[bps_u[dcc]])
                    for dcc in range(2):
                        tr.op("dve", lambda e: e.scalar_tensor_tensor(out=S[:, h, dcc, :], in0=S[:, h, dcc, :],
                                                                      scalar=HC["gchunk"][h], in1=ps_u[dcc][:, :],
                                                                      op0=ALU.mult, op1=ALU.add),
                              reads=[bps_u[dcc], bS[h]], writes=[bS[h]])
                    tr.op("act", lambda e: e.activation(out=Sb[:, h, :, :], in_=S[:, h, :, :], func=AF.Copy),
                          reads=[bS[h]], writes=[bSb[h]])

                def stage_b1(n):
                    j = n % NJ
                    tr.op("dve", lambda e: e.bn_stats(out=stats[j][:, :], in_=ps_y[j][:, :]), reads=[bps_y[j]], writes=[bst[j]])
                    tr.op("dve", lambda e: e.bn_aggr(out=mv[j][:, :], in_=stats[j][:, :]), reads=[bst[j]], writes=[bst[j]])
                    tr.op("act", lambda e: e.activation(out=rs[j][:, 0:1], in_=mv[j][:, 1:2], func=AF.Sqrt,
                                                        bias=K.eps[:, 0:1]), reads=[bst[j], b_const], writes=[bst[j]])

                def stage_b2(n):
                    c, h = iters[n]
                    j, i = n % NJ, c % NB
                    tr.op("dve", lambda e: e.reciprocal(out=rs[j][:, 0:1], in_=rs[j][:, 0:1]), reads=[bst[j]], writes=[bst[j]])
                    tr.op("dve", lambda e: e.scalar_tensor_tensor(out=rs[j][:, 1:2], in0=mv[j][:, 0:1], scalar=-1.0,
                                                                  in1=rs[j][:, 0:1], op0=ALU.mult, op1=ALU.mult),
                          reads=[bst[j]], writes=[bst[j]])
                    tr.op("act", lambda e: e.activation(out=yn[j][:, :], in_=ps_y[j][:, :], func=AF.Identity,
                                                        bias=rs[j][:, 1:2], scale=rs[j][:, 0:1]),
                          reads=[bps_y[j], bst[j]], writes=[byn[j]])
                    tr.op("pool", lambda e: e.tensor_tensor(out=z[j][:, :], in0=yn[j][:, :], in1=gm[i][:, h * 512:(h + 1) * 512],
                                                            op=ALU.mult), reads=[byn[j], bin_[i]], writes=[bz[j]])

                def stage_c(n):
                    c, h = iters[n]
                    j, j2, c4 = n % NJ, n % 2, c % 4
                    for q4 in range(4):
                        tr.op("pe", lambda e: e.transpose(out=ps_t[j2][:, q4, :], in_=z[j][:, q4 * 128:(q4 + 1) * 128],
                                                          identity=K.ident_b[:, :]),
                              reads=[bz[j], b_const], writes=[bps_t[j2]], signal=(q4 == 3))
                    tr.op("act", lambda e: e.activation(out=zT[0][:, h * 4:(h + 1) * 4, c4 * 128:(c4 + 1) * 128],
                                                        in_=ps_t[j2][:, :, :], func=AF.Copy),
                          reads=[bps_t[j2]], writes=[bzT])
                    if h == RET_H - 1 and (c4 == 3 or c == NCH - 1):
                        nt = (c4 + 1) * 128
                        t0 = (c - c4) * 128
                        tr.dma("sp", chz, [(zT_d[:, t0:t0 + nt].rearrange("(b p) t -> p b t", p=128), zT[0][:, :, 0:nt])],
                               reads=[bzT], writes=[DB["zT"]])

                load(0)
                for n in range(NIT + 3):
                    if n < NIT:
                        c, h = iters[n]
                        if h == 3 and c + 1 < NCH:
                            load(c + 1)
                        stage_a(n)
                    if 0 <= n - 1 < NIT:
                        stage_b1(n - 1)
                    if 0 <= n - 2 < NIT:
                        stage_b2(n - 2)
                    if 0 <= n - 3 < NIT:
                        stage_c(n - 3)
                tr.barrier()

        def ml_inproj(g, hT, W):
            def ep_fm(mode, env, a):
                if mode == "init":
                    ph, t = env, a
                    E = P()
                    E.o = [t(f"l_o{i}", [128, 512], BF16) for i in range(4)]
                    E.bo = [Buf() for _ in range(4)]
                    E.cho = [tr.dma_chan(f"o{i}") for i in range(4)]
                    E.i = 0
                    return E
                E = env
                pi, c0, tok0, ps0, bp0, ps1, bp1 = a
                isk = c0 >= 1024
                scl = (ML_DQK ** -0.5) if isk else 1.0
                dst = kT_d if isk else qT_d
                bdst = DB["kT"] if isk else DB["qT"]
                r0 = c0 - 1024 if isk else c0
                for fb, (ps, bp) in enumerate(((ps0, bp0), (ps1, bp1))):
                    k = E.i % 4
                    E.i += 1
                    if fb == 0:
                        tr.op("act", lambda e: e.activation(out=E.o[k][:, :], in_=ps[:, :], func=AF.Copy, scale=scl),
                              reads=[bp], writes=[E.bo[k]])
                    else:
                        tr.op("dve", lambda e: e.tensor_scalar(out=E.o[k][:, :], in0=ps[:, :], scalar1=scl, scalar2=None,
                                                               op0=ALU.mult), reads=[bp], writes=[E.bo[k]])
                    tr.dma("sp", E.cho[k], [(dst[r0 + fb * 128:r0 + fb * 128 + 128, tok0:tok0 + 512], E.o[k][:, :])],
                           reads=[E.bo[k]], writes=[bdst])

            gemm_fm(g, hT, W, [i * 256 for i in range(8)], ep_fm, "ml")

            def ep_tm(mode, env, a):
                if mode == "init":
                    ph, t = env, a
                    E = P()
                    E.o = [t(f"l_o{i}", [128, 512], BF16) for i in range(4)]
                    E.bo = [Buf() for _ in range(4)]
                    E.cho = [tr.dma_chan(f"o{i}") for i in range(4)]
                    E.og = [t(f"l_og{i}", [128, 16], F32) for i in range(2)]
                    E.bog = [Buf() for _ in range(2)]
                    E.chg = [tr.dma_chan(f"a{i}") for i in range(2)]
                    E.i = 0
                    return E
                E = env
                pi, c0, ncol, tok0, ps, bp = a
                k = E.i % 4
                E.i += 1
                o, bo, co = E.o[k], E.bo[k], E.cho[k]
                if c0 < 2048:
                    scl = ML_DQK ** -0.5
                    tr.op("act", lambda e: e.activation(out=o[:, :], in_=ps[:, :], func=AF.Copy, scale=scl), reads=[bp], writes=[bo])
                    tr.dma("sp", co, [(ktm_d[tok0:tok0 + 128, c0 - 1024:c0 - 1024 + 512], o[:, :])], reads=[bo], writes=[DB["ktm"]])
                elif c0 < 4096:
                    tr.op("dve", lambda e: e.tensor_copy(out=o[:, :], in_=ps[:, :]), reads=[bp], writes=[bo])
                    tr.dma("sp", co, [(vtm_d[tok0:tok0 + 128, c0 - 2048:c0 - 2048 + 512], o[:, :])], reads=[bo], writes=[DB["vtm"]])
                elif c0 < 6144:
                    tr.op("act", lambda e: e.activation(out=o[:, :], in_=ps[:, :], func=AF.Sigmoid), reads=[bp], writes=[bo])
                    tr.dma("sp", co, [(gtm_d[tok0:tok0 + 128, c0 - 4096:c0 - 4096 + 512], o[:, :])], reads=[bo], writes=[DB["gtm"]])
                else:
                    kk = E.i % 2
                    tr.op("dve", lambda e: e.tensor_copy(out=E.og[kk][:, :], in_=ps[:, 0:16]), reads=[bp], writes=[E.bog[kk]])
                    tr.dma("sp", E.chg[kk], [(gate_d[tok0:tok0 + 128, :], E.og[kk][:, :])], reads=[E.bog[kk]], writes=[DB["gate"]])

            gemm_tm(g, hT, W, [(1024 + i * 512, 512) for i in range(10)] + [(6144, 16)], ep_tm)

        def ml_mixer(jl):
            with ExitStack() as ph:
                t = lambda name, shape, dtype: ph.enter_context(nc.sbuf_tensor(name, list(shape), dtype))
                tri = t("x_tri", [128, 128], F32)
                negm = t("x_negm", [128, 128], F32)
                bgt = t("x_bg", [128, 16], F32)
                ngt = t("x_ng", [128, D], F32)
                onesb = t("x_1b", [128, 1], BF16)
                bc = Buf()
                ch = tr.dma_chan("ph0")
                tr.dma("sp", ch, [(tri[:, :], cin["tri_f"]), (negm[:, :], cin["negm_f"]), (bgt[:, :], ml_bg[:, jl, :]),
                                  (ngt[:, :], ml_ng[:, jl, :])], writes=[bc])
                tr.op("dve", lambda e: e.memset(onesb[:, :], 1.0), writes=[bc])
                NCH8 = NCH * 8
                gts = t("x_gts", [128, NCH, 16], F32)
                ilog = t("x_il", [128, NCH, 8], F32)
                flog = t("x_fl", [128, NCH, 8], F32)
                cS = t("x_cS", [128, NCH, 8], F32)
                wS = t("x_wS", [128, NCH, 8], F32)
                eBt = t("x_eBt", [128, NCH, 8], F32)
                bg_ = Buf()
                tr.dma("sp", tr.dma_chan("ph1"), [(gts[:, :, :], gate_d.rearrange("(c p) k -> p c k", p=128))], reads=[DB["gate"]], writes=[bg_])
                tr.op("dve", lambda e: e.tensor_tensor(out=gts[:, :, :], in0=gts[:, :, :],
                                                       in1=bgt[:, :].unsqueeze(1).to_broadcast([128, NCH, 16]), op=ALU.add),
                      reads=[bg_, bc], writes=[bg_])
                tr.op("act", lambda e: e.activation(out=gts[:, :, :], in_=gts[:, :, :], func=AF.Tanh, scale=1.0 / SOFTCAP),
                      reads=[bg_], writes=[bg_])
                bil, bfl = Buf(), Buf()
                tr.op("dve", lambda e: e.tensor_scalar(out=ilog[:, :, :], in0=gts[:, :, 0:8], scalar1=SOFTCAP, scalar2=None,
                                                       op0=ALU.mult), reads=[bg_], writes=[bil])
                tr.op("act", lambda e: e.activation(out=flog[:, :, :], in_=gts[:, :, 8:16], func=AF.Exp, scale=-SOFTCAP),
                      reads=[bg_], writes=[bfl])
                tr.op("act", lambda e: e.activation(out=flog[:, :, :], in_=flog[:, :, :], func=AF.Ln, bias=K.one[:, 0:1]),
                      reads=[bfl, b_const], writes=[bfl])
                tr.op("dve", lambda e: e.tensor_scalar(out=flog[:, :, :], in0=flog[:, :, :], scalar1=-1.0, scalar2=None,
                                                       op0=ALU.mult), reads=[bfl], writes=[bfl])
                ps = [ph.enter_context(nc.psum_tensor(f"xps{i}", [128, 512], F32)) for i in range(8)]
                bps = [Buf() for _ in range(8)]
                pB, pDl, pSc, pN0, pN1, pU0, pU1, pT = ps
                bB, bDl, bSc, bN0, bN1, bU0, bU1, bT = bps
                pTb = pT[:, :].bitcast(BF16)
                fl2 = flog[:, :, :].rearrange("p c h -> p (c h)")
                tr.op("pe", lambda e: e.matmul(pB[:, 0:NCH8], tri[:, :], fl2, start=True, stop=True), reads=[bc, bfl], writes=[bB])
                tr.op("pe", lambda e: e.matmul(pDl[:, 0:NCH8], K.ones_f[:, :], fl2, start=True, stop=True),
                      reads=[b_const, bfl], writes=[bDl])
                bcS, bwS, beBt = Buf(), Buf(), Buf()
                tr.op("dve", lambda e: e.tensor_tensor(out=cS[:, :, :].rearrange("p c h -> p (c h)"),
                                                       in0=ilog[:, :, :].rearrange("p c h -> p (c h)"), in1=pB[:, 0:NCH8],
                                                       op=ALU.subtract), reads=[bil, bB], writes=[bcS])
                tr.op("dve", lambda e: e.tensor_tensor(out=wS[:, :, :].rearrange("p c h -> p (c h)"),
                                                       in0=cS[:, :, :].rearrange("p c h -> p (c h)"), in1=pDl[:, 0:NCH8],
                                                       op=ALU.add), reads=[bcS, bDl], writes=[bwS])
                tr.op("act", lambda e: e.activation(out=wS[:, :, :], in_=wS[:, :, :], func=AF.Exp), reads=[bwS], writes=[bwS])
                tr.op("act", lambda e: e.activation(out=eBt[:, :, :].rearrange("p c h -> p (c h)"), in_=pDl[:, 0:NCH8], func=AF.Exp),
                      reads=[bDl], writes=[beBt])
                C = t("x_C", [128, ML_H, ML_DV], F32)
                Cb = t("x_Cb", [128, ML_H, ML_DV], BF16)
                nv = t("x_n", [128, ML_H], F32)
                nb = t("x_nb", [128, ML_H], BF16)
                bC = [Buf(), Buf()]
                bCb = [Buf(), Buf()]
                tr.op("pool", lambda e: e.memset(C[:, :, :], 0.0), writes=bC)
                tr.op("pool", lambda e: e.memset(Cb[:, :, :], 0.0), writes=bCb)
                tr.op("pool", lambda e: e.memset(nv[:, :], 0.0), writes=bC)
                tr.op("pool", lambda e: e.memset(nb[:, :], 0.0), writes=bCb)
                NB = 2
                qt2 = [t(f"x_q{i}", [128, ML_H, 256], BF16) for i in range(NB)]
                kt2 = [t(f"x_k{i}", [128, ML_H, 256], BF16) for i in range(NB)]
                bqk = [Buf() for _ in range(NB)]
                chqk = [tr.dma_chan(f"qk{i}") for i in range(NB)]
                km = [t(f"x_km{i}", [128, 1024], BF16) for i in range(NB)]
                vm = [t(f"x_vm{i}", [128, D], BF16) for i in range(NB)]
                om = [t(f"x_om{i}", [128, D], BF16) for i in range(NB)]
                bin_ = [Buf() for _ in range(NB)]
                chin = [tr.dma_chan(f"a{i}") for i in range(NB)]
                Ftri = [t(f"x_F{i}", [128, 4, 128], F32) for i in range(2)]
                R2 = [t(f"x_R{i}", [128, 4, 128], F32) for i in range(2)]
                eB = [t(f"x_eB{i}", [128, 4, 128], F32) for i in range(2)]
                dw = [t(f"x_dw{i}", [128, 4, 128], F32) for i in range(2)]
                PT = [t(f"x_PT{i}", [128, 4, 128], BF16) for i in range(2)]
                qd = [t(f"x_qd{i}", [128, 4, 128], BF16) for i in range(2)]
                kw = [t(f"x_kw{i}", [128, 4, 128], BF16) for i in range(2)]
                gso = [t(f"x_gso{i}", [128, 1024], F32) for i in range(2)]
                zz = [t(f"x_z{i}", [128, 1024], BF16) for i in range(2)]
                junk = t("x_junk", [128, 256], F32)
                sm = [t(f"x_sm{i}", [128, 16], F32) for i in range(2)]
                bF, bR, beB, bdw, bPT, bqd, bkw, bgso, bzz, bsm = ([Buf(), Buf()] for _ in range(10))
                bjunk = Buf()
                zT = t("x_zT", [128, 16, 512], BF16)
                bzT = Buf()
                chz = tr.dma_chan("o0")

                def load(c):
                    i = c % NB
                    sl = slice(c * 128, (c + 1) * 128)
                    if c % 2 == 0:
                        i2 = (c // 2) % NB
                        s2 = slice(c * 128, (c + 2) * 128)
                        tr.dma("sp", chqk[i2], [
                            (qt2[i2][:, :, :], qT_d[0:1024, s2].rearrange("(h p) t -> p h t", p=128)),
                            (kt2[i2][:, :, :], kT_d[0:1024, s2].rearrange("(h p) t -> p h t", p=128))],
                            reads=[DB["qT"], DB["kT"]], writes=[bqk[i2]])
                    tr.dma("sp", chin[i], [(km[i][:, :], ktm_d[sl, 0:1024]), (vm[i][:, :], vtm_d[sl, 0:D]), (om[i][:, :], gtm_d[sl, 0:D])],
                           reads=[DB["ktm"], DB["vtm"], DB["gtm"]], writes=[bin_[i]])

                if exch:
                    selw = t("x_selw", [128, 8], F32)
                    bsel = Buf()
                    tr.dma("sp", tr.dma_chan("ph2"), [(selw[:, :], selw_in)], writes=[bsel])
                    Bt = t("x_Bt", [128, NCH, 8], F32)
                    Sfx = t("x_Sfx", [128, NCH, 8], F32)
                    wF = t("x_wF", [128, NCH, 8], F32)
                    bBt, bSfx, bwF = Buf(), Buf(), Buf()
                    tr.op("dve", lambda e: e.tensor_copy(out=Bt[:, :, :].rearrange("p c h -> p (c h)"), in_=pDl[:, 0:NCH8]),
                          reads=[bDl], writes=[bBt])
                    tr.op("dve", lambda e: e.memset(Sfx[:, NCH - 1, :], 0.0), writes=[bSfx])
                    for c in range(NCH - 2, -1, -1):
                        tr.op("dve", lambda e: e.tensor_tensor(out=Sfx[:, c, :], in0=Sfx[:, c + 1, :], in1=Bt[:, c + 1, :], op=ALU.add),
                              reads=[bSfx, bBt], writes=[bSfx])
                    tr.op("dve", lambda e: e.tensor_tensor(out=wF[:, :, :], in0=cS[:, :, :], in1=Bt[:, :, :], op=ALU.add),
                          reads=[bcS, bBt], writes=[bwF])
                    tr.op("dve", lambda e: e.tensor_tensor(out=wF[:, :, :], in0=wF[:, :, :], in1=Sfx[:, :, :], op=ALU.add),
                          reads=[bwF, bSfx], writes=[bwF])
                    tr.op("act", lambda e: e.activation(out=wF[:, :, :], in_=wF[:, :, :], func=AF.Exp), reads=[bwF], writes=[bwF])
                    kwp = [t(f"x_kwp{i}", [128, 8, 128], BF16) for i in range(2)]
                    bkwp = [Buf(), Buf()]
                    accC = [(pN0, bN0), (pN1, bN1), (pU0, bU0), (pU1, bU1)]
                    for c in range(NCH):
                        i = c % NB
                        sl = slice(c * 128, (c + 1) * 128)
                        tr.dma("sp", chin[i], [(km[i][:, :], ktm_d[sl, 0:1024]), (vm[i][:, :], vtm_d[sl, 0:D])],
                               reads=[DB["ktm"], DB["vtm"]], writes=[bin_[i]])
                        kk = c % 2
                        tr.op("pool", lambda e: e.tensor_tensor(out=kwp[kk][:, :, :], in0=km[i][:, :].rearrange("p (h d) -> p h d", h=8),
                                                                in1=wF[:, c, :].unsqueeze(2).to_broadcast([128, 8, 128]), op=ALU.mult),
                              reads=[bin_[i], bwF], writes=[bkwp[kk]])
                        for h in range(8):
                            pa, bpa = accC[h // 2]
                            cs = (h % 2) * 256
                            tr.op("pe", lambda e: e.matmul(pa[:, cs:cs + 256], kwp[kk][:, h, :], vm[i][:, h * 256:(h + 1) * 256],
                                                           start=(c == 0), stop=(c == NCH - 1)),
                                  reads=[bkwp[kk], bin_[i]], writes=[bpa], signal=False)
                            tr.op("pe", lambda e: e.matmul(pSc[:, h:h + 1], kwp[kk][:, h, :], onesb[:, 0:1],
                                                           start=(c == 0), stop=(c == NCH - 1)),
                                  reads=[bkwp[kk], bc], writes=[bSc], signal=(h == 7))
                    cho2 = [tr.dma_chan("o1"), tr.dma_chan("o2"), tr.dma_chan("o3")]
                    for b4, (pa, bpa) in enumerate(accC):
                        g_ = gso[b4 // 2]
                        off = (b4 % 2) * 512
                        tr.op("act", lambda e: e.activation(out=g_[:, off:off + 512], in_=pa[:, :], func=AF.Copy),
                              reads=[bpa], writes=[bgso[b4 // 2]])
                    for k in range(2):
                        tr.dma("sp", cho2[k], [(Cx_d[k * 512:(k + 1) * 512, :].rearrange("(h p) e -> p h e", p=128),
                                                gso[k][:, :].rearrange("p (h e) -> p h e", h=4))], reads=[bgso[k]], writes=[DBX["Cx"]])
                    tr.op("dve", lambda e: e.tensor_copy(out=sm[0][:, 0:8], in_=pSc[:, 0:8]), reads=[bSc], writes=[bsm[0]])
                    tr.dma("sp", cho2[2], [(Cx_d[1024:1152, 0:8], sm[0][:, 0:8])], reads=[bsm[0]], writes=[DBX["Cx"]])
                    tr.collective(Cx_d, Gc_d, exch, reads=[DBX["Cx"]], writes=[DBX["Gc"]])
                    for r in range(exch):
                        for k in range(2):
                            tr.dma("sp", cho2[k], [(gso[k][:, :].rearrange("p (h e) -> p h e", h=4),
                                                    Gc_d[r * 1152 + k * 512:r * 1152 + (k + 1) * 512, :].rearrange("(h p) e -> p h e", p=128))],
                                   reads=[DBX["Gc"]], writes=[bgso[k]])
                            tr.op("dve", lambda e: e.scalar_tensor_tensor(out=C[:, k * 4:(k + 1) * 4, :].rearrange("p h e -> p (h e)"),
                                                                          in0=gso[k][:, :], scalar=selw[:, r:r + 1],
                                                                          in1=C[:, k * 4:(k + 1) * 4, :].rearrange("p h e -> p (h e)"),
                                                                          op0=ALU.mult, op1=ALU.add),
                                  reads=[bgso[k], bsel, bC[k]], writes=[bC[k]])
                        tr.dma("sp", cho2[2], [(sm[1][:, 0:8], Gc_d[r * 1152 + 1024:r * 1152 + 1152, 0:8])], reads=[DBX["Gc"]], writes=[bsm[1]])
                        tr.op("dve", lambda e: e.scalar_tensor_tensor(out=nv[:, :], in0=sm[1][:, 0:8], scalar=selw[:, r:r + 1], in1=nv[:, :],
                                                                      op0=ALU.mult, op1=ALU.add),
                              reads=[bsm[1], bsel] + bC, writes=bC)
                    for k in range(2):
                        tr.op("act", lambda e: e.activation(out=Cb[:, k * 4:(k + 1) * 4, :], in_=C[:, k * 4:(k + 1) * 4, :], func=AF.Copy),
                              reads=[bC[k]], writes=[bCb[k]])
                    tr.op("act", lambda e: e.activation(out=nb[:, :], in_=nv[:, :], func=AF.Copy), reads=bC, writes=bCb)
                load(0)
                it = 0
                for c in range(NCH):
                    i = c % NB
                    if c + 1 < NCH:
                        load(c + 1)
                    c4 = c % 4
                    i2 = (c // 2) % NB
                    co = (c % 2) * 128
                    for hh in range(2):
                        j = it % 2
                        it += 1
                        h0 = hh * 4
                        tr.op("dve", lambda e: e.tensor_tensor(out=Ftri[j][:, :, :], in0=tri[:, :].unsqueeze(1).to_broadcast([128, 4, 128]),
                                                               in1=flog[:, c, h0:h0 + 4].unsqueeze(2).to_broadcast([128, 4, 128]),
                                                               op=ALU.mult), reads=[bc, bfl], writes=[bF[j]])
                        tr.op("pool", lambda e: e.tensor_tensor(out=R2[j][:, :, :], in0=negm[:, :].unsqueeze(1).to_broadcast([128, 4, 128]),
                                                                in1=cS[:, c, h0:h0 + 4].unsqueeze(2).to_broadcast([128, 4, 128]),
                                                                op=ALU.add), reads=[bc, bcS], writes=[bR[j]])
                        F2 = Ftri[j][:, :, :].rearrange("p h n -> p (h n)")
                        R22 = R2[j][:, :, :].rearrange("p h n -> p (h n)")
                        tr.op("pe", lambda e: e.matmul(pB[:, :], K.ones_f[:, :], F2, start=True, stop=True),
                              reads=[b_const, bF[j]], writes=[bB])
                        tr.op("pe", lambda e: e.matmul(pDl[:, :], K.ones_f[:, :], F2, start=True, stop=False),
                              reads=[b_const, bF[j]], writes=[bDl], signal=False)
                        tr.op("pe", lambda e: e.matmul(pDl[:, :], K.ident_f[:, :], R22, start=False, stop=True),
                              reads=[b_const, bR[j]], writes=[bDl])
                        tr.op("act", lambda e: e.activation(out=eB[j][:, :, :].rearrange("p h n -> p (h n)"), in_=pB[:, :], func=AF.Exp),
                              reads=[bB], writes=[beB[j]])
                        tr.op("act", lambda e: e.activation(out=dw[j][:, :, :].rearrange("p h n -> p (h n)"), in_=pDl[:, :], func=AF.Exp),
                              reads=[bDl], writes=[bdw[j]])
                        for h in range(4):
                            tr.op("pe", lambda e: e.matmul(pSc[:, h * 128:(h + 1) * 128], kt2[i2][:, h0 + h, co:co + 128],
                                                           qt2[i2][:, h0 + h, co:co + 128], start=True, stop=True),
                                  reads=[bqk[i2]], writes=[bSc], signal=(h == 3))
                        tr.op("dve", lambda e: e.tensor_tensor(out=PT[j][:, :, :].rearrange("p h n -> p (h n)"), in0=pSc[:, :],
                                                               in1=dw[j][:, :, :].rearrange("p h n -> p (h n)"), op=ALU.mult),
                              reads=[bSc, bdw[j]], writes=[bPT[j]])
                        tr.op("pool", lambda e: e.tensor_tensor(out=qd[j][:, :, :], in0=qt2[i2][:, h0:h0 + 4, co:co + 128],
                                                                in1=eB[j][:, :, :], op=ALU.mult), reads=[bqk[i2], beB[j]], writes=[bqd[j]])
                        for h in range(4):
                            pn, bn_ = (pN0, bN0) if h < 2 else (pN1, bN1)
                            cs = (h % 2) * 256
                            tr.op("pe", lambda e: e.matmul(pn[:, cs:cs + 256], PT[j][:, h, :], vm[i][:, (h0 + h) * 256:(h0 + h + 1) * 256],
                                                           start=True, stop=False), reads=[bPT[j], bin_[i]], writes=[bn_], signal=False)
                            tr.op("pe", lambda e: e.matmul(pn[:, cs:cs + 256], qd[j][:, h, :], Cb[:, h0 + h, :], start=False, stop=True),
                                  reads=[bqd[j], bCb[hh]], writes=[bn_], signal=(h % 2 == 1))
                        for h in range(4):
                            tr.op("pe", lambda e: e.matmul(pDl[:, h:h + 1], PT[j][:, h, :], onesb[:, 0:1], start=True, stop=False),
                                  reads=[bPT[j], bc], writes=[bDl], signal=False)
                            tr.op("pe", lambda e: e.matmul(pDl[:, h:h + 1], qd[j][:, h, :], nb[:, h0 + h:h0 + h + 1], start=False, stop=True),
                                  reads=[bqd[j], bCb[hh]], writes=[bDl], signal=(h == 3))
                        s_ = sm[j]
                        tr.op("act", lambda e: e.activation(out=s_[:, 0:4], in_=pDl[:, 0:4], func=AF.Abs), reads=[bDl], writes=[bsm[j]])
                        tr.op("dve", lambda e: e.tensor_scalar(out=s_[:, 0:4], in0=s_[:, 0:4], scalar1=1.0, scalar2=None,
                                                               op0=ALU.max), reads=[bsm[j]], writes=[bsm[j]])
                        tr.op("dve", lambda e: e.reciprocal(out=s_[:, 4:8], in_=s_[:, 0:4]), reads=[bsm[j]], writes=[bsm[j]])
                        for h in range(4):
                            pn, bn_ = (pN0, bN0) if h < 2 else (pN1, bN1)
                            cs = (h % 2) * 256
                            tr.op("act", lambda e: e.activation(out=junk[:, :], in_=pn[:, cs:cs + 256], func=AF.Square,
                                                                scale=s_[:, 4 + h:5 + h], accum_out=s_[:, 8 + h:9 + h]),
                                  reads=[bn_, bsm[j]], writes=[bjunk, bsm[j]])
                        tr.op("act", lambda e: e.activation(out=s_[:, 8:12], in_=s_[:, 8:12], func=AF.Sqrt, scale=1.0 / ML_DV,
                                                            bias=K.eps[:, 0:1]), reads=[bsm[j], b_const], writes=[bsm[j]])
                        tr.op("dve", lambda e: e.reciprocal(out=s_[:, 8:12], in_=s_[:, 8:12]), reads=[bsm[j]], writes=[bsm[j]])
                        tr.op("dve", lambda e: e.tensor_tensor(out=s_[:, 12:16], in0=s_[:, 8:12], in1=s_[:, 4:8], op=ALU.mult),
                              reads=[bsm[j]], writes=[bsm[j]])
                        tr.op("pool", lambda e: e.tensor_tensor(out=gso[j][:, :], in0=om[i][:, h0 * 256:h0 * 256 + 1024],
                                                                in1=ngt[:, h0 * 256:h0 * 256 + 1024], op=ALU.mult),
                              reads=[bin_[i], bc], writes=[bgso[j]])
                        for h in range(4):
                            pn, bn_ = (pN0, bN0) if h < 2 else (pN1, bN1)
                            cs = (h % 2) * 256
                            tr.op("dve", lambda e: e.scalar_tensor_tensor(out=zz[j][:, h * 256:(h + 1) * 256], in0=pn[:, cs:cs + 256],
                                                                          scalar=s_[:, 12 + h:13 + h], in1=gso[j][:, h * 256:(h + 1) * 256],
                                                                          op0=ALU.mult, op1=ALU.mult),
                                  reads=[bn_, bsm[j], bgso[j]], writes=[bzz[j]])
                        for k8 in range(8):
                            tr.op("pe", lambda e: e.transpose(out=pTb[:, k8 * 128:(k8 + 1) * 128], in_=zz[j][:, k8 * 128:(k8 + 1) * 128],
                                                              identity=K.ident_b[:, :]), reads=[bzz[j], b_const], writes=[bT], signal=(k8 == 7))
                        tr.op("act", lambda e: e.activation(out=zT[:, h0 * 2:h0 * 2 + 8, c4 * 128:(c4 + 1) * 128],
                                                            in_=pTb.rearrange("p (k n) -> p k n", k=8), func=AF.Copy),
                              reads=[bT], writes=[bzT])
                        tr.op("pool", lambda e: e.tensor_tensor(out=kw[j][:, :, :],
                                                                in0=km[i][:, h0 * 128:(h0 + 4) * 128].rearrange("p (h d) -> p h d", h=4),
                                                                in1=wS[:, c, h0:h0 + 4].unsqueeze(2).to_broadcast([128, 4, 128]),
                                                                op=ALU.mult), reads=[bin_[i], bwS], writes=[bkw[j]])
                        for h in range(4):
                            pu, bu = (pU0, bU0) if h < 2 else (pU1, bU1)
                            cs = (h % 2) * 256
                            tr.op("pe", lambda e: e.matmul(pu[:, cs:cs + 256], kw[j][:, h, :], vm[i][:, (h0 + h) * 256:(h0 + h + 1) * 256],
                                                           start=True, stop=True), reads=[bkw[j], bin_[i]], writes=[bu], signal=(h % 2 == 1))
                        for h in range(4):
                            tr.op("pe", lambda e: e.matmul(pSc[:, h:h + 1], kw[j][:, h, :], onesb[:, 0:1], start=True, stop=True),
                                  reads=[bkw[j], bc], writes=[bSc], signal=(h == 3))
                        for h in range(4):
                            pu, bu = (pU0, bU0) if h < 2 else (pU1, bU1)
                            cs = (h % 2) * 256
                            tr.op("dve", lambda e: e.scalar_tensor_tensor(out=C[:, h0 + h, :], in0=C[:, h0 + h, :],
                                                                          scalar=eBt[:, c, h0 + h:h0 + h + 1], in1=pu[:, cs:cs + 256],
                                                                          op0=ALU.mult, op1=ALU.add),
                                  reads=[bu, beBt, bC[hh]], writes=[bC[hh]])
                        tr.op("dve", lambda e: e.tensor_tensor(out=nv[:, h0:h0 + 4], in0=nv[:, h0:h0 + 4], in1=eBt[:, c, h0:h0 + 4],
                                                               op=ALU.mult), reads=[beBt, bC[hh]], writes=[bC[hh]])
                        tr.op("dve", lambda e: e.tensor_tensor(out=nv[:, h0:h0 + 4], in0=nv[:, h0:h0 + 4], in1=pSc[:, 0:4], op=ALU.add),
                              reads=[bSc, bC[hh]], writes=[bC[hh]])
                        tr.op("act", lambda e: e.activation(out=Cb[:, h0:h0 + 4, :], in_=C[:, h0:h0 + 4, :], func=AF.Copy),
                              reads=[bC[hh]], writes=[bCb[hh]])
                        tr.op("act", lambda e: e.activation(out=nb[:, h0:h0 + 4], in_=nv[:, h0:h0 + 4], func=AF.Copy),
                              reads=[bC[hh]], writes=[bCb[hh]])
                    if c4 == 3 or c == NCH - 1:
                        nt = (c4 + 1) * 128
                        t0 = (c - c4) * 128
                        tr.dma("sp", chz, [(zT_d[0:D, t0:t0 + nt].rearrange("(b p) t -> p b t", p=128), zT[:, :, 0:nt])],
                               reads=[bzT], writes=[DB["zT"]])
                tr.barrier()

        def mlp1(g, hT, W):
            def ep(mode, env, a):
                if mode == "init":
                    ph, t = env, a
                    E = P()
                    E.tmp = [t(f"u_t{i}", [128, 512], F32) for i in range(4)]
                    E.bt = [Buf() for _ in range(4)]
                    E.o = [t(f"u_o{i}", [128, 512], BF16) for i in range(4)]
                    E.bo = [Buf() for _ in range(4)]
                    E.cho = [tr.dma_chan(f"o{i}") for i in range(4)]
                    E.i = 0
                    return E
                E = env
                pi, c0, tok0, ps0, bp0, ps1, bp1 = a
                for fb, (ps, bp) in enumerate(((ps0, bp0), (ps1, bp1))):
                    k = E.i % 4
                    E.i += 1
                    tr.op("act", lambda e: e.activation(out=E.tmp[k][:, :], in_=ps[:, :], func=AF.Relu), reads=[bp], writes=[E.bt[k]])
                    tr.op("pool", lambda e: e.tensor_tensor(out=E.o[k][:, :], in0=E.tmp[k][:, :], in1=E.tmp[k][:, :], op=ALU.mult),
                          reads=[E.bt[k]], writes=[E.bo[k]])
                    r0 = c0 + fb * 128
                    tr.dma("sp", E.cho[k], [(uT_d[r0:r0 + 128, tok0:tok0 + 512], E.o[k][:, :])], reads=[E.bo[k]], writes=[DB["uT"]])
            gemm_fm(g, hT, W, [i * 256 for i in range(DFF // 256)], ep, "mlp")

        phase_tables()
        class HT:
            def __enter__(self):
                self.cm = nc.sbuf_tensor("hT", [128, DC, TG], BF16)
                self.t = self.cm.__enter__()
                self.buf = Buf("hT")
                return self

            def __exit__(self, *a):
                return self.cm.__exit__(*a)

        def sc(name):
            return nc.named_scope(name)

        if True:
            src_x = xT_in
            for l in range(n_layers):
                last = (l == n_layers - 1)
                j = l // 2
                is_ret = (l % 2 == 0)
                for g in range(NG):
                    with HT() as hT:
                        with sc(f"L{l}g{g}_norm0"):
                            phase_norm(g, hT, src_x, None, None, l * 4 + 0, xT_d if l == 0 else None)
                        with sc(f"L{l}g{g}_inproj"):
                            if is_ret:
                                ret_inproj(g, hT, ret_w_in[j])
                            else:
                                ml_inproj(g, hT, ml_w_in[j])
                with sc(f"L{l}_mixer"):
                    if is_ret:
                        ret_mixer(None)
                    else:
                        ml_mixer(j)
                KCo = 32 if is_ret else 16
                Wo = ret_w_out[j] if is_ret else ml_w_out[j]
                src_x = xT_d
                for g in range(NG):
                    with sc(f"L{l}g{g}_outproj"):
                        gemm_stream(g, zT_d, DB["zT"], KCo, Wo, yT_d, DB["yT"])
                    with HT() as hT:
                        with sc(f"L{l}g{g}_an1"):
                            phase_norm(g, hT, xT_d, yT_d, l * 4 + 1, l * 4 + 2, xT_d)
                        with sc(f"L{l}g{g}_mlp1"):
                            mlp1(g, hT, w1_in[l])
                    with sc(f"L{l}g{g}_mlp2"):
                        gemm_stream(g, uT_d, DB["uT"], DFF // 128, w2_in[l], yT_d, DB["yT"])
                    with sc(f"L{l}g{g}_an2"):
                        if last:
                            phase_norm(g, None, xT_d, yT_d, l * 4 + 3, None, outT)
                        else:
                            phase_norm(g, None, xT_d, yT_d, l * 4 + 3, None, xT_d)
        tr.barrier()
        print("ninst", tr.ninst, "nwait", tr.nwait, flush=True)
    return nc_real


def make_in_maps(inputs, T, cores, exch=0):
    HC = host_consts(T)
    x = np.asarray(inputs["x"], dtype=np.float32)
    pos = np.asarray(inputs["positions"]).astype(np.int32)
    ng = np.asarray(inputs["norm_g"], dtype=np.float32)
    L = ng.shape[0]
    gcol = np.zeros((128, 16, DC), np.float32)
    gcol[:, :L * 4, :] = ng.reshape(L * 4, DC, 128).transpose(2, 0, 1)
    shared = {
        "gcol": gcol,
        "ret_w_in": np.ascontiguousarray(inputs["ret_w_in"], dtype=np.float32),
        "ret_w_out": np.ascontiguousarray(inputs["ret_w_out"], dtype=np.float32),
        "ml_w_in": np.ascontiguousarray(inputs["mlstm_w_in"], dtype=np.float32),
        "ml_bg": np.ascontiguousarray(np.broadcast_to(np.asarray(inputs["mlstm_b_gate"], np.float32)[None], (128, 2, 16))),
        "ml_ng": np.ascontiguousarray(np.broadcast_to(np.asarray(inputs["mlstm_norm_g"], np.float32)[None], (128, 2, D))),
        "ml_w_out": np.ascontiguousarray(inputs["mlstm_w_out"], dtype=np.float32),
        "mlp_w1": np.ascontiguousarray(inputs["mlp_w1"], dtype=np.float32),
        "mlp_w2": np.ascontiguousarray(inputs["mlp_w2"], dtype=np.float32),
    }
    for n in CONST_NAMES:
        shared["c_" + n] = HC[n]
    maps = []
    for (b, t0) in cores:
        m = dict(shared)
        m["xT"] = np.ascontiguousarray(x[b, t0:t0 + T, :].T)
        m["pos"] = np.ascontiguousarray(pos[b, t0:t0 + T].reshape(1, T))
        m["pos_tm"] = np.ascontiguousarray(pos[b, t0:t0 + T].reshape(T // 128, 128).T)
        if exch:
            sw = np.zeros((128, 8), np.float32)
            if t0 > 0:
                sw[:, cores.index((b, t0 - T))] = 1.0
            m["selw"] = sw
        maps.append(m)
    return maps


def kernel(**inputs):
    x = np.asarray(inputs["x"])
    B, S, _ = x.shape
    T = S
    cores = [(b, 0) for b in range(B)]
    nc = build(T, exch=0)
    in_maps = make_in_maps(inputs, T, cores, exch=0)
    res = run_bass_kernel_spmd(nc, in_maps, core_ids=list(range(len(cores))))
    out = np.zeros((B, S, D), np.float32)
    for i, (b, t0) in enumerate(cores):
        out[b, t0:t0 + T, :] = np.asarray(res.results[i]["outT"]).T
    return out
```

```python
import math
from contextlib import ExitStack
import numpy as np
import ml_dtypes
import concourse.bass as bass
import concourse.mybir as mybir
from concourse.bass_utils import run_bass_kernel_spmd

F32 = mybir.dt.float32
BF16 = mybir.dt.bfloat16
I32 = mybir.dt.int32
AF = mybir.ActivationFunctionType
ALU = mybir.AluOpType
AX = mybir.AxisListType

D = 2048
DC = 16
DFF = 8192
EPS = 1e-6
RET_H = 8
RET_DK = 256
RET_DV = 512
RET_IN = 12288
ML_H = 8
ML_DQK = 128
ML_DV = 256
ML_IN = 6160
SOFTCAP = 15.0
PI = math.pi


class Buf:
    __slots__ = ("name", "w", "r")

    def __init__(self, name=""):
        self.name = name
        self.w = None
        self.r = {}


class Chan:
    def __init__(self, name, sem, step):
        self.name = name
        self.sem = sem
        self.step = step
        self.cnt = 0


class Tracker:
    def __init__(self, nc, stack):
        self.nc = nc
        self.stack = stack
        self.engs = {"pe": nc.tensor, "act": nc.scalar, "dve": nc.vector,
                     "pool": nc.gpsimd, "sp": nc.sync}
        self.chan = {}
        for n in self.engs:
            sem = stack.enter_context(nc.semaphore("s_" + n))
            self.chan[n] = Chan(n, sem, 1)
        self.waited = {n: {} for n in self.engs}
        self.ninst = {n: 0 for n in self.engs}
        self.nwait = 0
        self.dma_pool = {}
        self.dram_bufs = set()

    def dma_chan(self, name):
        if name in self.dma_pool:
            return self.dma_pool[name]
        sem = self.stack.enter_context(self.nc.semaphore("d_" + name))
        c = Chan("d_" + name, sem, 16)
        self.chan[c.name] = c
        self.dma_pool[name] = c
        return c

    def _deps(self, eng, reads, writes):
        deps = {}

        def add(c, n):
            if deps.get(c, 0) < n:
                deps[c] = n
        for b in reads:
            if b.w is not None:
                add(*b.w)
        for b in writes:
            if b.w is not None and b.w[0] != eng:
                add(*b.w)
            for c, n in b.r.items():
                if c != eng:
                    add(c, n)
        return deps

    def _emit_waits(self, eng, deps):
        e = self.engs[eng]
        wd = self.waited[eng]
        for c, n in deps.items():
            if c == eng and eng == "pe":
                continue
            if self.chan[c].step == 16:
                n = max(n, self.chan[c].cnt)
            if wd.get(c, 0) >= n:
                continue
            e.wait_ge(self.chan[c].sem, n)
            wd[c] = n
            self.nwait += 1

    def op(self, eng, fn, reads=(), writes=(), signal=True):
        deps = self._deps(eng, reads, writes)
        self._emit_waits(eng, deps)
        ins = fn(self.engs[eng])
        ch = self.chan[eng]
        self.ninst[eng] += 1
        if signal:
            ch.cnt += 1
            ins.then_inc(ch.sem, 1)
            tag = (eng, ch.cnt)
        else:
            tag = (eng, ch.cnt + 1)
        for b in writes:
            b.w = tag
            b.r = {}
        for b in reads:
            if b.r.get(eng, 0) < tag[1]:
                b.r[eng] = tag[1]
        return ins

    def dma(self, q, ch, pairs, reads=(), writes=(), **kw):
        if q == "sp" and any(id(b) in self.dram_bufs for b in writes):
            q = "act"
        deps = self._deps("__dma__", reads, writes)
        if ch.cnt > 0:
            deps[ch.name] = max(deps.get(ch.name, 0), ch.cnt)
        self._emit_waits(q, deps)
        e = self.engs[q]
        for (o, i) in pairs:
            e.dma_start(out=o, in_=i, **kw).then_inc(ch.sem, 16)
            ch.cnt += 16
            self.ninst[q] += 1
        tag = (ch.name, ch.cnt)
        for b in writes:
            b.w = tag
            b.r = {}
        for b in reads:
            if b.r.get(ch.name, 0) < tag[1]:
                b.r[ch.name] = tag[1]

    def collective(self, src, dst, nranks, reads=(), writes=()):
        if "cc" not in self.chan:
            sem = self.stack.enter_context(self.nc.semaphore("cc_sem"))
            self.chan["cc"] = Chan("cc", sem, 1)
        ch = self.chan["cc"]
        deps = self._deps("__cc__", reads, writes)
        self._emit_waits("pool", deps)
        ins = self.nc.gpsimd.collective_compute("AllGather", ALU.bypass, replica_groups=[list(range(nranks))],
                                                ins=[src.opt()], outs=[dst.opt()])
        ins.then_inc(ch.sem)
        ch.cnt += 1
        self.ninst["pool"] += 1
        tag = ("cc", ch.cnt)
        for b in writes:
            b.w = tag
            b.r = {}
        for b in reads:
            if b.r.get("cc", 0) < tag[1]:
                b.r["cc"] = tag[1]

    def barrier(self):
        for eng in self.engs:
            deps = {}
            for c in self.chan.values():
                if c.cnt > 0 and c.name != eng:
                    deps[c.name] = c.cnt
            self._emit_waits(eng, deps)


def host_consts(T=2048):
    c = {}
    idx = np.arange(128)
    c["ones_f"] = np.ones((128, 128), np.float32)
    c["ident_f"] = np.eye(128, dtype=np.float32)
    c["ident_b"] = np.eye(128, dtype=np.float32).astype(ml_dtypes.bfloat16)
    c["tri_f"] = (idx[:, None] <= idx[None, :]).astype(np.float32)
    c["negm_f"] = np.where(idx[:, None] <= idx[None, :], 0.0, -30000.0).astype(np.float32)
    lg = np.log1p(-np.power(2.0, -5.0 - np.arange(RET_H, dtype=np.float64)))
    rel = (idx[None, :] - idx[:, None]).astype(np.float64)
    decT = np.where(rel[None] >= 0, np.exp(np.maximum(rel[None], 0) * lg[:, None, None]), 0.0)
    c["decayT"] = np.ascontiguousarray(decT.transpose(1, 0, 2)).astype(np.float32)
    xi = np.exp((idx[None, :] + 1.0) * lg[:, None])
    c["xirep"] = np.ascontiguousarray(np.broadcast_to(xi[None], (128, RET_H, 128))).astype(np.float32)
    zeta = np.exp((127.0 - idx[:, None]) * lg[None, :])
    c["zeta"] = zeta.astype(np.float32)
    c["gchunk"] = [float(np.exp(128.0 * v)) for v in lg]
    nch = T // 128
    tpos = (np.arange(nch)[None, :, None] * 128 + idx[:, None, None]).astype(np.float64)
    c["zetaF"] = np.exp((T - 1.0 - tpos) * lg[None, None, :]).astype(np.float32)
    invf = np.power(np.float32(10000.0), -np.linspace(0.0, 1.0, 128, dtype=np.float32)).astype(np.float32)
    c["invf_col"] = invf.reshape(128, 1).copy()
    c["invf_rep"] = np.ascontiguousarray(np.broadcast_to(invf[None, :], (128, 128))).astype(np.float32)
    return c


CONST_NAMES = ["ones_f", "ident_f", "ident_b", "tri_f", "negm_f", "decayT", "xirep", "zeta",
               "invf_col", "invf_rep", "zetaF"]


class P:
    pass


def build(T, n_layers=4, exch=0):
    HC = host_consts(T)
    TG = min(T, 2048)
    NG = T // TG
    NCH = T // 128
    nc_real = bass.Bass("TRN2", target_bir_lowering=False)

    class NCW:
        def __init__(self, n):
            self._n = n
            self._uid = 0

        def __getattr__(self, a):
            return getattr(self._n, a)

        def sbuf_tensor(self, name, shape, dtype):
            self._uid += 1
            return self._n.sbuf_tensor(f"{name}_u{self._uid}", shape, dtype)

        def psum_tensor(self, name, shape, dtype):
            self._uid += 1
            return self._n.psum_tensor(f"{name}_u{self._uid}", shape, dtype)

    nc = NCW(nc_real)
    dt = nc.dram_tensor

    def din(name, shape, dtype):
        return dt(name, list(shape), dtype, kind="ExternalInput").ap()

    def dscr(name, shape, dtype):
        return dt(name, list(shape), dtype, kind="Internal").ap()

    xT_in = din("xT", [D, T], F32)
    pos_in = din("pos", [1, T], I32)
    postm_in = din("pos_tm", [128, NCH], I32)
    gcol_in = din("gcol", [128, 16, DC], F32)
    ret_w_in = din("ret_w_in", [2, D, RET_IN], F32)
    ret_w_out = din("ret_w_out", [2, 2 * D, D], F32)
    ml_w_in = din("ml_w_in", [2, D, ML_IN], F32)
    ml_bg = din("ml_bg", [128, 2, 16], F32)
    ml_ng = din("ml_ng", [128, 2, D], F32)
    ml_w_out = din("ml_w_out", [2, D, D], F32)
    w1_in = din("mlp_w1", [4, D, DFF], F32)
    w2_in = din("mlp_w2", [4, DFF, D], F32)
    cin = {n: din("c_" + n, HC[n].shape, BF16 if n == "ident_b" else F32) for n in CONST_NAMES}
    outT = dt("outT", [D, T], F32, kind="ExternalOutput").ap()
    if exch:
        selw_in = din("selw", [128, 8], F32)
        Sx_d = dscr("Sx_d", [D, 512], F32)
        G_d = dscr("G_d", [exch * D, 512], F32)
        Cx_d = dscr("Cx_d", [1152, 256], F32)
        Gc_d = dscr("Gc_d", [exch * 1152, 256], F32)

    xT_d = dscr("xT_d", [D, T], F32)
    yT_d = dscr("yT_d", [D, T], F32)
    qT_d = dscr("qT_d", [D, T], BF16)
    kT_d = dscr("kT_d", [D, T], BF16)
    ktm_d = dscr("ktm_d", [T, D], BF16)
    vtm_d = dscr("vtm_d", [T, 2 * D], BF16)
    gtm_d = dscr("gtm_d", [T, 2 * D], BF16)
    zT_d = dscr("zT_d", [2 * D, T], BF16)
    uT_d = dscr("uT_d", [DFF, T], BF16)
    gate_d = dscr("gate_d", [T, 16], F32)
    cosT_d = dscr("cosT_d", [128, T], F32)
    sinT_d = dscr("sinT_d", [128, T], F32)
    costm_d = dscr("costm_d", [128, NCH, 128], F32)
    sintm_d = dscr("sintm_d", [128, NCH, 128], F32)

    st = ExitStack()
    with st:
        tr = Tracker(nc, st)
        sb = lambda name, shape, dtype: st.enter_context(nc.sbuf_tensor(name, list(shape), dtype))

        K = P()
        K.ones_f = sb("ones_f", [128, 128], F32)
        K.ident_f = sb("ident_f", [128, 128], F32)
        K.ident_b = sb("ident_b", [128, 128], BF16)
        K.ones_b = sb("ones_b", [128, 128], BF16)
        K.gcol = sb("gcol", [128, 16, DC], F32)
        K.eps = sb("eps_t", [128, 1], F32)
        K.negpi = sb("negpi_t", [128, 1], F32)
        K.one = sb("one_t", [128, 1], F32)
        b_const = Buf("const")
        cch = tr.dma_chan("const")
        tr.dma("sp", cch, [(K.ones_f[:, :], cin["ones_f"]), (K.ident_f[:, :], cin["ident_f"]),
                           (K.ident_b[:, :], cin["ident_b"]), (K.gcol[:, :, :], gcol_in)], writes=[b_const])
        tr.op("dve", lambda e: e.memset(K.eps[:, :], EPS), writes=[b_const])
        tr.op("dve", lambda e: e.memset(K.negpi[:, :], -PI), writes=[b_const])
        tr.op("dve", lambda e: e.memset(K.one[:, :], 1.0), writes=[b_const])
        tr.op("dve", lambda e: e.memset(K.ones_b[:, :], 1.0), writes=[b_const])
        tr.barrier()

        DB = {n: Buf(n) for n in ["xT", "yT", "qT", "kT", "ktm", "vtm", "gtm", "zT", "uT", "gate", "tab", "out"]}
        DBX = {n: Buf(n) for n in ["Sx", "G", "Cx", "Gc"]}
        tr.dram_bufs = set(id(b) for b in list(DB.values()) + list(DBX.values()))

        psum_names = [f"ps{i}" for i in range(8)]

        def phase_tables():
            with ExitStack() as ph:
                t = lambda name, shape, dtype: ph.enter_context(nc.sbuf_tensor(name, list(shape), dtype))
                ch = tr.dma_chan("ph0")
                cho = tr.dma_chan("ph1")
                invc = t("invc", [128, 1], F32)
                invr = t("invr", [128, 128], F32)
                b0 = Buf()
                tr.dma("sp", ch, [(invc[:, :], cin["invf_col"]), (invr[:, :], cin["invf_rep"])], writes=[b0])
                pi_ = t("tb_pi", [128, 512], I32)
                pf = t("tb_pf", [128, 512], F32)
                an = t("tb_an", [128, 512], F32)
                rs = t("tb_rs", [128, 512], F32)
                so = t("tb_so", [128, 512], F32)
                co = t("tb_co", [128, 512], F32)
                bpi, bpf, ban, brs, bso, bco = (Buf() for _ in range(6))
                ki = t("tb_ki", [128, 512], I32)
                mm_ = t("tb_m", [128, 512], F32)
                bki, bmm = Buf(), Buf()

                def wrap_clamp():
                    tr.op("dve", lambda e: e.tensor_scalar(out=mm_[:, :], in0=rs[:, :], scalar1=PI, scalar2=-2 * PI,
                                                           op0=ALU.is_gt, op1=ALU.mult), reads=[brs], writes=[bmm])
                    tr.op("dve", lambda e: e.tensor_tensor(out=rs[:, :], in0=rs[:, :], in1=mm_[:, :], op=ALU.add),
                          reads=[brs, bmm], writes=[brs])
                    tr.op("dve", lambda e: e.tensor_scalar(out=rs[:, :], in0=rs[:, :], scalar1=-PI, scalar2=PI,
                                                           op0=ALU.max, op1=ALU.min), reads=[brs], writes=[brs])

                def sincos():
                    tr.op("dve", lambda e: e.tensor_scalar(out=mm_[:, :], in0=an[:, :], scalar1=1.0 / (2 * PI), scalar2=None,
                                                           op0=ALU.mult), reads=[ban], writes=[bmm])
                    tr.op("dve", lambda e: e.tensor_copy(out=ki[:, :], in_=mm_[:, :]), reads=[bmm], writes=[bki])
                    tr.op("dve", lambda e: e.tensor_copy(out=mm_[:, :], in_=ki[:, :]), reads=[bki], writes=[bmm])
                    tr.op("dve", lambda e: e.scalar_tensor_tensor(out=rs[:, :], in0=mm_[:, :], scalar=-2 * PI, in1=an[:, :],
                                                                  op0=ALU.mult, op1=ALU.add), reads=[bmm, ban], writes=[brs])
                    wrap_clamp()
                    tr.op("act", lambda e: e.activation(out=so[:, :], in_=rs[:, :], func=AF.Sin), reads=[brs], writes=[bso])
                    tr.op("dve", lambda e: e.tensor_scalar(out=rs[:, :], in0=rs[:, :], scalar1=0.5 * PI, scalar2=None,
                                                           op0=ALU.add), reads=[brs], writes=[brs])
                    wrap_clamp()
                    tr.op("act", lambda e: e.activation(out=co[:, :], in_=rs[:, :], func=AF.Sin), reads=[brs], writes=[bco])
                for i in range(T // 512):
                    sl = slice(i * 512, (i + 1) * 512)
                    tr.dma("sp", ch, [(pi_[:, :], pos_in[:, sl].partition_broadcast(128))], writes=[bpi])
                    tr.op("dve", lambda e: e.tensor_copy(out=pf[:, :], in_=pi_[:, :]), reads=[bpi], writes=[bpf])
                    tr.op("dve", lambda e: e.tensor_scalar(out=an[:, :], in0=pf[:, :], scalar1=invc[:, 0:1], scalar2=None,
                                                           op0=ALU.mult), reads=[bpf, b0], writes=[ban])
                    sincos()
                    tr.dma("sp", cho, [(sinT_d[:, sl], so[:, :])], reads=[bso], writes=[DB["tab"]])
                    tr.dma("sp", cho, [(cosT_d[:, sl], co[:, :])], reads=[bco], writes=[DB["tab"]])
                pti = t("tb_pti", [128, NCH], I32)
                ptf = t("tb_ptf", [128, NCH], F32)
                bpt = Buf()
                tr.dma("sp", ch, [(pti[:, :], postm_in)], writes=[bpt])
                tr.op("dve", lambda e: e.tensor_copy(out=ptf[:, :], in_=pti[:, :]), reads=[bpt], writes=[bpt])
                for i in range(NCH // 4):
                    for j in range(4):
                        c = i * 4 + j
                        tr.op("dve", lambda e: e.tensor_scalar(out=an[:, j * 128:(j + 1) * 128], in0=invr[:, :],
                                                               scalar1=ptf[:, c:c + 1], scalar2=None, op0=ALU.mult),
                              reads=[bpt, b0], writes=[ban])
                    sincos()
                    tr.dma("sp", cho, [(sintm_d[:, i * 4:(i + 1) * 4, :], so[:, :].rearrange("p (c f) -> p c f", c=4))],
                           reads=[bso], writes=[DB["tab"]])
                    tr.dma("sp", cho, [(costm_d[:, i * 4:(i + 1) * 4, :], co[:, :].rearrange("p (c f) -> p c f", c=4))],
                           reads=[bco], writes=[DB["tab"]])
                tr.barrier()

        def phase_norm(g, hT, src_x, y_src, gpost, gnext, dst_x):
            with ExitStack() as ph:
                t = lambda name, shape, dtype: ph.enter_context(nc.sbuf_tensor(name, list(shape), dtype))
                BW = 256
                NBK = TG // BW
                NX = 4
                has_y = y_src is not None
                xs = [t(f"n_xs{i}", [128, DC, BW], F32) for i in range(NX)]
                bx = [Buf() for _ in range(NX)]
                chx = [tr.dma_chan(f"a{i}") for i in range(NX)]
                if has_y:
                    NY = 3
                    ys = [t(f"n_ys{i}", [128, DC, BW], F32) for i in range(NY)]
                    by = [Buf() for _ in range(NY)]
                    chy = [tr.dma_chan(f"y{i}") for i in range(NY)]
                    rsy = [t(f"n_rsy{i}", [128, BW], F32) for i in range(2)]
                    brsy = [Buf(), Buf()]
                    psy = [ph.enter_context(nc.psum_tensor(f"n_psy{i}", [128, BW], F32)) for i in range(2)]
                    bpsy = [Buf(), Buf()]
                    tmp = [t(f"n_tmp{i}", [128, BW], F32) for i in range(2)]
                    btmp = [Buf(), Buf()]
                sq = [t(f"n_sq{i}", [128, DC, BW], BF16) for i in range(2)]
                bsq = [Buf(), Buf()]
                rsx = [t(f"n_rsx{i}", [128, BW], F32) for i in range(2)]
                brsx = [Buf(), Buf()]
                psx = [ph.enter_context(nc.psum_tensor(f"n_psx{i}", [128, BW], F32)) for i in range(2)]
                bpsx = [Buf(), Buf()]
                cho = [tr.dma_chan(f"o{i}") for i in range(NX)]
                b_h = hT.buf if hT is not None else None

                def stats(src, bsrc, sqt, bsqt, ps, bps, rs, brs):
                    for q4 in range(4):
                        sl4 = slice(q4 * 4, (q4 + 1) * 4)
                        if q4 % 2 == 0:
                            tr.op("act", lambda e: e.activation(out=sqt[:, sl4, :], in_=src[:, sl4, :], func=AF.Square),
                                  reads=[bsrc], writes=[bsqt])
                        else:
                            tr.op("pool", lambda e: e.tensor_tensor(out=sqt[:, sl4, :], in0=src[:, sl4, :], in1=src[:, sl4, :],
                                                                    op=ALU.mult), reads=[bsrc], writes=[bsqt])
                    for dc in range(DC):
                        tr.op("pe", lambda e: e.matmul(ps[:, :], K.ones_b[:, :], sqt[:, dc, :], start=(dc == 0),
                                                       stop=(dc == DC - 1)), reads=[bsqt, b_const], writes=[bps],
                              signal=(dc == DC - 1))
                    tr.op("act", lambda e: e.activation(out=rs[:, :], in_=ps[:, :], func=AF.Sqrt, scale=1.0 / D,
                                                        bias=K.eps[:, 0:1]), reads=[bps, b_const], writes=[brs])

                def sl_of(n):
                    return slice(g * TG + n * BW, g * TG + (n + 1) * BW)

                def st_load(n):
                    k = n % NX
                    tr.dma("sp", chx[k], [(xs[k][:, :, :], src_x[:, sl_of(n)].rearrange("(dc p) t -> p dc t", p=128))],
                           reads=[DB["xT"]], writes=[bx[k]])
                    if has_y:
                        k3 = n % NY
                        tr.dma("sp", chy[k3], [(ys[k3][:, :, :], y_src[:, sl_of(n)].rearrange("(dc p) t -> p dc t", p=128))],
                               reads=[DB["yT"]], writes=[by[k3]])

                def st_s1(n):
                    k, k2 = n % NX, n % 2
                    if has_y:
                        k3 = n % NY
                        stats(ys[k3], by[k3], sq[k2], bsq[k2], psy[k2], bpsy[k2], rsy[k2], brsy[k2])
                    else:
                        if dst_x is not None:
                            tr.dma("sp", cho[k], [(dst_x[:, sl_of(n)].rearrange("(dc p) t -> p dc t", p=128), xs[k][:, :, :])],
                                   reads=[bx[k]], writes=[DB["out"] if dst_x is outT else DB["xT"]])
                        if gnext is not None:
                            stats(xs[k], bx[k], sq[k2], bsq[k2], psx[k2], bpsx[k2], rsx[k2], brsx[k2])

                def st_s2(n):
                    if not has_y:
                        return
                    k, k2 = n % NX, n % 2
                    tr.op("dve", lambda e: e.reciprocal(out=rsy[k2][:, :], in_=rsy[k2][:, :]), reads=[brsy[k2]], writes=[brsy[k2]])
                    for dc in range(DC):
                        tt = dc % 2
                        tr.op("dve", lambda e: e.scalar_tensor_tensor(out=tmp[tt][:, :], in0=ys[n % NY][:, dc, :],
                                                                      scalar=K.gcol[:, gpost, dc:dc + 1], in1=rsy[k2][:, :],
                                                                      op0=ALU.mult, op1=ALU.mult),
                              reads=[by[n % NY], brsy[k2], b_const], writes=[btmp[tt]])
                        tr.op("pool", lambda e: e.tensor_tensor(out=xs[k][:, dc, :], in0=xs[k][:, dc, :], in1=tmp[tt][:, :],
                                                                op=ALU.add), reads=[bx[k], btmp[tt]], writes=[bx[k]])
                    if dst_x is not None:
                        tr.dma("sp", cho[k], [(dst_x[:, sl_of(n)].rearrange("(dc p) t -> p dc t", p=128), xs[k][:, :, :])],
                               reads=[bx[k]], writes=[DB["out"] if dst_x is outT else DB["xT"]])
                    if gnext is not None:
                        stats(xs[k], bx[k], sq[k2], bsq[k2], psx[k2], bpsx[k2], rsx[k2], brsx[k2])

                def st_h(n):
                    if gnext is None:
                        return
                    k, k2 = n % NX, n % 2
                    tr.op("dve", lambda e: e.reciprocal(out=rsx[k2][:, :], in_=rsx[k2][:, :]), reads=[brsx[k2]], writes=[brsx[k2]])
                    for dc in range(DC):
                        tr.op("dve", lambda e: e.scalar_tensor_tensor(out=hT.t[:, dc, n * BW:(n + 1) * BW],
                                                                      in0=xs[k][:, dc, :], scalar=K.gcol[:, gnext, dc:dc + 1],
                                                                      in1=rsx[k2][:, :], op0=ALU.mult, op1=ALU.mult),
                              reads=[bx[k], brsx[k2], b_const], writes=[b_h])

                dS2 = 2 if has_y else None
                dH = 3 if has_y else 2
                for step in range(NBK + dH):
                    if step < NBK:
                        st_load(step)
                    if 0 <= step - 1 < NBK:
                        st_s1(step - 1)
                    if 0 <= step - dH < NBK:
                        st_h(step - dH)
                    if has_y and 0 <= step - dS2 < NBK:
                        st_s2(step - dS2)
                tr.barrier()

        def load_w_panel(wt, bw, chw, W, c0, ncols, KC):
            Wv = W.rearrange("(kc p) n -> p kc n", p=128)
            pairs = []
            for k0 in range(0, KC, 16):
                k1 = min(KC, k0 + 16)
                pairs.append((wt[:, k0:k1, 0:ncols], Wv[:, k0:k1, c0:c0 + ncols]))
            tr.dma("pool", chw, pairs, writes=[bw])

        def gemm_fm(g, hT, W, panels, epilogue, tag):
            with ExitStack() as ph:
                t = lambda name, shape, dtype: ph.enter_context(nc.sbuf_tensor(name, list(shape), dtype))
                wt = [t(f"gw{i}", [128, DC, 512], BF16) for i in range(2)]
                bw = [Buf(), Buf()]
                chw = [tr.dma_chan("w0"), tr.dma_chan("w1")]
                ps = [ph.enter_context(nc.psum_tensor(f"gps{i}", [128, 512], F32)) for i in range(8)]
                bps = [Buf() for _ in range(8)]
                NTB = TG // 512
                env = epilogue("init", ph, t)
                assert len(panels) % 2 == 0 and all(panels[i + 1] == panels[i] + 256 for i in range(0, len(panels), 2))
                load_w_panel(wt[0], bw[0], chw[0], W, panels[0], 512, DC)
                for pi, c0 in enumerate(panels):
                    s = (pi // 2) % 2
                    po = (pi % 2) * 256
                    if pi % 2 == 0 and pi + 2 < len(panels):
                        load_w_panel(wt[1 - s], bw[1 - s], chw[1 - s], W, panels[pi + 2], 512, DC)
                    for tb in range(NTB):
                        for fb in range(2):
                            bank = (tb % 4) * 2 + fb
                            for kc in range(DC):
                                tr.op("pe", lambda e: e.matmul(ps[bank][:, :], wt[s][:, kc, po + fb * 128:po + (fb + 1) * 128],
                                                               hT.t[:, kc, tb * 512:(tb + 1) * 512], start=(kc == 0),
                                                               stop=(kc == DC - 1)),
                                      reads=[bw[s], hT.buf], writes=[bps[bank]], signal=(kc == DC - 1))
                        b0, b1 = (tb % 4) * 2, (tb % 4) * 2 + 1
                        epilogue("ep", env, (pi, c0, g * TG + tb * 512, ps[b0], bps[b0], ps[b1], bps[b1]))
                tr.barrier()

        def gemm_tm(g, hT, W, panels, epilogue):
            with ExitStack() as ph:
                t = lambda name, shape, dtype: ph.enter_context(nc.sbuf_tensor(name, list(shape), dtype))
                wt = [t(f"gw{i}", [128, DC, 512], BF16) for i in range(2)]
                bw = [Buf(), Buf()]
                chw = [tr.dma_chan("w0"), tr.dma_chan("w1")]
                ps = [ph.enter_context(nc.psum_tensor(f"gps{i}", [128, 512], F32)) for i in range(8)]
                bps = [Buf() for _ in range(8)]
                NTT = TG // 128
                env = epilogue("init", ph, t)
                load_w_panel(wt[0], bw[0], chw[0], W, panels[0][0], panels[0][1], DC)
                for pi, (c0, ncol) in enumerate(panels):
                    s = pi % 2
                    if pi + 1 < len(panels):
                        load_w_panel(wt[1 - s], bw[1 - s], chw[1 - s], W, panels[pi + 1][0], panels[pi + 1][1], DC)
                    for tt in range(NTT):
                        bank = tt % 8
                        for kc in range(DC):
                            tr.op("pe", lambda e: e.matmul(ps[bank][:, 0:ncol], hT.t[:, kc, tt * 128:(tt + 1) * 128],
                                                           wt[s][:, kc, 0:ncol], start=(kc == 0), stop=(kc == DC - 1)),
                                  reads=[bw[s], hT.buf], writes=[bps[bank]], signal=(kc == DC - 1))
                        epilogue("ep", env, (pi, c0, ncol, g * TG + tt * 128, ps[bank], bps[bank]))
                tr.barrier()

        def gemm_stream(g, aT_d, b_a, KC, W, y_d, b_y):
            with ExitStack() as ph:
                t = lambda name, shape, dtype: ph.enter_context(nc.sbuf_tensor(name, list(shape), dtype))
                NW = 2
                wt = [t(f"sw{i}", [128, KC, 512], BF16) for i in range(NW)]
                bw = [Buf() for _ in range(NW)]
                chw = [tr.dma_chan(f"w{i}") for i in range(NW)]
                NTH = 2 if TG >= 1024 else 1
                TH = TG // NTH
                NTB = TH // 512
                NA = 3
                KG = 8
                at = [t(f"sa{i}", [128, KG, TH], BF16) for i in range(NA)]
                ba = [Buf() for _ in range(NA)]
                cha = [tr.dma_chan(f"a{i}") for i in range(NA)]
                ot = [t(f"so{i}", [128, 512], F32) for i in range(4)]
                bo = [Buf() for _ in range(4)]
                cho = [tr.dma_chan(f"o{i}") for i in range(4)]
                ps = [ph.enter_context(nc.psum_tensor(f"gps{i}", [128, 512], F32)) for i in range(8)]
                bps = [Buf() for _ in range(8)]
                av = aT_d.rearrange("(kc p) t -> p kc t", p=128)
                npair = D // 512

                def load_pair(pp):
                    load_w_panel(wt[pp % 2], bw[pp % 2], chw[pp % 2], W, pp * 512, 512, KC)

                load_pair(0)
                ai = 0
                oi = 0
                for pp in range(npair):
                    if pp + 1 < npair:
                        load_pair(pp + 1)
                    for th in range(NTH):
                        t0 = g * TG + th * TH
                        for kg in range(KC // KG):
                            a = ai % NA
                            ai += 1
                            tr.dma("sp", cha[a], [(at[a][:, :, :], av[:, kg * KG:(kg + 1) * KG, t0:t0 + TH])], reads=[b_a], writes=[ba[a]])
                            for fb4 in range(4):
                                k = pp % 2
                                for tb in range(NTB):
                                    bank = fb4 * 2 + tb
                                    for kk in range(KG):
                                        kc = kg * KG + kk
                                        tr.op("pe", lambda e: e.matmul(ps[bank][:, :], wt[k][:, kc, fb4 * 128:(fb4 + 1) * 128],
                                                                       at[a][:, kk, tb * 512:(tb + 1) * 512], start=(kc == 0),
                                                                       stop=(kc == KC - 1)),
                                              reads=[bw[k], ba[a]], writes=[bps[bank]],
                                              signal=(kc == KC - 1 or (fb4 == 3 and tb == NTB - 1 and kk == KG - 1)))
                        for fb4 in range(4):
                            for tb in range(NTB):
                                bank = fb4 * 2 + tb
                                o = oi % 4
                                oi += 1
                                if o % 2 == 0:
                                    tr.op("act", lambda e: e.activation(out=ot[o][:, :], in_=ps[bank][:, :], func=AF.Copy),
                                          reads=[bps[bank]], writes=[bo[o]])
                                else:
                                    tr.op("dve", lambda e: e.tensor_copy(out=ot[o][:, :], in_=ps[bank][:, :]),
                                          reads=[bps[bank]], writes=[bo[o]])
                                r0 = pp * 512 + fb4 * 128
                                c0 = t0 + tb * 512
                                tr.dma("sp", cho[o], [(y_d[r0:r0 + 128, c0:c0 + 512], ot[o][:, :])], reads=[bo[o]], writes=[b_y])
                tr.barrier()

        def ret_inproj(g, hT, W):
            def ep_fm(mode, env, a):
                if mode == "init":
                    ph, t = env, a
                    E = P()
                    E.cos = t("r_cos", [128, TG], F32)
                    E.sin = t("r_sin", [128, TG], F32)
                    E.btab = Buf()
                    ch = tr.dma_chan("ph0")
                    tr.dma("sp", ch, [(E.cos[:, :], cosT_d[:, g * TG:(g + 1) * TG]), (E.sin[:, :], sinT_d[:, g * TG:(g + 1) * TG])],
                           reads=[DB["tab"]], writes=[E.btab])
                    E.tmp = [t(f"r_t{i}", [128, 512], F32) for i in range(4)]
                    E.bt = [Buf() for _ in range(4)]
                    E.o = [t(f"r_o{i}", [128, 512], BF16) for i in range(4)]
                    E.bo = [Buf() for _ in range(4)]
                    E.cho = [tr.dma_chan(f"o{i}") for i in range(4)]
                    E.i = 0
                    return E
                E = env
                pi, c0, tok0, ps0, bp0, ps1, bp1 = a
                isk = c0 >= D
                scl = (RET_DK ** -0.5) if isk else 1.0
                dst = kT_d if isk else qT_d
                bdst = DB["kT"] if isk else DB["qT"]
                r0 = c0 - D if isk else c0
                lt = slice(tok0 - g * TG, tok0 - g * TG + 512)
                cs, sn = E.cos[:, lt], E.sin[:, lt]
                A, B_, C, Dd = E.tmp
                bA, bB, bC, bD = E.bt
                o1, o2 = E.o[(E.i * 2) % 4], E.o[(E.i * 2 + 1) % 4]
                bo1, bo2 = E.bo[(E.i * 2) % 4], E.bo[(E.i * 2 + 1) % 4]
                c1, c2 = E.cho[(E.i * 2) % 4], E.cho[(E.i * 2 + 1) % 4]
                E.i += 1
                stt = lambda out, in0, in1: (lambda e: e.scalar_tensor_tensor(out=out, in0=in0, scalar=scl, in1=in1,
                                                                              op0=ALU.mult, op1=ALU.mult))
                tr.op("dve", stt(A[:, :], ps0[:, :], cs), reads=[bp0, E.btab], writes=[bA])
                tr.op("dve", stt(B_[:, :], ps1[:, :], sn), reads=[bp1, E.btab], writes=[bB])
                tr.op("dve", stt(C[:, :], ps1[:, :], cs), reads=[bp1, E.btab], writes=[bC])
                tr.op("dve", stt(Dd[:, :], ps0[:, :], sn), reads=[bp0, E.btab], writes=[bD])
                tr.op("pool", lambda e: e.tensor_tensor(out=o1[:, :], in0=A[:, :], in1=B_[:, :], op=ALU.subtract),
                      reads=[bA, bB], writes=[bo1])
                tr.op("pool", lambda e: e.tensor_tensor(out=o2[:, :], in0=C[:, :], in1=Dd[:, :], op=ALU.add),
                      reads=[bC, bD], writes=[bo2])
                tr.dma("sp", c1, [(dst[r0:r0 + 128, tok0:tok0 + 512], o1[:, :])], reads=[bo1], writes=[bdst])
                tr.dma("sp", c2, [(dst[r0 + 128:r0 + 256, tok0:tok0 + 512], o2[:, :])], reads=[bo2], writes=[bdst])

            gemm_fm(g, hT, W, [h * 256 for h in range(16)], ep_fm, "ret")

            def ep_tm(mode, env, a):
                if mode == "init":
                    ph, t = env, a
                    E = P()
                    NCG = TG // 128
                    E.cos = t("r_cos", [128, NCG, 128], F32)
                    E.sin = t("r_sin", [128, NCG, 128], F32)
                    E.btab = Buf()
                    ch = tr.dma_chan("ph0")
                    tr.dma("sp", ch, [(E.cos[:, :, :], costm_d[:, g * NCG:(g + 1) * NCG, :]),
                                      (E.sin[:, :, :], sintm_d[:, g * NCG:(g + 1) * NCG, :])],
                           reads=[DB["tab"]], writes=[E.btab])
                    E.tmp = [t(f"r_t{i}", [128, 2, 128], F32) for i in range(4)]
                    E.bt = [Buf() for _ in range(4)]
                    E.o = [t(f"r_o{i}", [128, 512], BF16) for i in range(4)]
                    E.bo = [Buf() for _ in range(4)]
                    E.cho = [tr.dma_chan(f"o{i}") for i in range(4)]
                    E.i = 0
                    return E
                E = env
                pi, c0, ncol, tok0, ps, bp = a
                o = E.o[E.i % 4]
                bo = E.bo[E.i % 4]
                co = E.cho[E.i % 4]
                par = E.i % 2
                E.i += 1
                if c0 < 2 * D:
                    lc = (tok0 - g * TG) // 128
                    scl = RET_DK ** -0.5
                    pv = ps[:, :].rearrange("p (h two f) -> p h two f", h=2, two=2)
                    ov = o[:, :].rearrange("p (h two f) -> p h two f", h=2, two=2)
                    cs = E.cos[:, lc, :].unsqueeze(1).to_broadcast([128, 2, 128])
                    sn = E.sin[:, lc, :].unsqueeze(1).to_broadcast([128, 2, 128])
                    A, B_, C, Dd = E.tmp
                    bA, bB, bC, bD = E.bt
                    stt = lambda out, in0, in1: (lambda e: e.scalar_tensor_tensor(out=out, in0=in0, scalar=scl, in1=in1,
                                                                                  op0=ALU.mult, op1=ALU.mult))
                    tr.op("dve", stt(A[:, :, :], pv[:, :, 0, :], cs), reads=[bp, E.btab], writes=[bA])
                    tr.op("dve", stt(B_[:, :, :], pv[:, :, 1, :], sn), reads=[bp, E.btab], writes=[bB])
                    tr.op("dve", stt(C[:, :, :], pv[:, :, 1, :], cs), reads=[bp, E.btab], writes=[bC])
                    tr.op("dve", stt(Dd[:, :, :], pv[:, :, 0, :], sn), reads=[bp, E.btab], writes=[bD])
                    tr.op("pool", lambda e: e.tensor_tensor(out=ov[:, :, 0, :], in0=A[:, :, :], in1=B_[:, :, :], op=ALU.subtract),
                          reads=[bA, bB], writes=[bo])
                    tr.op("pool", lambda e: e.tensor_tensor(out=ov[:, :, 1, :], in0=C[:, :, :], in1=Dd[:, :, :], op=ALU.add),
                          reads=[bC, bD], writes=[bo])
                    tr.dma("sp", co, [(ktm_d[tok0:tok0 + 128, c0 - D:c0 - D + 512], o[:, :])], reads=[bo], writes=[DB["ktm"]])
                elif c0 < 4 * D:
                    if par == 0:
                        tr.op("act", lambda e: e.activation(out=o[:, :], in_=ps[:, :], func=AF.Copy), reads=[bp], writes=[bo])
                    else:
                        tr.op("dve", lambda e: e.tensor_copy(out=o[:, :], in_=ps[:, :]), reads=[bp], writes=[bo])
                    tr.dma("sp", co, [(vtm_d[tok0:tok0 + 128, c0 - 2 * D:c0 - 2 * D + 512], o[:, :])], reads=[bo], writes=[DB["vtm"]])
                else:
                    tr.op("act", lambda e: e.activation(out=o[:, :], in_=ps[:, :], func=AF.Silu), reads=[bp], writes=[bo])
                    tr.dma("sp", co, [(gtm_d[tok0:tok0 + 128, c0 - 4 * D:c0 - 4 * D + 512], o[:, :])], reads=[bo], writes=[DB["gtm"]])

            gemm_tm(g, hT, W, [(D + i * 512, 512) for i in range(20)], ep_tm)

        def ret_mixer(state):
            with ExitStack() as ph:
                t = lambda name, shape, dtype: ph.enter_context(nc.sbuf_tensor(name, list(shape), dtype))
                NJ = 4
                decT = t("m_dec", [128, RET_H, 128], F32)
                xir = t("m_xi", [128, RET_H, 128], F32)
                zet = t("m_zeta", [128, RET_H], F32)
                bc = Buf()
                ch = tr.dma_chan("ph0")
                tr.dma("sp", ch, [(decT[:, :, :], cin["decayT"]), (xir[:, :, :], cin["xirep"]), (zet[:, :], cin["zeta"])], writes=[bc])
                S = t("m_S", [128, RET_H, 2, RET_DV], F32)
                Sb = t("m_Sb", [128, RET_H, 2, RET_DV], BF16)
                bS = [Buf() for _ in range(RET_H)]
                bSb = [Buf() for _ in range(RET_H)]
                tr.op("pool", lambda e: e.memset(S[:, :, :, :], 0.0), writes=bS)
                tr.op("pool", lambda e: e.memset(Sb[:, :, :, :], 0.0), writes=bSb)
                NB = 2
                qt2 = [t(f"m_q{i}", [128, RET_H, 2, 256], BF16) for i in range(NB)]
                kt2 = [t(f"m_k{i}", [128, RET_H, 2, 256], BF16) for i in range(NB)]
                bqk = [Buf() for _ in range(NB)]
                chqk = [tr.dma_chan(f"qk{i}") for i in range(NB)]
                km = [t(f"m_km{i}", [128, D], BF16) for i in range(NB)]
                vm = [t(f"m_v{i}", [128, 2 * D], BF16) for i in range(NB)]
                gm = [t(f"m_g{i}", [128, 2 * D], BF16) for i in range(NB)]
                bin_ = [Buf() for _ in range(NB)]
                chin = [tr.dma_chan(f"a{i}") for i in range(NB)]
                qx = [t(f"m_qx{i}", [128, 2, 128], BF16) for i in range(NJ)]
                bqx = [Buf() for _ in range(NJ)]
                kz = [t(f"m_kz{i}", [128, 256], BF16) for i in range(NJ)]
                bkz = [Buf() for _ in range(NJ)]
                sT = [t(f"m_sT{i}", [128, 128], BF16) for i in range(NJ)]
                bsT = [Buf() for _ in range(NJ)]
                yn = [t(f"m_yn{i}", [128, RET_DV], F32) for i in range(NJ)]
                byn = [Buf() for _ in range(NJ)]
                z = [t(f"m_z{i}", [128, RET_DV], BF16) for i in range(NJ)]
                bz = [Buf() for _ in range(NJ)]
                stats = [t(f"m_st{i}", [128, 6], F32) for i in range(NJ)]
                mv = [t(f"m_mv{i}", [128, 2], F32) for i in range(NJ)]
                rs = [t(f"m_rs{i}", [128, 2], F32) for i in range(NJ)]
                bst = [Buf() for _ in range(NJ)]
                zT = [t(f"m_zT{i}", [128, 32, 512], BF16) for i in range(1)]
                bzT = Buf()
                chz = tr.dma_chan("o0")
                ps_y = [ph.enter_context(nc.psum_tensor(f"mps_y{i}", [128, 512], F32)) for i in range(3)]
                ps_u = [ph.enter_context(nc.psum_tensor(f"mps_u{i}", [128, 512], F32)) for i in range(2)]
                ps_t = [ph.enter_context(nc.psum_tensor("mps_t0", [128, 4, 128], BF16))] * 2
                ps_s = [ph.enter_context(nc.psum_tensor(f"mps_s{i}", [128, 128], F32)) for i in range(2)]
                bps_s, bps_y, bps_u, bps_t = ([Buf() for _ in range(NJ)] for _ in range(4))

                def load(c):
                    i = c % NB
                    sl = slice(c * 128, (c + 1) * 128)
                    if c % 2 == 0:
                        i2 = (c // 2) % NB
                        s2 = slice(c * 128, (c + 2) * 128)
                        tr.dma("sp", chqk[i2], [
                            (qt2[i2][:, :, :, :], qT_d[:, s2].rearrange("(h two p) t -> p h two t", two=2, p=128)),
                            (kt2[i2][:, :, :, :], kT_d[:, s2].rearrange("(h two p) t -> p h two t", two=2, p=128))],
                            reads=[DB["qT"], DB["kT"]], writes=[bqk[i2]])
                    tr.dma("sp", chin[i], [
                        (km[i][:, :], ktm_d[sl, :]), (vm[i][:, :], vtm_d[sl, :]), (gm[i][:, :], gtm_d[sl, :])],
                        reads=[DB["ktm"], DB["vtm"], DB["gtm"]], writes=[bin_[i]])

                if exch:
                    zF = t("m_zF", [128, NCH, RET_H], F32)
                    selw = t("m_selw", [128, 8], F32)
                    kzp = [t(f"m_kzp{i}", [128, 2, 256], BF16) for i in range(2)]
                    bkzp = [Buf(), Buf()]
                    bzf = Buf()
                    tr.dma("sp", tr.dma_chan("ph1"), [(zF[:, :, :], cin["zetaF"]), (selw[:, :], selw_in)], writes=[bzf])
                    accs = [(ps_y[0], bps_y[0]), (ps_y[1], bps_y[1]), (ps_u[0], bps_u[0]), (ps_u[1], bps_u[1])]
                    li = 0
                    cho2 = [tr.dma_chan("o1"), tr.dma_chan("o2")]
                    for hp in range(RET_H // 2):
                        for c in range(NCH):
                            i = li % NB
                            li += 1
                            sl = slice(c * 128, (c + 1) * 128)
                            tr.dma("sp", chin[i], [(km[i][:, 0:512], ktm_d[sl, hp * 512:(hp + 1) * 512]),
                                                   (vm[i][:, 0:1024], vtm_d[sl, hp * 1024:(hp + 1) * 1024])],
                                   reads=[DB["ktm"], DB["vtm"]], writes=[bin_[i]])
                            kk = c % 2
                            tr.op("pool", lambda e: e.tensor_tensor(out=kzp[kk][:, :, :],
                                                                    in0=km[i][:, 0:512].rearrange("p (h d) -> p h d", h=2),
                                                                    in1=zF[:, c, hp * 2:hp * 2 + 2].unsqueeze(2).to_broadcast([128, 2, 256]),
                                                                    op=ALU.mult), reads=[bin_[i], bzf], writes=[bkzp[kk]])
                            for hl in range(2):
                                for dcc in range(2):
                                    pa, bpa = accs[hl * 2 + dcc]
                                    tr.op("pe", lambda e: e.matmul(pa[:, :], kzp[kk][:, hl, dcc * 128:(dcc + 1) * 128],
                                                                   vm[i][:, hl * 512:(hl + 1) * 512], start=(c == 0), stop=(c == NCH - 1)),
                                          reads=[bkzp[kk], bin_[i]], writes=[bpa], signal=(c == NCH - 1 or (hl == 1 and dcc == 1)))
                        for hl in range(2):
                            for dcc in range(2):
                                pa, bpa = accs[hl * 2 + dcc]
                                k2 = (hl * 2 + dcc) % 2
                                tr.op("act", lambda e: e.activation(out=yn[k2][:, :], in_=pa[:, :], func=AF.Copy), reads=[bpa], writes=[byn[k2]])
                                r0 = ((hp * 2 + hl) * 2 + dcc) * 128
                                tr.dma("sp", cho2[k2], [(Sx_d[r0:r0 + 128, :], yn[k2][:, :])], reads=[byn[k2]], writes=[DBX["Sx"]])
                    tr.collective(Sx_d, G_d, exch, reads=[DBX["Sx"]], writes=[DBX["G"]])
                    Gv = G_d.rearrange("(r b p) e -> r b p e", r=exch, p=128)
                    li = 0
                    for r in range(exch):
                        for b16 in range(16):
                            k2 = li % 2
                            li += 1
                            h_, dcc = b16 // 2, b16 % 2
                            tr.dma("sp", cho2[k2], [(yn[k2][:, :], Gv[r, b16, :, :])], reads=[DBX["G"]], writes=[byn[k2]])
                            tr.op("dve", lambda e: e.scalar_tensor_tensor(out=S[:, h_, dcc, :], in0=yn[k2][:, :], scalar=selw[:, r:r + 1],
                                                                          in1=S[:, h_, dcc, :], op0=ALU.mult, op1=ALU.add),
                                  reads=[byn[k2], bzf, bS[h_]], writes=[bS[h_]])
                    for h_ in range(RET_H):
                        tr.op("act", lambda e: e.activation(out=Sb[:, h_, :, :], in_=S[:, h_, :, :], func=AF.Copy),
                              reads=[bS[h_]], writes=[bSb[h_]])
                iters = [(c, h) for c in range(NCH) for h in range(RET_H)]
                NIT = len(iters)

                def stage_a(n):
                    c, h = iters[n]
                    j, j2, i, i2, co = n % NJ, n % 2, c % NB, (c // 2) % NB, (c % 2) * 128
                    for dcc in range(2):
                        tr.op("pe", lambda e: e.matmul(ps_s[j2][:, :], kt2[i2][:, h, dcc, co:co + 128], qt2[i2][:, h, dcc, co:co + 128],
                                                       start=(dcc == 0), stop=(dcc == 1)),
                              reads=[bqk[i2]], writes=[bps_s[j2]], signal=(dcc == 1))
                    tr.op("dve", lambda e: e.tensor_tensor(out=sT[j][:, :], in0=ps_s[j2][:, :], in1=decT[:, h, :], op=ALU.mult),
                          reads=[bps_s[j2], bc], writes=[bsT[j]])
                    tr.op("pool", lambda e: e.tensor_tensor(out=qx[j][:, :, :], in0=qt2[i2][:, h, :, co:co + 128],
                                                            in1=xir[:, h, :].unsqueeze(1).to_broadcast([128, 2, 128]),
                                                            op=ALU.mult), reads=[bqk[i2], bc], writes=[bqx[j]])
                    tr.op("pool", lambda e: e.tensor_tensor(out=kz[j][:, :], in0=km[i][:, h * 256:(h + 1) * 256],
                                                            in1=zet[:, h:h + 1].to_broadcast([128, 256]), op=ALU.mult),
                          reads=[bin_[i], bc], writes=[bkz[j]])
                    tr.op("pe", lambda e: e.matmul(ps_y[n % 3][:, :], sT[j][:, :], vm[i][:, h * 512:(h + 1) * 512],
                                                   start=True, stop=False),
                          reads=[bsT[j], bin_[i]], writes=[bps_y[n % 3]], signal=False)
                    for dcc in range(2):
                        tr.op("pe", lambda e: e.matmul(ps_y[n % 3][:, :], qx[j][:, dcc, :], Sb[:, h, dcc, :],
                                                       start=False, stop=(dcc == 1)),
                              reads=[bqx[j], bSb[h]], writes=[bps_y[n % 3]], signal=(dcc == 1))
                    for dcc in range(2):
                        tr.op("pe", lambda e: e.matmul(ps_u[dcc][:, :], kz[j][:, dcc * 128:(dcc + 1) * 128],
                                                       vm[i][:, h * 512:(h + 1) * 512], start=True, stop=True),
                              reads=[bkz[j], bin_[i]], writes=[bps_u[dcc]])
                    for dcc in range(2):
                        tr.op("dve", lambda e: e.scalar_tensor_tensor(out=S[:, h, dcc, :], in0=S[:, h, dcc, :],
                                                                      scalar=HC["gchunk"][h], in1=ps_u[dcc][:, :],
                                                                      op0=ALU.mult, op1=ALU.add),
                              reads=[bps_u[dcc], bS[h]], writes=[bS[h]])
                    tr.op("act", lambda e: e.activation(out=Sb[:, h, :, :], in_=S[:, h, :, :], func=AF.Copy),
                          reads=[bS[h]], writes=[bSb[h]])

                def stage_b1(n):
                    j = n % NJ
                    tr.op("dve", lambda e: e.bn_stats(out=stats[j][:, :], in_=ps_y[n % 3][:, :]), reads=[bps_y[n % 3]], writes=[bst[j]])
                    tr.op("dve", lambda e: e.bn_aggr(out=mv[j][:, :], in_=stats[j][:, :]), reads=[bst[j]], writes=[bst[j]])
                    tr.op("act", lambda e: e.activation(out=rs[j][:, 0:1], in_=mv[j][:, 1:2], func=AF.Sqrt,
                                                        bias=K.eps[:, 0:1]), reads=[bst[j], b_const], writes=[bst[j]])

                def stage_b2(n):
                    c, h = iters[n]
                    j, i = n % NJ, c % NB
                    tr.op("dve", lambda e: e.reciprocal(out=rs[j][:, 0:1], in_=rs[j][:, 0:1]), reads=[bst[j]], writes=[bst[j]])
                    tr.op("dve", lambda e: e.scalar_tensor_tensor(out=rs[j][:, 1:2], in0=mv[j][:, 0:1], scalar=-1.0,
                                                                  in1=rs[j][:, 0:1], op0=ALU.mult, op1=ALU.mult),
                          reads=[bst[j]], writes=[bst[j]])
                    tr.op("act", lambda e: e.activation(out=yn[j][:, :], in_=ps_y[n % 3][:, :], func=AF.Identity,
                                                        bias=rs[j][:, 1:2], scale=rs[j][:, 0:1]),
                          reads=[bps_y[n % 3], bst[j]], writes=[byn[j]])
                    tr.op("pool", lambda e: e.tensor_tensor(out=z[j][:, :], in0=yn[j][:, :], in1=gm[i][:, h * 512:(h + 1) * 512],
                                                            op=ALU.mult), reads=[byn[j], bin_[i]], writes=[bz[j]])

                def stage_c(n):
                    c, h = iters[n]
                    j, j2, c4 = n % NJ, n % 2, c % 4
                    for q4 in range(4):
                        tr.op("pe", lambda e: e.transpose(out=ps_t[j2][:, q4, :], in_=z[j][:, q4 * 128:(q4 + 1) * 128],
                                                          identity=K.ident_b[:, :]),
                              reads=[bz[j], b_const], writes=[bps_t[0]], signal=(q4 == 3))
                    tr.op("act", lambda e: e.activation(out=zT[0][:, h * 4:(h + 1) * 4, c4 * 128:(c4 + 1) * 128],
                                                        in_=ps_t[j2][:, :, :], func=AF.Copy),
                          reads=[bps_t[0]], writes=[bzT])
                    if h == RET_H - 1 and (c4 == 3 or c == NCH - 1):
                        nt = (c4 + 1) * 128
                        t0 = (c - c4) * 128
                        tr.dma("sp", chz, [(zT_d[:, t0:t0 + nt].rearrange("(b p) t -> p b t", p=128), zT[0][:, :, 0:nt])],
                               reads=[bzT], writes=[DB["zT"]])

                load(0)
                for n in range(NIT + 3):
                    if n < NIT:
                        c, h = iters[n]
                        if h == 3 and c + 1 < NCH:
                            load(c + 1)
                        stage_a(n)
                    if 0 <= n - 1 < NIT:
                        stage_b1(n - 1)
                    if 0 <= n - 2 < NIT:
                        stage_b2(n - 2)
                    if 0 <= n - 3 < NIT:
                        stage_c(n - 3)
                tr.barrier()

        def ml_inproj(g, hT, W):
            def ep_fm(mode, env, a):
                if mode == "init":
                    ph, t = env, a
                    E = P()
                    E.o = [t(f"l_o{i}", [128, 512], BF16) for i in range(4)]
                    E.bo = [Buf() for _ in range(4)]
                    E.cho = [tr.dma_chan(f"o{i}") for i in range(4)]
                    E.i = 0
                    return E
                E = env
                pi, c0, tok0, ps0, bp0, ps1, bp1 = a
                isk = c0 >= 1024
                scl = (ML_DQK ** -0.5) if isk else 1.0
                dst = kT_d if isk else qT_d
                bdst = DB["kT"] if isk else DB["qT"]
                r0 = c0 - 1024 if isk else c0
                for fb, (ps, bp) in enumerate(((ps0, bp0), (ps1, bp1))):
                    k = E.i % 4
                    E.i += 1
                    if fb == 0:
                        tr.op("act", lambda e: e.activation(out=E.o[k][:, :], in_=ps[:, :], func=AF.Copy, scale=scl),
                              reads=[bp], writes=[E.bo[k]])
                    else:
                        tr.op("dve", lambda e: e.tensor_scalar(out=E.o[k][:, :], in0=ps[:, :], scalar1=scl, scalar2=None,
                                                               op0=ALU.mult), reads=[bp], writes=[E.bo[k]])
                    tr.dma("sp", E.cho[k], [(dst[r0 + fb * 128:r0 + fb * 128 + 128, tok0:tok0 + 512], E.o[k][:, :])],
                           reads=[E.bo[k]], writes=[bdst])

            gemm_fm(g, hT, W, [i * 256 for i in range(8)], ep_fm, "ml")

            def ep_tm(mode, env, a):
                if mode == "init":
                    ph, t = env, a
                    E = P()
                    E.o = [t(f"l_o{i}", [128, 512], BF16) for i in range(4)]
                    E.bo = [Buf() for _ in range(4)]
                    E.cho = [tr.dma_chan(f"o{i}") for i in range(4)]
                    E.og = [t(f"l_og{i}", [128, 16], F32) for i in range(2)]
                    E.bog = [Buf() for _ in range(2)]
                    E.chg = [tr.dma_chan(f"a{i}") for i in range(2)]
                    E.i = 0
                    return E
                E = env
                pi, c0, ncol, tok0, ps, bp = a
                k = E.i % 4
                E.i += 1
                o, bo, co = E.o[k], E.bo[k], E.cho[k]
                if c0 < 2048:
                    scl = ML_DQK ** -0.5
                    tr.op("act", lambda e: e.activation(out=o[:, :], in_=ps[:, :], func=AF.Copy, scale=scl), reads=[bp], writes=[bo])
                    tr.dma("sp", co, [(ktm_d[tok0:tok0 + 128, c0 - 1024:c0 - 1024 + 512], o[:, :])], reads=[bo], writes=[DB["ktm"]])
                elif c0 < 4096:
                    tr.op("dve", lambda e: e.tensor_copy(out=o[:, :], in_=ps[:, :]), reads=[bp], writes=[bo])
                    tr.dma("sp", co, [(vtm_d[tok0:tok0 + 128, c0 - 2048:c0 - 2048 + 512], o[:, :])], reads=[bo], writes=[DB["vtm"]])
                elif c0 < 6144:
                    tr.op("act", lambda e: e.activation(out=o[:, :], in_=ps[:, :], func=AF.Sigmoid), reads=[bp], writes=[bo])
                    tr.dma("sp", co, [(gtm_d[tok0:tok0 + 128, c0 - 4096:c0 - 4096 + 512], o[:, :])], reads=[bo], writes=[DB["gtm"]])
                else:
                    kk = E.i % 2
                    tr.op("dve", lambda e: e.tensor_copy(out=E.og[kk][:, :], in_=ps[:, 0:16]), reads=[bp], writes=[E.bog[kk]])
                    tr.dma("sp", E.chg[kk], [(gate_d[tok0:tok0 + 128, :], E.og[kk][:, :])], reads=[E.bog[kk]], writes=[DB["gate"]])

            gemm_tm(g, hT, W, [(1024 + i * 512, 512) for i in range(10)] + [(6144, 16)], ep_tm)

        def ml_mixer(jl):
            with ExitStack() as ph:
                t = lambda name, shape, dtype: ph.enter_context(nc.sbuf_tensor(name, list(shape), dtype))
                tri = t("x_tri", [128, 128], F32)
                negm = t("x_negm", [128, 128], F32)
                bgt = t("x_bg", [128, 16], F32)
                ngt = t("x_ng", [128, D], F32)
                onesb = t("x_1b", [128, 1], BF16)
                bc = Buf()
                ch = tr.dma_chan("ph0")
                tr.dma("sp", ch, [(tri[:, :], cin["tri_f"]), (negm[:, :], cin["negm_f"]), (bgt[:, :], ml_bg[:, jl, :]),
                                  (ngt[:, :], ml_ng[:, jl, :])], writes=[bc])
                tr.op("dve", lambda e: e.memset(onesb[:, :], 1.0), writes=[bc])
                NCH8 = NCH * 8
                gts = t("x_gts", [128, NCH, 16], F32)
                ilog = t("x_il", [128, NCH, 8], F32)
                flog = t("x_fl", [128, NCH, 8], F32)
                cS = t("x_cS", [128, NCH, 8], F32)
                wS = t("x_wS", [128, NCH, 8], F32)
                eBt = t("x_eBt", [128, NCH, 8], F32)
                bg_ = Buf()
                tr.dma("sp", tr.dma_chan("ph1"), [(gts[:, :, :], gate_d.rearrange("(c p) k -> p c k", p=128))], reads=[DB["gate"]], writes=[bg_])
                tr.op("dve", lambda e: e.tensor_tensor(out=gts[:, :, :], in0=gts[:, :, :],
                                                       in1=bgt[:, :].unsqueeze(1).to_broadcast([128, NCH, 16]), op=ALU.add),
                      reads=[bg_, bc], writes=[bg_])
                tr.op("act", lambda e: e.activation(out=gts[:, :, :], in_=gts[:, :, :], func=AF.Tanh, scale=1.0 / SOFTCAP),
                      reads=[bg_], writes=[bg_])
                bil, bfl = Buf(), Buf()
                tr.op("dve", lambda e: e.tensor_scalar(out=ilog[:, :, :], in0=gts[:, :, 0:8], scalar1=SOFTCAP, scalar2=None,
                                                       op0=ALU.mult), reads=[bg_], writes=[bil])
                tr.op("act", lambda e: e.activation(out=flog[:, :, :], in_=gts[:, :, 8:16], func=AF.Exp, scale=-SOFTCAP),
                      reads=[bg_], writes=[bfl])
                tr.op("act", lambda e: e.activation(out=flog[:, :, :], in_=flog[:, :, :], func=AF.Ln, bias=K.one[:, 0:1]),
                      reads=[bfl, b_const], writes=[bfl])
                tr.op("dve", lambda e: e.tensor_scalar(out=flog[:, :, :], in0=flog[:, :, :], scalar1=-1.0, scalar2=None,
                                                       op0=ALU.mult), reads=[bfl], writes=[bfl])
                ps = [ph.enter_context(nc.psum_tensor(f"xps{i}", [128, 512], F32)) for i in range(8)]
                bps = [Buf() for _ in range(8)]
                pB, pDl, pSc, pN0, pN1, pU0, pU1, pT = ps
                bB, bDl, bSc, bN0, bN1, bU0, bU1, bT = bps
                pTb = pT[:, :].bitcast(BF16)
                fl2 = flog[:, :, :].rearrange("p c h -> p (c h)")
                tr.op("pe", lambda e: e.matmul(pB[:, 0:NCH8], tri[:, :], fl2, start=True, stop=True), reads=[bc, bfl], writes=[bB])
                tr.op("pe", lambda e: e.matmul(pDl[:, 0:NCH8], K.ones_f[:, :], fl2, start=True, stop=True),
                      reads=[b_const, bfl], writes=[bDl])
                bcS, bwS, beBt = Buf(), Buf(), Buf()
                tr.op("dve", lambda e: e.tensor_tensor(out=cS[:, :, :].rearrange("p c h -> p (c h)"),
                                                       in0=ilog[:, :, :].rearrange("p c h -> p (c h)"), in1=pB[:, 0:NCH8],
                                                       op=ALU.subtract), reads=[bil, bB], writes=[bcS])
                tr.op("dve", lambda e: e.tensor_tensor(out=wS[:, :, :].rearrange("p c h -> p (c h)"),
                                                       in0=cS[:, :, :].rearrange("p c h -> p (c h)"), in1=pDl[:, 0:NCH8],
                                                       op=ALU.add), reads=[bcS, bDl], writes=[bwS])
                tr.op("act", lambda e: e.activation(out=wS[:, :, :], in_=wS[:, :, :], func=AF.Exp), reads=[bwS], writes=[bwS])
                tr.op("act", lambda e: e.activation(out=eBt[:, :, :].rearrange("p c h -> p (c h)"), in_=pDl[:, 0:NCH8], func=AF.Exp),
                      reads=[bDl], writes=[beBt])
                C = t("x_C", [128, ML_H, ML_DV], F32)
                Cb = t("x_Cb", [128, ML_H, ML_DV], BF16)
                nv = t("x_n", [128, ML_H], F32)
                nb = t("x_nb", [128, ML_H], BF16)
                bC = [Buf(), Buf()]
                bCb = [Buf(), Buf()]
                tr.op("pool", lambda e: e.memset(C[:, :, :], 0.0), writes=bC)
                tr.op("pool", lambda e: e.memset(Cb[:, :, :], 0.0), writes=bCb)
                tr.op("pool", lambda e: e.memset(nv[:, :], 0.0), writes=bC)
                tr.op("pool", lambda e: e.memset(nb[:, :], 0.0), writes=bCb)
                NB = 2
                qt2 = [t(f"x_q{i}", [128, ML_H, 256], BF16) for i in range(NB)]
                kt2 = [t(f"x_k{i}", [128, ML_H, 256], BF16) for i in range(NB)]
                bqk = [Buf() for _ in range(NB)]
                chqk = [tr.dma_chan(f"qk{i}") for i in range(NB)]
                km = [t(f"x_km{i}", [128, 1024], BF16) for i in range(NB)]
                vm = [t(f"x_vm{i}", [128, D], BF16) for i in range(NB)]
                om = [t(f"x_om{i}", [128, D], BF16) for i in range(NB)]
                bin_ = [Buf() for _ in range(NB)]
                chin = [tr.dma_chan(f"a{i}") for i in range(NB)]
                Ftri = [t(f"x_F{i}", [128, 4, 128], F32) for i in range(2)]
                R2 = [t(f"x_R{i}", [128, 4, 128], F32) for i in range(2)]
                eB = [t(f"x_eB{i}", [128, 4, 128], F32) for i in range(2)]
                dw = [t(f"x_dw{i}", [128, 4, 128], F32) for i in range(2)]
                PT = [t(f"x_PT{i}", [128, 4, 128], BF16) for i in range(2)]
                qd = [t(f"x_qd{i}", [128, 4, 128], BF16) for i in range(2)]
                kw = [t(f"x_kw{i}", [128, 4, 128], BF16) for i in range(2)]
                gso = [t(f"x_gso{i}", [128, 1024], F32) for i in range(2)]
                zz = [t(f"x_z{i}", [128, 1024], BF16) for i in range(2)]
                junk = t("x_junk", [128, 256], F32)
                sm = [t(f"x_sm{i}", [128, 16], F32) for i in range(2)]
                bF, bR, beB, bdw, bPT, bqd, bkw, bgso, bzz, bsm = ([Buf(), Buf()] for _ in range(10))
                bjunk = Buf()
                zT = t("x_zT", [128, 16, 512], BF16)
                bzT = Buf()
                chz = tr.dma_chan("o0")

                def load(c):
                    i = c % NB
                    sl = slice(c * 128, (c + 1) * 128)
                    if c % 2 == 0:
                        i2 = (c // 2) % NB
                        s2 = slice(c * 128, (c + 2) * 128)
                        tr.dma("sp", chqk[i2], [
                            (qt2[i2][:, :, :], qT_d[0:1024, s2].rearrange("(h p) t -> p h t", p=128)),
                            (kt2[i2][:, :, :], kT_d[0:1024, s2].rearrange("(h p) t -> p h t", p=128))],
                            reads=[DB["qT"], DB["kT"]], writes=[bqk[i2]])
                    tr.dma("sp", chin[i], [(km[i][:, :], ktm_d[sl, 0:1024]), (vm[i][:, :], vtm_d[sl, 0:D]), (om[i][:, :], gtm_d[sl, 0:D])],
                           reads=[DB["ktm"], DB["vtm"], DB["gtm"]], writes=[bin_[i]])

                if exch:
                    selw = t("x_selw", [128, 8], F32)
                    bsel = Buf()
                    tr.dma("sp", tr.dma_chan("ph2"), [(selw[:, :], selw_in)], writes=[bsel])
                    Bt = t("x_Bt", [128, NCH, 8], F32)
                    Sfx = t("x_Sfx", [128, NCH, 8], F32)
                    wF = t("x_wF", [128, NCH, 8], F32)
                    bBt, bSfx, bwF = Buf(), Buf(), Buf()
                    tr.op("dve", lambda e: e.tensor_copy(out=Bt[:, :, :].rearrange("p c h -> p (c h)"), in_=pDl[:, 0:NCH8]),
                          reads=[bDl], writes=[bBt])
                    tr.op("dve", lambda e: e.memset(Sfx[:, NCH - 1, :], 0.0), writes=[bSfx])
                    for c in range(NCH - 2, -1, -1):
                        tr.op("dve", lambda e: e.tensor_tensor(out=Sfx[:, c, :], in0=Sfx[:, c + 1, :], in1=Bt[:, c + 1, :], op=ALU.add),
                              reads=[bSfx, bBt], writes=[bSfx])
                    tr.op("dve", lambda e: e.tensor_tensor(out=wF[:, :, :], in0=cS[:, :, :], in1=Bt[:, :, :], op=ALU.add),
                          reads=[bcS, bBt], writes=[bwF])
                    tr.op("dve", lambda e: e.tensor_tensor(out=wF[:, :, :], in0=wF[:, :, :], in1=Sfx[:, :, :], op=ALU.add),
                          reads=[bwF, bSfx], writes=[bwF])
                    tr.op("act", lambda e: e.activation(out=wF[:, :, :], in_=wF[:, :, :], func=AF.Exp), reads=[bwF], writes=[bwF])
                    kwp = [t(f"x_kwp{i}", [128, 8, 128], BF16) for i in range(2)]
                    bkwp = [Buf(), Buf()]
                    accC = [(pN0, bN0), (pN1, bN1), (pU0, bU0), (pU1, bU1)]
                    for c in range(NCH):
                        i = c % NB
                        sl = slice(c * 128, (c + 1) * 128)
                        tr.dma("sp", chin[i], [(km[i][:, :], ktm_d[sl, 0:1024]), (vm[i][:, :], vtm_d[sl, 0:D])],
                               reads=[DB["ktm"], DB["vtm"]], writes=[bin_[i]])
                        kk = c % 2
                        tr.op("pool", lambda e: e.tensor_tensor(out=kwp[kk][:, :, :], in0=km[i][:, :].rearrange("p (h d) -> p h d", h=8),
                                                                in1=wF[:, c, :].unsqueeze(2).to_broadcast([128, 8, 128]), op=ALU.mult),
                              reads=[bin_[i], bwF], writes=[bkwp[kk]])
                        for h in range(8):
                            pa, bpa = accC[h // 2]
                            cs = (h % 2) * 256
                            tr.op("pe", lambda e: e.matmul(pa[:, cs:cs + 256], kwp[kk][:, h, :], vm[i][:, h * 256:(h + 1) * 256],
                                                           start=(c == 0), stop=(c == NCH - 1)),
                                  reads=[bkwp[kk], bin_[i]], writes=[bpa], signal=False)
                            tr.op("pe", lambda e: e.matmul(pSc[:, h:h + 1], kwp[kk][:, h, :], onesb[:, 0:1],
                                                           start=(c == 0), stop=(c == NCH - 1)),
                                  reads=[bkwp[kk], bc], writes=[bSc], signal=(h == 7))
                    cho2 = [tr.dma_chan("o1"), tr.dma_chan("o2"), tr.dma_chan("o3")]
                    for b4, (pa, bpa) in enumerate(accC):
                        g_ = gso[b4 // 2]
                        off = (b4 % 2) * 512
                        tr.op("act", lambda e: e.activation(out=g_[:, off:off + 512], in_=pa[:, :], func=AF.Copy),
                              reads=[bpa], writes=[bgso[b4 // 2]])
                    for k in range(2):
                        tr.dma("sp", cho2[k], [(Cx_d[k * 512:(k + 1) * 512, :].rearrange("(h p) e -> p h e", p=128),
                                                gso[k][:, :].rearrange("p (h e) -> p h e", h=4))], reads=[bgso[k]], writes=[DBX["Cx"]])
                    tr.op("dve", lambda e: e.tensor_copy(out=sm[0][:, 0:8], in_=pSc[:, 0:8]), reads=[bSc], writes=[bsm[0]])
                    tr.dma("sp", cho2[2], [(Cx_d[1024:1152, 0:8], sm[0][:, 0:8])], reads=[bsm[0]], writes=[DBX["Cx"]])
                    tr.collective(Cx_d, Gc_d, exch, reads=[DBX["Cx"]], writes=[DBX["Gc"]])
                    for r in range(exch):
                        for k in range(2):
                            tr.dma("sp", cho2[k], [(gso[k][:, :].rearrange("p (h e) -> p h e", h=4),
                                                    Gc_d[r * 1152 + k * 512:r * 1152 + (k + 1) * 512, :].rearrange("(h p) e -> p h e", p=128))],
                                   reads=[DBX["Gc"]], writes=[bgso[k]])
                            tr.op("dve", lambda e: e.scalar_tensor_tensor(out=C[:, k * 4:(k + 1) * 4, :].rearrange("p h e -> p (h e)"),
                                                                          in0=gso[k][:, :], scalar=selw[:, r:r + 1],
                                                                          in1=C[:, k * 4:(k + 1) * 4, :].rearrange("p h e -> p (h e)"),
                                                                          op0=ALU.mult, op1=ALU.add),
                                  reads=[bgso[k], bsel, bC[k]], writes=[bC[k]])
                        tr.dma("sp", cho2[2], [(sm[1][:, 0:8], Gc_d[r * 1152 + 1024:r * 1152 + 1152, 0:8])], reads=[DBX["Gc"]], writes=[bsm[1]])
                        tr.op("dve", lambda e: e.scalar_tensor_tensor(out=nv[:, :], in0=sm[1][:, 0:8], scalar=selw[:, r:r + 1], in1=nv[:, :],
                                                                      op0=ALU.mult, op1=ALU.add),
                              reads=[bsm[1], bsel] + bC, writes=bC)
                    for k in range(2):
                        tr.op("act", lambda e: e.activation(out=Cb[:, k * 4:(k + 1) * 4, :], in_=C[:, k * 4:(k + 1) * 4, :], func=AF.Copy),
                              reads=[bC[k]], writes=[bCb[k]])
                    tr.op("act", lambda e: e.activation(out=nb[:, :], in_=nv[:, :], func=AF.Copy), reads=bC, writes=bCb)
                load(0)
                it = 0
                for c in range(NCH):
                    i = c % NB
                    if c + 1 < NCH:
                        load(c + 1)
                    c4 = c % 4
                    i2 = (c // 2) % NB
                    co = (c % 2) * 128
                    for hh in range(2):
                        j = it % 2
                        it += 1
                        h0 = hh * 4
                        tr.op("dve", lambda e: e.tensor_tensor(out=Ftri[j][:, :, :], in0=tri[:, :].unsqueeze(1).to_broadcast([128, 4, 128]),
                                                               in1=flog[:, c, h0:h0 + 4].unsqueeze(2).to_broadcast([128, 4, 128]),
                                                               op=ALU.mult), reads=[bc, bfl], writes=[bF[j]])
                        tr.op("pool", lambda e: e.tensor_tensor(out=R2[j][:, :, :], in0=negm[:, :].unsqueeze(1).to_broadcast([128, 4, 128]),
                                                                in1=cS[:, c, h0:h0 + 4].unsqueeze(2).to_broadcast([128, 4, 128]),
                                                                op=ALU.add), reads=[bc, bcS], writes=[bR[j]])
                        F2 = Ftri[j][:, :, :].rearrange("p h n -> p (h n)")
                        R22 = R2[j][:, :, :].rearrange("p h n -> p (h n)")
                        tr.op("pe", lambda e: e.matmul(pB[:, :], K.ones_f[:, :], F2, start=True, stop=True),
                              reads=[b_const, bF[j]], writes=[bB])
                        tr.op("pe", lambda e: e.matmul(pDl[:, :], K.ones_f[:, :], F2, start=True, stop=False),
                              reads=[b_const, bF[j]], writes=[bDl], signal=False)
                        tr.op("pe", lambda e: e.matmul(pDl[:, :], K.ident_f[:, :], R22, start=False, stop=True),
                              reads=[b_const, bR[j]], writes=[bDl])
                        tr.op("act", lambda e: e.activation(out=eB[j][:, :, :].rearrange("p h n -> p (h n)"), in_=pB[:, :], func=AF.Exp),
                              reads=[bB], writes=[beB[j]])
                        tr.op("act", lambda e: e.activation(out=dw[j][:, :, :].rearrange("p h n -> p (h n)"), in_=pDl[:, :], func=AF.Exp),
                              reads=[bDl], writes=[bdw[j]])
                        for h in range(4):
                            tr.op("pe", lambda e: e.matmul(pSc[:, h * 128:(h + 1) * 128], kt2[i2][:, h0 + h, co:co + 128],
                                                           qt2[i2][:, h0 + h, co:co + 128], start=True, stop=True),
                                  reads=[bqk[i2]], writes=[bSc], signal=(h == 3))
                        tr.op("dve", lambda e: e.tensor_tensor(out=PT[j][:, :, :].rearrange("p h n -> p (h n)"), in0=pSc[:, :],
                                                               in1=dw[j][:, :, :].rearrange("p h n -> p (h n)"), op=ALU.mult),
                              reads=[bSc, bdw[j]], writes=[bPT[j]])
                        tr.op("pool", lambda e: e.tensor_tensor(out=qd[j][:, :, :], in0=qt2[i2][:, h0:h0 + 4, co:co + 128],
                                                                in1=eB[j][:, :, :], op=ALU.mult), reads=[bqk[i2], beB[j]], writes=[bqd[j]])
                        for h in range(4):
                            pn, bn_ = (pN0, bN0) if h < 2 else (pN1, bN1)
                            cs = (h % 2) * 256
                            tr.op("pe", lambda e: e.matmul(pn[:, cs:cs + 256], PT[j][:, h, :], vm[i][:, (h0 + h) * 256:(h0 + h + 1) * 256],
                                                           start=True, stop=False), reads=[bPT[j], bin_[i]], writes=[bn_], signal=False)
                            tr.op("pe", lambda e: e.matmul(pn[:, cs:cs + 256], qd[j][:, h, :], Cb[:, h0 + h, :], start=False, stop=True),
                                  reads=[bqd[j], bCb[hh]], writes=[bn_], signal=(h % 2 == 1))
                        for h in range(4):
                            tr.op("pe", lambda e: e.matmul(pDl[:, h:h + 1], PT[j][:, h, :], onesb[:, 0:1], start=True, stop=False),
                                  reads=[bPT[j], bc], writes=[bDl], signal=False)
                            tr.op("pe", lambda e: e.matmul(pDl[:, h:h + 1], qd[j][:, h, :], nb[:, h0 + h:h0 + h + 1], start=False, stop=True),
                                  reads=[bqd[j], bCb[hh]], writes=[bDl], signal=(h == 3))
                        s_ = sm[j]
                        tr.op("act", lambda e: e.activation(out=s_[:, 0:4], in_=pDl[:, 0:4], func=AF.Abs), reads=[bDl], writes=[bsm[j]])
                        tr.op("dve", lambda e: e.tensor_scalar(out=s_[:, 0:4], in0=s_[:, 0:4], scalar1=1.0, scalar2=None,
                                                               op0=ALU.max), reads=[bsm[j]], writes=[bsm[j]])
                        tr.op("dve", lambda e: e.reciprocal(out=s_[:, 4:8], in_=s_[:, 0:4]), reads=[bsm[j]], writes=[bsm[j]])
                        for h in range(4):
                            pn, bn_ = (pN0, bN0) if h < 2 else (pN1, bN1)
                            cs = (h % 2) * 256
                            tr.op("act", lambda e: e.activation(out=junk[:, :], in_=pn[:, cs:cs + 256], func=AF.Square,
                                                                scale=s_[:, 4 + h:5 + h], accum_out=s_[:, 8 + h:9 + h]),
                                  reads=[bn_, bsm[j]], writes=[bjunk, bsm[j]])
                        tr.op("act", lambda e: e.activation(out=s_[:, 8:12], in_=s_[:, 8:12], func=AF.Sqrt, scale=1.0 / ML_DV,
                                                            bias=K.eps[:, 0:1]), reads=[bsm[j], b_const], writes=[bsm[j]])
                        tr.op("dve", lambda e: e.reciprocal(out=s_[:, 8:12], in_=s_[:, 8:12]), reads=[bsm[j]], writes=[bsm[j]])
                        tr.op("dve", lambda e: e.tensor_tensor(out=s_[:, 12:16], in0=s_[:, 8:12], in1=s_[:, 4:8], op=ALU.mult),
                              reads=[bsm[j]], writes=[bsm[j]])
                        tr.op("pool", lambda e: e.tensor_tensor(out=gso[j][:, :], in0=om[i][:, h0 * 256:h0 * 256 + 1024],
                                                                in1=ngt[:, h0 * 256:h0 * 256 + 1024], op=ALU.mult),
                              reads=[bin_[i], bc], writes=[bgso[j]])
                        for h in range(4):
                            pn, bn_ = (pN0, bN0) if h < 2 else (pN1, bN1)
                            cs = (h % 2) * 256
                            tr.op("dve", lambda e: e.scalar_tensor_tensor(out=zz[j][:, h * 256:(h + 1) * 256], in0=pn[:, cs:cs + 256],
                                                                          scalar=s_[:, 12 + h:13 + h], in1=gso[j][:, h * 256:(h + 1) * 256],
                                                                          op0=ALU.mult, op1=ALU.mult),
                                  reads=[bn_, bsm[j], bgso[j]], writes=[bzz[j]])
                        for k8 in range(8):
                            tr.op("pe", lambda e: e.transpose(out=pTb[:, k8 * 128:(k8 + 1) * 128], in_=zz[j][:, k8 * 128:(k8 + 1) * 128],
                                                              identity=K.ident_b[:, :]), reads=[bzz[j], b_const], writes=[bT], signal=(k8 == 7))
                        tr.op("act", lambda e: e.activation(out=zT[:, h0 * 2:h0 * 2 + 8, c4 * 128:(c4 + 1) * 128],
                                                            in_=pTb.rearrange("p (k n) -> p k n", k=8), func=AF.Copy),
                              reads=[bT], writes=[bzT])
                        tr.op("pool", lambda e: e.tensor_tensor(out=kw[j][:, :, :],
                                                                in0=km[i][:, h0 * 128:(h0 + 4) * 128].rearrange("p (h d) -> p h d", h=4),
                                                                in1=wS[:, c, h0:h0 + 4].unsqueeze(2).to_broadcast([128, 4, 128]),
                                                                op=ALU.mult), reads=[bin_[i], bwS], writes=[bkw[j]])
                        for h in range(4):
                            pu, bu = (pU0, bU0) if h < 2 else (pU1, bU1)
                            cs = (h % 2) * 256
                            tr.op("pe", lambda e: e.matmul(pu[:, cs:cs + 256], kw[j][:, h, :], vm[i][:, (h0 + h) * 256:(h0 + h + 1) * 256],
                                                           start=True, stop=True), reads=[bkw[j], bin_[i]], writes=[bu], signal=(h % 2 == 1))
                        for h in range(4):
                            tr.op("pe", lambda e: e.matmul(pSc[:, h:h + 1], kw[j][:, h, :], onesb[:, 0:1], start=True, stop=True),
                                  reads=[bkw[j], bc], writes=[bSc], signal=(h == 3))
                        for h in range(4):
                            pu, bu = (pU0, bU0) if h < 2 else (pU1, bU1)
                            cs = (h % 2) * 256
                            tr.op("dve", lambda e: e.scalar_tensor_tensor(out=C[:, h0 + h, :], in0=C[:, h0 + h, :],
                                                                          scalar=eBt[:, c, h0 + h:h0 + h + 1], in1=pu[:, cs:cs + 256],
                                                                          op0=ALU.mult, op1=ALU.add),
                                  reads=[bu, beBt, bC[hh]], writes=[bC[hh]])
                        tr.op("dve", lambda e: e.tensor_tensor(out=nv[:, h0:h0 + 4], in0=nv[:, h0:h0 + 4], in1=eBt[:, c, h0:h0 + 4],
                                                               op=ALU.mult), reads=[beBt, bC[hh]], writes=[bC[hh]])
                        tr.op("dve", lambda e: e.tensor_tensor(out=nv[:, h0:h0 + 4], in0=nv[:, h0:h0 + 4], in1=pSc[:, 0:4], op=ALU.add),
                              reads=[bSc, bC[hh]], writes=[bC[hh]])
                        tr.op("act", lambda e: e.activation(out=Cb[:, h0:h0 + 4, :], in_=C[:, h0:h0 + 4, :], func=AF.Copy),
                              reads=[bC[hh]], writes=[bCb[hh]])
                        tr.op("act", lambda e: e.activation(out=nb[:, h0:h0 + 4], in_=nv[:, h0:h0 + 4], func=AF.Copy),
                              reads=[bC[hh]], writes=[bCb[hh]])
                    if c4 == 3 or c == NCH - 1:
                        nt = (c4 + 1) * 128
                        t0 = (c - c4) * 128
                        tr.dma("sp", chz, [(zT_d[0:D, t0:t0 + nt].rearrange("(b p) t -> p b t", p=128), zT[:, :, 0:nt])],
                               reads=[bzT], writes=[DB["zT"]])
                tr.barrier()

        def mlp1(g, hT, W):
            def ep(mode, env, a):
                if mode == "init":
                    ph, t = env, a
                    E = P()
                    E.tmp = [t(f"u_t{i}", [128, 512], F32) for i in range(4)]
                    E.bt = [Buf() for _ in range(4)]
                    E.o = [t(f"u_o{i}", [128, 512], BF16) for i in range(4)]
                    E.bo = [Buf() for _ in range(4)]
                    E.cho = [tr.dma_chan(f"o{i}") for i in range(4)]
                    E.i = 0
                    return E
                E = env
                pi, c0, tok0, ps0, bp0, ps1, bp1 = a
                for fb, (ps, bp) in enumerate(((ps0, bp0), (ps1, bp1))):
                    k = E.i % 4
                    E.i += 1
                    tr.op("act", lambda e: e.activation(out=E.tmp[k][:, :], in_=ps[:, :], func=AF.Relu), reads=[bp], writes=[E.bt[k]])
                    tr.op("pool", lambda e: e.tensor_tensor(out=E.o[k][:, :], in0=E.tmp[k][:, :], in1=E.tmp[k][:, :], op=ALU.mult),
                          reads=[E.bt[k]], writes=[E.bo[k]])
                    r0 = c0 + fb * 128
                    tr.dma("sp", E.cho[k], [(uT_d[r0:r0 + 128, tok0:tok0 + 512], E.o[k][:, :])], reads=[E.bo[k]], writes=[DB["uT"]])
            gemm_fm(g, hT, W, [i * 256 for i in range(DFF // 256)], ep, "mlp")

        phase_tables()
        class HT:
            def __enter__(self):
                self.cm = nc.sbuf_tensor("hT", [128, DC, TG], BF16)
                self.t = self.cm.__enter__()
                self.buf = Buf("hT")
                return self

            def __exit__(self, *a):
                return self.cm.__exit__(*a)

        def sc(name):
            return nc.named_scope(name)

        if True:
            src_x = xT_in
            for l in range(n_layers):
                last = (l == n_layers - 1)
                j = l // 2
                is_ret = (l % 2 == 0)
                for g in range(NG):
                    with HT() as hT:
                        with sc(f"L{l}g{g}_norm0"):
                            phase_norm(g, hT, src_x, None, None, l * 4 + 0, xT_d if l == 0 else None)
                        with sc(f"L{l}g{g}_inproj"):
                            if is_ret:
                                ret_inproj(g, hT, ret_w_in[j])
                            else:
                                ml_inproj(g, hT, ml_w_in[j])
                with sc(f"L{l}_mixer"):
                    if is_ret:
                        ret_mixer(None)
                    else:
                        ml_mixer(j)
                KCo = 32 if is_ret else 16
                Wo = ret_w_out[j] if is_ret else ml_w_out[j]
                src_x = xT_d
                for g in range(NG):
                    with sc(f"L{l}g{g}_outproj"):
                        gemm_stream(g, zT_d, DB["zT"], KCo, Wo, yT_d, DB["yT"])
                    with HT() as hT:
                        with sc(f"L{l}g{g}_an1"):
                            phase_norm(g, hT, xT_d, yT_d, l * 4 + 1, l * 4 + 2, xT_d)
                        with sc(f"L{l}g{g}_mlp1"):
                            mlp1(g, hT, w1_in[l])
                    with sc(f"L{l}g{g}_mlp2"):
                        gemm_stream(g, uT_d, DB["uT"], DFF // 128, w2_in[l], yT_d, DB["yT"])
                    with sc(f"L{l}g{g}_an2"):
                        if last:
                            phase_norm(g, None, xT_d, yT_d, l * 4 + 3, None, outT)
                        else:
                            phase_norm(g, None, xT_d, yT_d, l * 4 + 3, None, xT_d)
        tr.barrier()
        print("ninst", tr.ninst, "nwait", tr.nwait, flush=True)
    return nc_real


def make_in_maps(inputs, T, cores, exch=0):
    HC = host_consts(T)
    x = np.asarray(inputs["x"], dtype=np.float32)
    pos = np.asarray(inputs["positions"]).astype(np.int32)
    ng = np.asarray(inputs["norm_g"], dtype=np.float32)
    L = ng.shape[0]
    gcol = np.zeros((128, 16, DC), np.float32)
    gcol[:, :L * 4, :] = ng.reshape(L * 4, DC, 128).transpose(2, 0, 1)
    shared = {
        "gcol": gcol,
        "ret_w_in": np.ascontiguousarray(inputs["ret_w_in"], dtype=np.float32),
        "ret_w_out": np.ascontiguousarray(inputs["ret_w_out"], dtype=np.float32),
        "ml_w_in": np.ascontiguousarray(inputs["mlstm_w_in"], dtype=np.float32),
        "ml_bg": np.ascontiguousarray(np.broadcast_to(np.asarray(inputs["mlstm_b_gate"], np.float32)[None], (128, 2, 16))),
        "ml_ng": np.ascontiguousarray(np.broadcast_to(np.asarray(inputs["mlstm_norm_g"], np.float32)[None], (128, 2, D))),
        "ml_w_out": np.ascontiguousarray(inputs["mlstm_w_out"], dtype=np.float32),
        "mlp_w1": np.ascontiguousarray(inputs["mlp_w1"], dtype=np.float32),
        "mlp_w2": np.ascontiguousarray(inputs["mlp_w2"], dtype=np.float32),
    }
    for n in CONST_NAMES:
        shared["c_" + n] = HC[n]
    maps = []
    for (b, t0) in cores:
        m = dict(shared)
        m["xT"] = np.ascontiguousarray(x[b, t0:t0 + T, :].T)
        m["pos"] = np.ascontiguousarray(pos[b, t0:t0 + T].reshape(1, T))
        m["pos_tm"] = np.ascontiguousarray(pos[b, t0:t0 + T].reshape(T // 128, 128).T)
        if exch:
            sw = np.zeros((128, 8), np.float32)
            if t0 > 0:
                sw[:, cores.index((b, t0 - T))] = 1.0
            m["selw"] = sw
        maps.append(m)
    return maps


def kernel(**inputs):
    x = np.asarray(inputs["x"])
    B, S, _ = x.shape
    T = S
    cores = [(b, 0) for b in range(B)]
    nc = build(T, exch=0)
    in_maps = make_in_maps(inputs, T, cores, exch=0)
    res = run_bass_kernel_spmd(nc, in_maps, core_ids=list(range(len(cores))))
    out = np.zeros((B, S, D), np.float32)
    for i, (b, t0) in enumerate(cores):
        out[b, t0:t0 + T, :] = np.asarray(res.results[i]["outT"]).T
    return out
```
